# Optimizing a Trainium2 kernel written in Bass

```python
import jax, jax.numpy as jnp
from jax import lax
import numpy as np

D_MODEL = 1024
BATCH = 8
SEQ = 2048
DEPTH = 1

D_MIX = D_MODEL
D_MLSTM = D_MIX // 2
D_ATTN = D_MIX - D_MLSTM
D_IN_PROJ = 2 * D_MLSTM + 3 * D_ATTN
MLSTM_HEADS = 4
MLSTM_HEAD_DIM = D_MLSTM // MLSTM_HEADS
MLSTM_QKV_BLOCK = 4
MLSTM_CONV = 5
MLSTM_CHUNK = 64
ATTN_HEADS = 8
ATTN_HEAD_DIM = D_ATTN // ATTN_HEADS
ROPE_DIM = ATTN_HEAD_DIM // 4
ROPE_THETA = 500000.0
DILATED_PATTERNS = ((128, 1), (512, 4), (2048, 16))
BAND_BLOCK = 64
N_EXPERTS = 16
EC_CAPACITY = 2
D_EXPERT = 2816
NORM_EPS = 1e-6
NEG_INF = -1e30

kernel_name = "hybrid_mlstm_dilated_attn_ec_moe"


def rms_norm(x, g):
    xf = x.astype(jnp.float32)
    y = xf * lax.rsqrt(jnp.mean(xf * xf, axis=-1, keepdims=True) + NORM_EPS)
    return (y * g.astype(jnp.float32)).astype(x.dtype)


def rope_partial(t, positions):
    half = ROPE_DIM // 2
    inv_freq = ROPE_THETA ** (-2.0 * jnp.arange(half, dtype=jnp.float32) / ROPE_DIM)
    ang = positions.astype(jnp.float32)[:, None] * inv_freq[None, :]
    cos, sin = jnp.cos(ang), jnp.sin(ang)
    t1, t2 = t[..., :half], t[..., half:ROPE_DIM]
    return jnp.concatenate([t1 * cos - t2 * sin, t2 * cos + t1 * sin, t[..., ROPE_DIM:]], axis=-1)


def mlstm_chunkwise(q, k, v, i_pre, f_pre):
    B, H, S, DH = q.shape
    L = MLSTM_CHUNK
    NC = S // L
    qc = q.reshape(B, H, NC, L, DH)
    kc = k.reshape(B, H, NC, L, DH)
    vc = v.reshape(B, H, NC, L, DH)
    ig = i_pre.reshape(B, H, NC, L)
    b = jnp.cumsum(jax.nn.log_sigmoid(f_pre).reshape(B, H, NC, L), axis=-1)
    g = b[..., -1]
    a = g[..., None] - b + ig

    def step(carry, inp):
        C, n, m = carry
        k_c, v_c, a_c, g_c = inp
        m_new = jnp.maximum(g_c + m, a_c.max(-1))
        decay = jnp.exp(g_c + m - m_new)
        w = jnp.exp(a_c - m_new[..., None])
        C_new = decay[..., None, None] * C + jnp.einsum('bhs,bhsd,bhse->bhde', w, v_c, k_c)
        n_new = decay[..., None] * n + jnp.einsum('bhs,bhse->bhe', w, k_c)
        return (C_new, n_new, m_new), (C, n, m)

    init = (jnp.zeros((B, H, DH, DH), jnp.float32), jnp.zeros((B, H, DH), jnp.float32),
            jnp.zeros((B, H), jnp.float32))
    xs = (jnp.moveaxis(kc, 2, 0), jnp.moveaxis(vc, 2, 0), jnp.moveaxis(a, 2, 0), jnp.moveaxis(g, 2, 0))
    _, (C_prev, n_prev, m_prev) = lax.scan(step, init, xs)
    C_prev = jnp.moveaxis(C_prev, 0, 2)
    n_prev = jnp.moveaxis(n_prev, 0, 2)
    m_prev = jnp.moveaxis(m_prev, 0, 2)

    lower = jnp.tril(jnp.ones((L, L), dtype=bool))
    log_D = jnp.where(lower, b[..., :, None] - b[..., None, :] + ig[..., None, :], NEG_INF)
    inter_log = b + m_prev[..., None]
    m_t = jnp.maximum(inter_log, log_D.max(-1))
    W = jnp.exp(log_D - m_t[..., None]) * jnp.einsum('bhctd,bhcsd->bhcts', qc, kc)
    inter_scale = jnp.exp(inter_log - m_t)
    num = (jnp.einsum('bhcts,bhcsd->bhctd', W, vc)
           + inter_scale[..., None] * jnp.einsum('bhcde,bhcte->bhctd', C_prev, qc))
    den = W.sum(-1) + inter_scale * jnp.einsum('bhcte,bhce->bhct', qc, n_prev)
    h = num / jnp.maximum(jnp.abs(den), jnp.exp(-m_t))[..., None]
    return h.reshape(B, H, S, DH)


def mlstm_mixer(x_m, z, conv_w, conv_b, wq, wk, wv, w_if_f, b_if_f, w_if_b, b_if_b, norm_g, skip):
    B, S, _ = x_m.shape
    H, DH = MLSTM_HEADS, MLSTM_HEAD_DIM
    xf = x_m.astype(jnp.float32)
    x_c = lax.conv_general_dilated(
        xf, conv_w.astype(jnp.float32)[:, None, :], window_strides=(1,),
        padding=[(MLSTM_CONV // 2, MLSTM_CONV // 2)], dimension_numbers=('NWC', 'WIO', 'NWC'),
        feature_group_count=D_MLSTM) + conv_b.astype(jnp.float32)
    x_c = jax.nn.silu(x_c)

    def blockdiag(t, w):
        tb = t.reshape(B, S, D_MLSTM // MLSTM_QKV_BLOCK, MLSTM_QKV_BLOCK)
        return jnp.einsum('bsgi,gio->bsgo', tb, w.astype(jnp.float32)).reshape(B, S, D_MLSTM)

    q = blockdiag(x_c, wq)
    k = blockdiag(x_c, wk) * (MLSTM_HEAD_DIM ** -0.5)
    v = blockdiag(xf, wv)
    qkv = jnp.concatenate([q, k, v], axis=-1)

    def heads(t):
        return t.reshape(B, S, H, DH).transpose(0, 2, 1, 3)

    qh, kh, vh = heads(q), heads(k), heads(v)

    def run(w_if, b_if, flip):
        gates = qkv @ w_if.astype(jnp.float32) + b_if.astype(jnp.float32)
        args = (qh, kh, vh, gates[..., :H].transpose(0, 2, 1), gates[..., H:].transpose(0, 2, 1))
        if flip:
            args = tuple(jnp.flip(t, axis=2) for t in args)
        h = mlstm_chunkwise(*args)
        return jnp.flip(h, axis=2) if flip else h

    h = run(w_if_f, b_if_f, False) + run(w_if_b, b_if_b, True)
    h = rms_norm(h.transpose(0, 2, 1, 3), norm_g.reshape(H, DH)).reshape(B, S, D_MLSTM)
    return (h + skip.astype(jnp.float32) * x_c) * jax.nn.silu(z.astype(jnp.float32))


def banded_window_stats(q, k, v, half):
    lead = q.shape[:-2]
    L, D = q.shape[-2], q.shape[-1]
    blk = BAND_BLOCK
    nb = -(-L // blk)
    pad = nb * blk - L
    padw = [(0, 0)] * len(lead)
    qb = jnp.pad(q, padw + [(0, pad), (0, 0)]).reshape(*lead, nb, blk, D)

    def windows(t):
        tp = jnp.pad(t, padw + [(blk, blk + pad), (0, 0)]).reshape(*lead, nb + 2, blk, D)
        return jnp.concatenate([tp[..., :-2, :, :], tp[..., 1:-1, :, :], tp[..., 2:, :, :]], axis=-2)

    kw, vw = windows(k), windows(v)
    qpos = jnp.arange(nb)[:, None] * blk + jnp.arange(blk)[None, :]
    kpos = jnp.arange(nb)[:, None] * blk - blk + jnp.arange(3 * blk)[None, :]
    mask = ((jnp.abs(qpos[:, :, None] - kpos[:, None, :]) <= half)
            & (kpos[:, None, :] >= 0) & (kpos[:, None, :] < L))
    scores = jnp.where(mask, jnp.einsum('...nqd,...nkd->...nqk', qb, kw), NEG_INF)
    m = scores.max(-1)
    p = jnp.exp(scores - m[..., None])
    s = p.sum(-1)
    num = jnp.einsum('...nqk,...nkd->...nqd', p, vw)
    return (m.reshape(*lead, nb * blk)[..., :L], s.reshape(*lead, nb * blk)[..., :L],
            num.reshape(*lead, nb * blk, D)[..., :L, :])


def dilated_branch(q, k, v, dil, half):
    B, H, S, D = q.shape

    def to_res(t):
        return t.reshape(B, H, S // dil, dil, D).swapaxes(2, 3)

    m, s, num = banded_window_stats(to_res(q), to_res(k), to_res(v), half)
    return (m.swapaxes(2, 3).reshape(B, H, S), s.swapaxes(2, 3).reshape(B, H, S),
            num.swapaxes(2, 3).reshape(B, H, S, D))


def dilated_attention_mixer(qa, ka, va):
    B, S, _ = qa.shape

    def heads(t):
        return t.astype(jnp.float32).reshape(B, S, ATTN_HEADS, ATTN_HEAD_DIM).transpose(0, 2, 1, 3)

    pos = jnp.arange(S)
    q = rope_partial(heads(qa), pos) * (ATTN_HEAD_DIM ** -0.5)
    k = rope_partial(heads(ka), pos)
    v = heads(va)
    stats = [dilated_branch(q, k, v, dil, win // (2 * dil)) for win, dil in DILATED_PATTERNS]
    m_all = jnp.stack([st[0] for st in stats])
    w = jnp.exp(m_all - m_all.max(0))
    den = jnp.sum(w * jnp.stack([st[1] for st in stats]), axis=0)
    num = jnp.sum(w[..., None] * jnp.stack([st[2] for st in stats]), axis=0)
    out = num / den[..., None]
    return out.transpose(0, 2, 1, 3).reshape(B, S, D_ATTN)


def expert_choice_ffn(h, w_router, w1, w3, w2):
    B, S, D = h.shape
    cap = EC_CAPACITY * S // N_EXPERTS
    logits = jnp.einsum('bsd,de->bse', h.astype(jnp.float32), w_router.astype(jnp.float32))
    aff = jax.nn.softmax(logits, axis=-1)
    gate, idx = lax.top_k(aff.transpose(0, 2, 1), cap)
    xs = jax.vmap(lambda hb, ib: hb[ib])(h, idx)
    up = jnp.einsum('becd,edf->becf', xs, w1)
    gt = jnp.einsum('becd,edf->becf', xs, w3)
    y = jnp.einsum('becf,efd->becd', jax.nn.silu(up) * gt, w2) * gate[..., None].astype(h.dtype)
    return jax.vmap(lambda yb, ib: jax.ops.segment_sum(
        yb.reshape(-1, D), ib.reshape(-1), num_segments=S))(y, idx)


def setup_inputs(seed: int = 0) -> dict:
    key = jax.random.key(seed)
    ks = jax.random.split(key, 24)

    def nrm(k, shape, scale):
        return jax.random.normal(k, shape, jnp.float32) * scale

    H = MLSTM_HEADS
    nblk = D_MLSTM // MLSTM_QKV_BLOCK
    f_bias = jnp.linspace(3.0, 6.0, H, dtype=jnp.float32)[None, :]
    return {
        "x": nrm(ks[0], (BATCH, SEQ, D_MODEL), 1.0),
        "norm1_g": 1.0 + nrm(ks[1], (DEPTH, D_MODEL), 0.02),
        "w_in": nrm(ks[2], (DEPTH, D_MODEL, D_IN_PROJ), D_MODEL ** -0.5),
        "conv_w": nrm(ks[3], (DEPTH, MLSTM_CONV, D_MLSTM), MLSTM_CONV ** -0.5),
        "conv_b": nrm(ks[4], (DEPTH, D_MLSTM), 0.01),
        "wq_m": nrm(ks[5], (DEPTH, nblk, MLSTM_QKV_BLOCK, MLSTM_QKV_BLOCK), MLSTM_QKV_BLOCK ** -0.5),
        "wk_m": nrm(ks[6], (DEPTH, nblk, MLSTM_QKV_BLOCK, MLSTM_QKV_BLOCK), MLSTM_QKV_BLOCK ** -0.5),
        "wv_m": nrm(ks[7], (DEPTH, nblk, MLSTM_QKV_BLOCK, MLSTM_QKV_BLOCK), MLSTM_QKV_BLOCK ** -0.5),
        "w_if_fwd": nrm(ks[8], (DEPTH, 3 * D_MLSTM, 2 * H), (3 * D_MLSTM) ** -0.5),
        "b_if_fwd": jnp.concatenate([nrm(ks[9], (DEPTH, H), 0.1),
                                     f_bias + nrm(ks[10], (DEPTH, H), 0.1)], axis=-1),
        "w_if_bwd": nrm(ks[11], (DEPTH, 3 * D_MLSTM, 2 * H), (3 * D_MLSTM) ** -0.5),
        "b_if_bwd": jnp.concatenate([nrm(ks[12], (DEPTH, H), 0.1),
                                     f_bias + nrm(ks[13], (DEPTH, H), 0.1)], axis=-1),
        "mlstm_norm_g": 1.0 + nrm(ks[14], (DEPTH, D_MLSTM), 0.02),
        "mlstm_skip": 1.0 + nrm(ks[15], (DEPTH, D_MLSTM), 0.02),
        "attn_norm_g": 1.0 + nrm(ks[16], (DEPTH, D_ATTN), 0.02),
        "w_out": nrm(ks[17], (DEPTH, D_MIX, D_MODEL), D_MIX ** -0.5),
        "norm2_g": 1.0 + nrm(ks[18], (DEPTH, D_MODEL), 0.02),
        "w_router": nrm(ks[19], (DEPTH, D_MODEL, N_EXPERTS), D_MODEL ** -0.5),
        "w1": nrm(ks[20], (DEPTH, N_EXPERTS, D_MODEL, D_EXPERT), D_MODEL ** -0.5),
        "w3": nrm(ks[21], (DEPTH, N_EXPERTS, D_MODEL, D_EXPERT), D_MODEL ** -0.5),
        "w2": nrm(ks[22], (DEPTH, N_EXPERTS, D_EXPERT, D_MODEL), D_EXPERT ** -0.5),
        "norm_f_g": 1.0 + nrm(ks[23], (D_MODEL,), 0.02),
    }


def reference(x, norm1_g, w_in, conv_w, conv_b, wq_m, wk_m, wv_m, w_if_fwd, b_if_fwd,
              w_if_bwd, b_if_bwd, mlstm_norm_g, mlstm_skip, attn_norm_g, w_out, norm2_g,
              w_router, w1, w3, w2, norm_f_g):
    splits = [D_MLSTM, 2 * D_MLSTM, 2 * D_MLSTM + D_ATTN, 2 * D_MLSTM + 2 * D_ATTN]
    for l in range(DEPTH):
        h = rms_norm(x, norm1_g[l])
        proj = h @ w_in[l]
        x_m, z, qa, ka, va = jnp.split(proj, splits, axis=-1)
        y_m = mlstm_mixer(x_m, z, conv_w[l], conv_b[l], wq_m[l], wk_m[l], wv_m[l],
                          w_if_fwd[l], b_if_fwd[l], w_if_bwd[l], b_if_bwd[l],
                          mlstm_norm_g[l], mlstm_skip[l])
        y_a = rms_norm(dilated_attention_mixer(qa, ka, va), attn_norm_g[l])
        mixed = jnp.concatenate([y_m, y_a], axis=-1).astype(x.dtype)
        x = x + mixed @ w_out[l]
        h = rms_norm(x, norm2_g[l])
        x = x + expert_choice_ffn(h, w_router[l], w1[l], w3[l], w2[l])
    return rms_norm(x, norm_f_g)
```

```python
import numpy as np
import concourse.bass as bass
import concourse.mybir as mybir

F32 = mybir.dt.float32
BF16 = mybir.dt.bfloat16
I32 = mybir.dt.int32
U8 = mybir.dt.uint8
AF = mybir.ActivationFunctionType
ALU = mybir.AluOpType
AX = mybir.AxisListType
DTSIZE = {F32: 4, BF16: 2, I32: 4, U8: 1}

ENGS = ["pe", "act", "dve", "pool", "sp"]
SAME_ENG_SYNC = {"pe": False, "act": True, "dve": True, "pool": True, "sp": False}


class Op:
    __slots__ = ("eng", "fn", "deps", "is_dma", "semkey", "semval", "signal", "sigcount", "gid")

    def __init__(self, eng, fn):
        self.eng = eng
        self.fn = fn
        self.deps = set()
        self.is_dma = False
        self.semkey = None
        self.semval = 0
        self.signal = False
        self.sigcount = 0
        self.gid = 0


class Prog:
    def __init__(self, nc):
        self.nc = nc
        self.ops = {e: [] for e in ENGS}
        self.lastw = {}
        self.readers = {}
        self.dmacount = {}
        self.nops = 0
        self.barrier_set = None
        self.barrier_applied = {e: True for e in ENGS}
        self.last_dma = {}

    def _add(self, op, reads, writes):
        op.gid = self.nops
        self.nops += 1
        deps = op.deps
        for r in reads:
            w = self.lastw.get(r)
            if w is not None:
                deps.add(w)
        for r in writes:
            w = self.lastw.get(r)
            if w is not None:
                deps.add(w)
            for o in self.readers.get(r, {}).values():
                deps.add(o)
        rk = (op.eng, op.semkey) if op.is_dma else op.eng
        for r in reads:
            self.readers.setdefault(r, {})[rk] = op
        for r in writes:
            self.lastw[r] = op
            self.readers[r] = {}
        if not self.barrier_applied[op.eng]:
            deps |= self.barrier_set
            self.barrier_applied[op.eng] = True
        deps.discard(op)
        self.ops[op.eng].append(op)
        return op

    def op(self, eng, fn, reads=(), writes=()):
        return self._add(Op(eng, fn), reads, writes)

    RING = 16

    def dma(self, eng, out, in_, semkey=None, reads=(), writes=(), **kw):
        def fn(e, out=out, in_=in_, kw=kw):
            return e.dma_start(out=out, in_=in_, **kw)
        op = Op(eng, fn)
        op.is_dma = True
        rc = self.__dict__.setdefault("_ringcnt", {})
        i = rc.get(eng, 0)
        rc[eng] = i + 1
        semkey = "%s_r%d" % (eng, i % self.RING)
        op.semkey = semkey
        prev = self.last_dma.get(semkey)
        if prev is not None:
            op.deps.add(prev)
        self.dmacount[semkey] = self.dmacount.get(semkey, 0) + 16
        op.semval = self.dmacount[semkey]
        self.last_dma[semkey] = op
        return self._add(op, reads, writes)


    def mm(self, out, lhsT, rhs, start=True, stop=True, reads=(), writes=(), **kw):
        return self.op("pe", lambda e: e.matmul(out, lhsT=lhsT, rhs=rhs, start=start, stop=stop, **kw), reads, writes)

    def tr(self, out, in_, ident, reads=(), writes=()):
        return self.op("pe", lambda e: e.transpose(out, in_, ident), reads, writes)

    def act(self, out, in_, func, reads=(), writes=(), eng="act", **kw):
        return self.op(eng, lambda e: e.activation(out=out, in_=in_, func=func, **kw), reads, writes)

    def tt(self, eng, out, in0, in1, op, reads=(), writes=()):
        return self.op(eng, lambda e: e.tensor_tensor(out=out, in0=in0, in1=in1, op=op), reads, writes)

    def ts(self, eng, out, in0, s1, op0, s2=None, op1=None, reads=(), writes=(), **kw):
        if op1 is None:
            return self.op(eng, lambda e: e.tensor_scalar(out=out, in0=in0, scalar1=s1, scalar2=None, op0=op0, **kw), reads, writes)
        return self.op(eng, lambda e: e.tensor_scalar(out=out, in0=in0, scalar1=s1, scalar2=s2, op0=op0, op1=op1, **kw), reads, writes)

    def stt(self, out, in0, scalar, in1, op0, op1, reads=(), writes=(), eng="dve"):
        return self.op(eng, lambda e: e.scalar_tensor_tensor(out=out, in0=in0, scalar=scalar, in1=in1, op0=op0, op1=op1), reads, writes)

    def copy(self, eng, out, in_, reads=(), writes=()):
        if eng == "act":
            return self.op(eng, lambda e: e.copy(out=out, in_=in_), reads, writes)
        return self.op(eng, lambda e: e.tensor_copy(out=out, in_=in_), reads, writes)

    def recip(self, out, in_, reads=(), writes=()):
        return self.op("dve", lambda e: e.reciprocal(out=out, in_=in_), reads, writes)

    def memset(self, eng, ap, val, reads=(), writes=()):
        return self.op(eng, lambda e: e.memset(ap, val), reads, writes)

    def barrier(self):
        s = set()
        for e in ENGS:
            if self.ops[e]:
                s.add(self.ops[e][-1])
        for op in self.last_dma.values():
            s.add(op)
        self.barrier_set = s
        self.barrier_applied = {e: False for e in ENGS}

    def emit(self, final_dma_keys):
        nc = self.nc
        for e in ENGS:
            for op in self.ops[e]:
                for d in op.deps:
                    if not d.is_dma:
                        if d.eng == op.eng and not SAME_ENG_SYNC[op.eng]:
                            continue
                        d.signal = True
        for e in ENGS:
            c = 0
            for op in self.ops[e]:
                if (not op.is_dma) and op.signal:
                    c += 1
                op.sigcount = c
        from contextlib import ExitStack
        with ExitStack() as es:
            engsem = {e: es.enter_context(nc.semaphore("S_" + e)) for e in ENGS if e != "sp"}
            dmasem = {k: es.enter_context(nc.semaphore("D_" + str(k))) for k in self.dmacount}
            print("semaphores used:", len(engsem) + len(dmasem), "ops:", {e: len(self.ops[e]) for e in ENGS})
            block = es.enter_context(nc.Block())

            def run(ename, eng):
                waited = {}
                for op in self.ops[ename]:
                    for d in sorted(op.deps, key=lambda o: o.gid):
                        if d.is_dma:
                            key = ("d", d.semkey)
                            sem, val = dmasem[d.semkey], d.semval
                        else:
                            if d.eng == op.eng and not SAME_ENG_SYNC[op.eng]:
                                continue
                            key = ("e", d.eng)
                            sem, val = engsem[d.eng], d.sigcount
                        if waited.get(key, 0) >= val:
                            continue
                        eng.wait_ge(sem, val)
                        waited[key] = val
                    inst = op.fn(eng)
                    if op.is_dma:
                        inst.then_inc(dmasem[op.semkey], 16)
                    elif op.signal:
                        inst.then_inc(engsem[op.eng], 1)
                if ename == "sp":
                    for k in sorted(self.dmacount):
                        if waited.get(("d", k), 0) < self.dmacount[k]:
                            eng.wait_ge(dmasem[k], self.dmacount[k])

            block.tensor(lambda e: run("pe", e))
            block.scalar(lambda e: run("act", e))
            block.vector(lambda e: run("dve", e))
            block.gpsimd(lambda e: run("pool", e))
            block.sync(lambda e: run("sp", e))


class Arena:
    def __init__(self, nc, nbytes):
        self.nc = nc
        self.t = nc.alloc_sbuf_tensor("arena", [128, nbytes], U8)
        self.nbytes = nbytes

    def at(self, off, shape, dt):
        n = int(np.prod(shape[1:])) * DTSIZE[dt]
        assert off + n <= self.nbytes, (off, n, self.nbytes)
        assert off % 4 == 0
        ap = self.t[0:shape[0], off:off + n].bitcast(dt)
        if len(shape) == 3:
            ap = ap.rearrange("p (a b) -> p a b", a=shape[1])
        elif len(shape) == 4:
            ap = ap.rearrange("p (a b c) -> p a b c", a=shape[1], b=shape[2])
        return ap


from concourse.bass_utils import run_bass_kernel_spmd

S = 2048
D = 1024
NT = 16
DIN = 2560
EPS = 1.0000001e-6
KB = 1024
NE = 16
CAP = 256
DFF = 2816
NFC = 22

O_CONST = 0
O_XM = 12 * KB
O_SZ = 28 * KB
O_MIX = 44 * KB
O_QA = 76 * KB
O_KA = 92 * KB
O_VA = 108 * KB
O_T = 125 * KB
ARENA = 206 * KB


def build_consts(P, A, dr):
    c = {}
    off = [O_CONST]

    def alloc(shape, dt):
        n = int(np.prod(shape[1:])) * DTSIZE[dt]
        n = (n + 31) // 32 * 32
        ap = A.at(off[0], shape, dt)
        off[0] += n
        assert off[0] <= 3 * KB
        return ap

    c["ident_b"] = alloc([128, 128], BF16)
    c["ident_f"] = alloc([128, 128], F32)
    c["ones_b"] = alloc([128, 128], BF16)
    c["ropePT"] = alloc([128, 128], BF16)
    c["g1"] = alloc([128, 8], F32)
    for k in ("ident_b", "ident_f", "ropePT", "g1"):
        P.dma("sp", c[k], dr[k], "c_" + k, writes=[k])
    P.memset("dve", c["ones_b"], 1.0, writes=["ones_b"])
    return c


def phase_A1(P, A, dr, c, ps):
    nc = P.nc
    xmT = A.at(O_XM, [128, 4, S], BF16)
    szT = A.at(O_SZ, [128, 4, S], BF16)
    qaT = A.at(O_QA, [128, 4, S], BF16)
    kaT = A.at(O_KA, [128, 4, S], BF16)
    va = A.at(O_VA, [128, NT, 8, 65], BF16)
    xTs = A.at(O_MIX, [128, 8, 512], F32)
    xTb = A.at(O_MIX + 16 * KB, [128, 8, 512], BF16)
    sq = A.at(O_MIX + 24 * KB, [128, 8, 512], BF16)
    Wb = A.at(O_T, [128, 8, DIN], BF16)
    Wst = [A.at(O_T + 40 * KB + i * 10 * KB, [128, DIN], F32) for i in range(2)]
    o = O_T + 60 * KB
    cosT = A.at(o, [128, S], BF16); o += 4 * KB
    sinT = A.at(o, [128, S], BF16); o += 4 * KB
    rstd_bc = A.at(o, [128, 512], F32); o += 2 * KB
    tmpA = [A.at(o + i * 2 * KB, [128, 512], F32) for i in range(2)]; o += 4 * KB
    tmpAb = [tmpA[i].bitcast(BF16)[:, 0:512] for i in range(2)]
    tmp1 = [A.at(o + i * 2 * KB, [128, 512], F32) for i in range(2)]; o += 4 * KB
    tmp2 = [A.at(o, [128, 512], F32) for i in range(2)]; o += 2 * KB
    rstd_t = A.at(o, [128, 4], F32); o += 32
    assert o <= ARENA

    P.dma("sp", cosT, dr["ropeC"], "c_cos", writes=["cosT"])
    P.dma("sp", sinT, dr["ropeS"], "c_sin", writes=["sinT"])
    P.memset("pool", va[:, :, :, 64:65], 1.0, writes=["va_ones"])

    for kc in range(8):
        s = kc % 2
        P.dma("sp", Wst[s], dr["w_in"][kc * 128:(kc + 1) * 128, :], "wst%d" % s, writes=["Wst%d" % s])
        if kc % 2 == 0:
            P.ts("dve", Wb[:, kc, :], Wst[s], c["g1"][:, kc:kc + 1], ALU.mult, reads=["Wst%d" % s, "g1"], writes=[("Wb", kc)])
        else:
            P.act(Wb[:, kc, :], Wst[s], AF.Copy, scale=c["g1"][:, kc:kc + 1], reads=["Wst%d" % s, "g1"], writes=[("Wb", kc)])

    xT_v = dr["xT"].rearrange("(kc p) t -> p kc t", p=128)
    ev = 0
    for n in range(4):
        tok = slice(n * 512, (n + 1) * 512)
        P.dma("sp", xTs, xT_v[:, :, tok], "xTs", writes=["xTs"])
        P.act(sq, xTs, AF.Square, reads=["xTs"], writes=["sq"])
        P.copy("dve", xTb, xTs, reads=["xTs"], writes=["xTb"])
        for kc in range(8):
            P.mm(ps[4][:, :], c["ones_b"], sq[:, kc, :], start=(kc == 0), stop=(kc == 7), reads=["sq", "ones_b"], writes=["ps4"])
        for tt in range(4):
            for kc in range(8):
                P.mm(ps[7][:, tt:tt + 1], sq[:, kc, tt * 128:(tt + 1) * 128], c["ones_b"][:, 0:1], start=(kc == 0), stop=(kc == 7),
                     reads=["sq", "ones_b"], writes=["ps7"])
        P.act(rstd_bc, ps[4][:, :], AF.Sqrt, bias=EPS, scale=1.0 / D, reads=["ps4"], writes=["rstd_bc"])
        P.recip(rstd_bc, rstd_bc, reads=["rstd_bc"], writes=["rstd_bc"])
        P.act(rstd_t, ps[7][:, 0:4], AF.Sqrt, bias=EPS, scale=1.0 / D, reads=["ps7"], writes=["rstd_t"])
        P.recip(rstd_t, rstd_t, reads=["rstd_t"], writes=["rstd_t"])
        for oc in range(16):
            b = oc % 4
            pb = ps[b]
            pk = "ps%d" % b
            for kc in range(8):
                P.mm(pb[:, :], Wb[:, kc, oc * 128:(oc + 1) * 128], xTb[:, kc, :], start=(kc == 0), stop=(kc == 7),
                     reads=["xTb", ("Wb", kc)], writes=[pk])
            if oc < 4:
                P.tt("dve", xmT[:, oc, tok], pb[:, :], rstd_bc, ALU.mult, reads=[pk, "rstd_bc"], writes=[("xmT", oc, n)])
            elif oc < 8:
                t = tmpA[ev % 2]; tk = "tmpA%d" % (ev % 2); ev += 1
                P.tt("dve", t, pb[:, :], rstd_bc, ALU.mult, reads=[pk, "rstd_bc"], writes=[tk])
                P.act(szT[:, oc - 4, tok], t, AF.Silu, reads=[tk], writes=[("szT", oc - 4, n)])
            else:
                isq = oc < 12
                dst = qaT if isq else kaT
                cc = (oc - 8) % 4
                i2 = ev % 2; ev += 1
                t = tmpAb[i2]; tk = "tmpA%d" % i2
                P.stt(t, pb[:, :], (0.125 if isq else 1.0), rstd_bc, ALU.mult, ALU.mult, reads=[pk, "rstd_bc"], writes=[tk])
                P.mm(ps[5][:, :], c["ropePT"], t, reads=[tk, "ropePT"], writes=["ps5"])
                t1 = tmp1[i2]; t1k = "tmp1%d" % i2
                t2 = tmp2[i2]; t2k = "tmp2"
                P.tt("pool", t1, t, cosT[:, tok], ALU.mult, reads=[tk, "cosT"], writes=[t1k])
                P.tt("dve", t2, ps[5][:, :], sinT[:, tok], ALU.mult, reads=["ps5", "sinT"], writes=[t2k])
                P.tt("pool", dst[:, cc, tok], t1, t2, ALU.add, reads=[t1k, t2k], writes=[("qaT" if isq else "kaT", cc, n)])
        for tt in range(4):
            T = n * 4 + tt
            for kc in range(8):
                P.mm(ps[6][:, :], xTb[:, kc, tt * 128:(tt + 1) * 128], Wb[:, kc, 2048:2560], start=(kc == 0), stop=(kc == 7),
                     reads=["xTb", ("Wb", kc)], writes=["ps6"])
            P.act(va[:, T, :, 0:64], ps[6][:, :].rearrange("p (h d) -> p h d", h=8), AF.Copy, scale=rstd_t[:, tt:tt + 1],
                  reads=["ps6", "rstd_t"], writes=[("va", T)])
    return dict(xmT=xmT, szT=szT, qaT=qaT, kaT=kaT, va=va)


def rope_tables():
    half = 8
    inv_freq = 500000.0 ** (-2.0 * np.arange(half, dtype=np.float64) / 16.0)
    pos = np.arange(S, dtype=np.float64)
    ang = (pos[:, None].astype(np.float32) * inv_freq[None, :].astype(np.float32)).astype(np.float32).astype(np.float64)
    cosT = np.ones((128, S), np.float32)
    sinT = np.zeros((128, S), np.float32)
    for p in range(128):
        f = p % 64
        if f < 16:
            cosT[p] = np.cos(ang[:, f % 8])
            sinT[p] = np.sin(ang[:, f % 8])
    PT = np.zeros((128, 128), np.float32)
    for m in range(128):
        f = m % 64
        if f < 8:
            PT[m + 8, m] = -1.0
        elif f < 16:
            PT[m - 8, m] = 1.0
    return cosT, sinT, PT


def toeplitz_mask():
    U0 = 1408
    W = 2944
    p = np.arange(128)[:, None]
    u = np.arange(W)[None, :]
    d = p - u + U0
    ad = np.abs(d)
    cnt = (ad <= 64).astype(np.float32) + ((d % 4 == 0) & (ad <= 256)) + ((d % 16 == 0) & (ad <= 1024))
    return cnt.astype(np.float32)


def phase_A3(P, A, dr, c, ps, a1):
    qaT, kaT, va = a1["qaT"], a1["kaT"], a1["va"]
    mixedT = A.at(O_MIX, [128, 8, S], BF16)
    o = O_T
    yA = A.at(o, [128, NT, 512], F32); o += 32 * KB
    tmask = A.at(o, [128, 2944], BF16); o += 5888
    sqt = A.at(o, [128, S], BF16); o += 4 * KB
    E = [A.at(o + i * KB, [128, 512], BF16) for i in range(3)]; o += 3 * KB
    PT = [A.at(o + i * KB, [128, 512], BF16) for i in range(4)]; o += 4 * KB
    mx = A.at(o, [128, 16, 4], F32); o += 256
    mx2 = A.at(o, [128, 16], F32); o += 64
    negm = A.at(o, [128, 8], F32); o += 32
    rden = [A.at(o + i * 16, [128, 4], F32) for i in range(2)]; o += 32
    ssa = A.at(o, [128, 16], F32); o += 64
    rstdA = A.at(o, [128, 16], F32); o += 64
    halfones = A.at(o, [128, 2, 128], BF16); o += 512
    attn_g = A.at(o, [128, 4], F32); o += 32
    yn = A.at(o, [128, 4, 512], BF16); o += 4 * KB
    junk = A.at(o, [128, 512], BF16); o += KB
    assert o <= ARENA

    P.dma("sp", tmask, dr["tmask"], "c_tmask", writes=["tmask"])
    P.dma("sp", halfones, dr["halfones"], "c_halfones", writes=["halfones"])
    P.dma("sp", attn_g, dr["attn_g"], "c_attn_g", writes=["attn_g"])

    nb = 0
    for qk, src, nm in ((0, qaT, "qaT"), (1, kaT, "kaT")):
        for cc in range(4):
            P.act(sqt, src[:, cc, :], AF.Square, reads=[(nm, cc, n) for n in range(4)], writes=["sqt"])
            for a in range(2):
                for g in range(4):
                    b = nb % 4; nb += 1
                    P.mm(ps[b][:, :], halfones[:, a, :], sqt[:, g * 512:(g + 1) * 512], reads=["sqt", "halfones"], writes=["ps%d" % b])
                    P.op("dve", lambda e, b=b, col=qk * 8 + cc * 2 + a, g=g: e.reduce_max(out=mx[:, col, g:g + 1], in_=ps[b][:, :], axis=AX.X),
                         reads=["ps%d" % b], writes=["mx"])
    P.op("dve", lambda e: e.reduce_max(out=mx2, in_=mx, axis=AX.X), reads=["mx"], writes=["mx2"])
    P.tt("dve", negm, mx2[:, 0:8], mx2[:, 8:16], ALU.mult, reads=["mx2"], writes=["negm"])
    P.act(negm, negm, AF.Sqrt, reads=["negm"], writes=["negm"])
    P.ts("dve", negm, negm, -1.0, ALU.mult, reads=["negm"], writes=["negm"])

    SKEW = 2
    iters = []
    grp = 0
    for h in range(8):
        for G in range(4):
            Js = list(range(max(0, 4 * G - 8), min(15, 4 * G + 3 + 8) + 1))
            lastJ = [max(j for j in Js if abs(j - (4 * G + i)) <= 8) for i in range(4)]
            firstJ = [min(j for j in Js if abs(j - (4 * G + i)) <= 8) for i in range(4)]
            for J in Js:
                iters.append((h, G, J, firstJ, lastJ, J == Js[-1], grp))
            grp += 1
    n_it = len(iters)
    sbanks = [0, 1, 6]

    def front(it):
        h, G, J = iters[it][0:3]
        cc, a = h // 2, h % 2
        pr = slice(a * 64, a * 64 + 64)
        sb = sbanks[it % 3]
        e_i = it % 3
        p_i = it % 4
        P.mm(ps[sb][:, :], kaT[pr, cc, J * 128:(J + 1) * 128], qaT[pr, cc, G * 512:(G + 1) * 512],
             reads=[("kaT", cc, J // 4), ("qaT", cc, G)], writes=["ps%d" % sb])
        P.act(E[e_i], ps[sb][:, :], AF.Exp, bias=negm[:, h:h + 1], reads=["ps%d" % sb, "negm"], writes=[("E", e_i)])
        u0 = 1408 - (J * 128 - G * 512)
        P.tt("dve", PT[p_i], E[e_i], tmask[:, u0:u0 + 512], ALU.mult, reads=[("E", e_i), "tmask"], writes=[("PT", p_i)])

    def back(it):
        h, G, J, firstJ, lastJ, is_last, g = iters[it]
        p_i = it % 4
        for i in range(4):
            I = 4 * G + i
            if abs(I - J) > 8:
                continue
            P.mm(ps[2 + i][:, 0:65], PT[p_i][:, i * 128:(i + 1) * 128], va[:, J, h, :], start=(J == firstJ[i]), stop=(J == lastJ[i]),
                 reads=[("PT", p_i), ("va", J), "va_ones"], writes=["ps%d" % (2 + i)])
        if is_last:
            rd = rden[g % 2]; rk = "rden%d" % (g % 2)
            for i in range(4):
                I = 4 * G + i
                P.recip(rd[:, i:i + 1], ps[2 + i][:, 64:65], reads=["ps%d" % (2 + i)], writes=[(rk, i)])
                P.ts("dve", yA[:, I, h * 64:(h + 1) * 64], ps[2 + i][:, 0:64], rd[:, i:i + 1], ALU.mult,
                     reads=["ps%d" % (2 + i), (rk, i)], writes=[("yA", I)])

    for it in range(n_it + SKEW):
        if it < n_it:
            front(it)
        if it - SKEW >= 0:
            back(it - SKEW)

    for T in range(NT):
        P.act(junk, yA[:, T, :], AF.Square, accum_out=ssa[:, T:T + 1], reads=[("yA", T)], writes=["junk", "ssa"])
    P.act(rstdA, ssa, AF.Sqrt, bias=EPS, scale=1.0 / 512, reads=["ssa"], writes=["rstdA"])
    P.recip(rstdA, rstdA, reads=["rstdA"], writes=["rstdA"])
    for Tg in range(4):
        for i in range(4):
            T = Tg * 4 + i
            P.ts("dve", yn[:, i, :], yA[:, T, :], rstdA[:, T:T + 1], ALU.mult, reads=[("yA", T), "rstdA"], writes=[("yn", i)])
        for cc in range(4):
            b = cc % 2
            pT = ps[b][:, :].bitcast(BF16)
            for i in range(4):
                P.tr(pT[:, i * 128:(i + 1) * 128], yn[:, i, cc * 128:(cc + 1) * 128], c["ident_b"], reads=[("yn", i), "ident_b"], writes=["ps%d" % b])
            P.act(mixedT[:, 4 + cc, Tg * 512:(Tg + 1) * 512], pT[:, 0:512], AF.Copy, scale=attn_g[:, cc:cc + 1],
                  reads=["ps%d" % b, "attn_g"], writes=[("mixedT", 4 + cc, Tg)])
    return dict(mixedT=mixedT, yA=yA)


def phase_A2(P, A, dr, c, ps, a1, mixedT):
    xmT, szT = a1["xmT"], a1["szT"]
    o = O_QA
    xcT = A.at(o, [128, 4, S], BF16); o += 16 * KB
    qT = A.at(o, [128, 4, S], BF16); o += 16 * KB
    kT = A.at(o, [128, 4, S], BF16); o += 16 * KB
    ktok = A.at(o, [128, NT, 4, 128], BF16); o += 16 * KB
    vtok = A.at(o, [128, NT, 4, 129], BF16); o += 16512
    hsum = A.at(o, [128, NT, 2, 128], F32); o += 16 * KB
    vTt = [A.at(o, [128, S], BF16) for i in range(2)]; o += 4 * KB
    acc = A.at(o, [128, S], F32); o += 8 * KB
    acc3 = acc.rearrange("p (t d) -> p t d", t=NT)
    wbd_f = acc[:, 0:1536].rearrange("p (a b) -> p a b", a=12)
    wbd = A.at(o, [128, 3, 4, 128], BF16); o += 3 * KB
    wif_f = acc[:, 1536:1728].rearrange("p (a b) -> p a b", a=12)
    wif = A.at(o, [128, 12, 16], BF16); o += 384
    bif = A.at(o, [128, 256], F32); o += KB
    gates = A.at(o, [128, NT, 16], F32); o += KB
    gflat = gates.rearrange("p t g -> p (t g)")
    small = {}
    for nm in ("lf", "ii", "bcs", "esc", "wk", "enb", "eg", "tg1", "tg2"):
        small[nm] = A.at(o, [128, 2, NT, 4], F32); o += 512
    tri_f = A.at(o, [128, 2, 128], F32); o += KB
    tri_b = A.at(o, [128, 2, 128], BF16); o += 512
    ones_f = A.at(o, [128, 128], F32); o += 512
    St32 = [A.at(o + i * 516, [128, 129], F32) for i in range(4)]; o += 4 * 516
    Stb = [[A.at(o + (i * 2 + p) * 260, [128, 129], BF16) for p in range(2)] for i in range(4)]; o += 8 * 260
    WT = [A.at(o + i * 256, [128, 128], BF16) for i in range(8)]; o += 2 * KB
    khat = [A.at(o + i * 256, [128, 128], BF16) for i in range(8)]; o += 2 * KB
    tmpS = [wbd.rearrange("p a b c -> p (a b c)")[:, i * 128:(i + 1) * 128] for i in range(8)]
    den2 = [A.at(o + i * 8, [128, 2], F32) for i in range(8)]; o += 64
    den = [A.at(o + i * 4, [128, 1], F32) for i in range(8)]; o += 32
    ssh = A.at(o, [128, NT], F32); o += 64
    rstdh = A.at(o, [128, NT], F32); o += 64
    hn = A.at(11 * KB, [128, 4, 128], BF16)
    tmpx = [A.at(3 * KB + i * 2 * KB, [128, 512], F32) for i in range(2)]
    tmpy = [A.at(7 * KB + i * 2 * KB, [128, 512], F32) for i in range(2)]
    convw = A.at(o, [128, 4, 5], F32); o += 96
    convb = A.at(o, [128, 4], F32); o += 32
    mng = A.at(o, [128, 4], F32); o += 32
    msk = A.at(o, [128, 4], F32); o += 32
    assert o <= ARENA, o

    for nm, ap in (("bif", bif), ("tri", tri_f), ("convw", convw), ("convb", convb), ("mng", mng), ("msk", msk)):
        P.dma("sp", ap, dr[nm], "c2_" + nm, writes=["c2_" + nm])
    P.dma("sp", wbd_f, dr["wbd"], "c2_wbd", writes=["acc"])
    P.dma("sp", wif_f, dr["wif"], "c2_wif", writes=["acc"])
    P.copy("dve", wbd.rearrange("p a b c -> p (a b) c"), wbd_f, reads=["acc"], writes=["wbd"])
    P.copy("dve", wif, wif_f, reads=["acc"], writes=["wif"])
    P.copy("dve", tri_b, tri_f, reads=["c2_tri"], writes=["tri_b"])
    P.memset("dve", ones_f, 1.0, writes=["ones_f"])
    P.memset("pool", vtok[:, :, :, 128:129], 1.0, writes=["vtok_ones"])

    inv_sqrt_dh = 128 ** -0.5
    nb = 0
    first_gate = True
    for hc in range(4):
        x = xmT[:, hc, :]
        xm_res = [("xmT", hc, n) for n in range(4)]
        P.ts("dve", acc, x, convw[:, hc, 2:3], ALU.mult, reads=xm_res + ["c2_convw"], writes=["acc"])
        for j in (0, 1, 3, 4):
            sh = j - 2
            if sh < 0:
                P.stt(acc[:, -sh:S], x[:, 0:S + sh], convw[:, hc, j:j + 1], acc[:, -sh:S], ALU.mult, ALU.add, reads=["acc"], writes=["acc"])
            else:
                P.stt(acc[:, 0:S - sh], x[:, sh:S], convw[:, hc, j:j + 1], acc[:, 0:S - sh], ALU.mult, ALU.add, reads=["acc"], writes=["acc"])
        P.act(xcT[:, hc, :], acc, AF.Silu, bias=convb[:, hc:hc + 1], reads=["acc", "c2_convb"], writes=[("xcT", hc)])
        vt = vTt[0]; vk = "vTt0"
        for g in range(4):
            tok = slice(g * 512, (g + 1) * 512)
            b = nb % 4; nb += 1
            P.mm(ps[b][:, :], wbd[:, 0, hc, :], xcT[:, hc, tok], reads=["wbd", ("xcT", hc)], writes=["ps%d" % b])
            P.copy("act", qT[:, hc, tok], ps[b][:, :], reads=["ps%d" % b], writes=[("qT", hc, g)])
            b = nb % 4; nb += 1
            P.mm(ps[b][:, :], wbd[:, 1, hc, :], xcT[:, hc, tok], reads=["wbd", ("xcT", hc)], writes=["ps%d" % b])
            P.ts("dve", kT[:, hc, tok], ps[b][:, :], inv_sqrt_dh, ALU.mult, reads=["ps%d" % b], writes=[("kT", hc, g)])
            b = nb % 4; nb += 1
            P.mm(ps[b][:, :], wbd[:, 2, hc, :], x[:, tok], reads=["wbd"] + xm_res, writes=["ps%d" % b])
            P.copy("act", vt[:, tok], ps[b][:, :], reads=["ps%d" % b], writes=[(vk, g)])
        for g in range(4):
            b = nb % 4; nb += 1
            for i in range(4):
                T = 4 * g + i
                P.mm(ps[b][:, i * 128:(i + 1) * 128], xcT[:, hc, T * 128:(T + 1) * 128], wbd[:, 1, hc, :], reads=["wbd", ("xcT", hc)], writes=["ps%d" % b])
            P.ts("dve", ktok[:, 4 * g:4 * g + 4, hc, :], ps[b][:, :].rearrange("p (i d) -> p i d", i=4), inv_sqrt_dh, ALU.mult,
                 reads=["ps%d" % b], writes=[("ktok", hc, g)])
            b = nb % 4; nb += 1
            for i in range(4):
                T = 4 * g + i
                P.mm(ps[b][:, i * 128:(i + 1) * 128], x[:, T * 128:(T + 1) * 128], wbd[:, 2, hc, :], reads=["wbd"] + xm_res, writes=["ps%d" % b])
            P.copy("act", vtok[:, 4 * g:4 * g + 4, hc, 0:128], ps[b][:, :].rearrange("p (i d) -> p i d", i=4),
                   reads=["ps%d" % b], writes=[("vtok", hc, g)])
        for ch, src, rk in ((hc, qT[:, hc, :], [("qT", hc, g) for g in range(4)]),
                            (4 + hc, kT[:, hc, :], [("kT", hc, g) for g in range(4)]),
                            (8 + hc, vt, [(vk, g) for g in range(4)])):
            for T in range(NT):
                P.mm(ps[4][:, T * 16:(T + 1) * 16], src[:, T * 128:(T + 1) * 128], wif[:, ch, :], reads=rk + ["wif"], writes=["ps4"])
            if first_gate:
                P.tt("dve", gflat, ps[4][:, 0:256], bif, ALU.add, reads=["ps4", "c2_bif"], writes=["gates"])
                first_gate = False
            else:
                P.tt("dve", gflat, ps[4][:, 0:256], gflat, ALU.add, reads=["ps4", "gates"], writes=["gates"])

    sm = small
    fl = lambda ap: ap.rearrange("p d t h -> p (d t h)")
    for d in range(2):
        i_pre = gates[:, :, d * 8:d * 8 + 4]
        f_pre = gates[:, :, d * 8 + 4:d * 8 + 8]
        P.act(sm["tg1"][:, d], f_pre, AF.Exp, scale=-1.0, reads=["gates"], writes=[("tg1", d)])
        P.act(sm["tg2"][:, d], sm["tg1"][:, d], AF.Ln, bias=1.0, reads=[("tg1", d)], writes=[("tg2", d)])
        P.ts("dve", sm["lf"][:, d], sm["tg2"][:, d], -1.0, ALU.mult, reads=[("tg2", d)], writes=[("lf", d)])
        P.copy("dve", sm["ii"][:, d], i_pre, reads=["gates"], writes=[("ii", d)])
    lf2 = [sm["lf"][:, d].rearrange("p t h -> p (t h)") for d in range(2)]
    P.mm(ps[5][:, 0:64], tri_f[:, 0, :], lf2[0], reads=[("lf", 0), "c2_tri"], writes=["ps5"])
    P.mm(ps[5][:, 64:128], tri_f[:, 1, :], lf2[1], reads=[("lf", 1), "c2_tri"], writes=["ps5"])
    P.mm(ps[6][:, 0:128], ones_f, fl(sm["lf"]), reads=[("lf", 0), ("lf", 1), "ones_f"], writes=["ps6"])
    P.copy("dve", fl(sm["bcs"]), ps[5][:, 0:128], reads=["ps5"], writes=["bcs"])
    P.tt("dve", fl(sm["tg1"]), fl(sm["ii"]), fl(sm["bcs"]), ALU.subtract, reads=[("ii", 0), ("ii", 1), "bcs", ("tg1", 0), ("tg1", 1)],
         writes=[("tg1", 0), ("tg1", 1)])
    P.act(fl(sm["esc"]), fl(sm["tg1"]), AF.Exp, reads=[("tg1", 0), ("tg1", 1)], writes=["esc"])
    P.act(fl(sm["enb"]), fl(sm["bcs"]), AF.Exp, scale=-1.0, reads=["bcs"], writes=["enb"])
    P.act(fl(sm["eg"]), ps[6][:, 0:128], AF.Exp, reads=["ps6"], writes=["eg"])
    P.tt("dve", fl(sm["wk"]), fl(sm["esc"]), fl(sm["eg"]), ALU.mult, reads=["esc", "eg"], writes=["wk"])

    cnt = {"w": 0, "k": 0, "d": 0}
    for pair in range(2):
        hp = (2 * pair, 2 * pair + 1)
        streams = [(d, hl) for d in range(2) for hl in range(2)]
        for si in range(4):
            P.memset("pool", St32[si], 0.0, writes=[("St32", si)])
            P.memset("pool", Stb[si][1], 0.0, writes=[("Stb", si, 1)])
        for step in range(NT):
            inf = []
            for k, (d, hl) in enumerate(streams):
                T = step if d == 0 else NT - 1 - step
                w = cnt["w"] % 8; cnt["w"] += 1
                kk = cnt["k"] % 8; cnt["k"] += 1
                inf.append(dict(d=d, hl=hl, h=hp[hl], si=d * 2 + hl, T=T, Tsl=slice(T * 128, (T + 1) * 128), g4=T // 4, w=w, kk=kk, k=k))
            bS = step % 2
            for q in inf:
                d, h, T, Tsl, g4, w, k = q["d"], q["h"], q["T"], q["Tsl"], q["g4"], q["w"], q["k"]
                rS = "ps%d" % bS
                P.mm(ps[bS][:, k * 128:(k + 1) * 128], kT[:, h, Tsl], qT[:, h, Tsl], reads=[("kT", h, g4), ("qT", h, g4)], writes=[rS])
                P.act(tmpS[w], ps[bS][:, k * 128:(k + 1) * 128], AF.Copy, scale=sm["esc"][:, d, T, h:h + 1], reads=[rS, "esc"], writes=[("tmpS", w)])
                P.tt("pool", WT[w], tmpS[w], tri_b[:, d, :], ALU.mult, reads=[("tmpS", w), "tri_b"], writes=[("WT", w)])
                if step < NT - 1:
                    P.act(khat[q["kk"]], ktok[:, T, h, :], AF.Copy, scale=sm["wk"][:, d, T, h:h + 1], reads=[("ktok", h, g4), "wk"],
                          writes=[("khat", q["kk"])])
            if step < NT - 1:
                for q in inf:
                    d, h, si, T, g4, k, kk = q["d"], q["h"], q["si"], q["T"], q["g4"], q["k"], q["kk"]
                    bC = 6 + k // 2
                    co = (k % 2) * 256
                    rC = "ps%d" % bC
                    P.mm(ps[bC][:, co:co + 129], khat[kk], vtok[:, T, h, :], reads=[("khat", kk), ("vtok", h, g4), "vtok_ones"], writes=[rC])
                    P.stt(St32[si], St32[si], sm["eg"][:, d, T, h:h + 1], ps[bC][:, co:co + 129], ALU.mult, ALU.add,
                          reads=[("St32", si), "eg", rC], writes=[("St32", si)])
                    P.copy("dve", Stb[si][step % 2], St32[si], reads=[("St32", si)], writes=[("Stb", si, step % 2)])
            pendO = []
            for q in inf:
                d, hl, h, si, T, Tsl, g4, w, k = q["d"], q["hl"], q["h"], q["si"], q["T"], q["Tsl"], q["g4"], q["w"], q["k"]
                bO = 2 + (step % 2) * 2 + k // 2
                co = (k % 2) * 256
                rO = "ps%d" % bO
                P.mm(ps[bO][:, co:co + 129], WT[w], vtok[:, T, h, :], start=True, stop=False,
                     reads=[("WT", w), ("vtok", h, g4), "vtok_ones"], writes=[rO])
                P.mm(ps[bO][:, co:co + 129], qT[:, h, Tsl], Stb[si][(step + 1) % 2], start=False, stop=True,
                     reads=[("qT", h, g4), ("Stb", si, (step + 1) % 2)], writes=[rO])
                if k % 2 == 1:
                    dn = den2[cnt["d"] % 8]; dk = ("den2", cnt["d"] % 8); cnt["d"] += 1
                    h0 = hp[0]
                    P.stt(dn, ps[bO][:, 128::256], -1.0, sm["enb"][:, d, T, h0:h0 + 2], ALU.mult, ALU.max, reads=[rO, "enb"], writes=[dk])
                    P.tt("dve", dn, dn, ps[bO][:, 128::256], ALU.max, reads=[rO, dk], writes=[dk])
                    P.recip(dn, dn, reads=[dk], writes=[dk])
                    pendO.append((dn, dk))
                continue
            for q in inf:
                d, hl, h, si, T, Tsl, g4, w, k = q["d"], q["hl"], q["h"], q["si"], q["T"], q["Tsl"], q["g4"], q["w"], q["k"]
                bO = 2 + (step % 2) * 2 + k // 2
                co = (k % 2) * 256
                rO = "ps%d" % bO
                dnp, dk = pendO[k // 2]
                dn = dnp[:, k % 2:k % 2 + 1]
                is_first = (d == 0) == (T < 8)
                if is_first:
                    P.act(hsum[:, T, hl, :], ps[bO][:, co:co + 128], AF.Copy, scale=dn, reads=[rO, dk], writes=[("hsum", T, hl)])
                else:
                    P.stt(hsum[:, T, hl, :], ps[bO][:, co:co + 128], dn, hsum[:, T, hl, :], ALU.mult, ALU.add,
                          reads=[rO, dk, ("hsum", T, hl)], writes=[("hsum", T, hl)])
        for hl in range(2):
            h = hp[hl]
            P.act(acc3, hsum[:, :, hl, :], AF.Square, reads=[("hsum", T, hl) for T in range(NT)], writes=["acc"])
            P.op("dve", lambda e: e.reduce_sum(out=ssh, in_=acc3, axis=AX.X), reads=["acc"], writes=["ssh"])
            P.act(rstdh, ssh, AF.Sqrt, bias=EPS, scale=1.0 / 128, reads=["ssh"], writes=["rstdh"])
            P.recip(rstdh, rstdh, reads=["rstdh"], writes=["rstdh"])
            for Tg in range(4):
                tok = slice(Tg * 512, (Tg + 1) * 512)
                b = 6 + Tg % 2
                pT = ps[b][:, :].bitcast(BF16)
                for i in range(4):
                    T = Tg * 4 + i
                    P.act(hn[:, i, :], hsum[:, T, hl, :], AF.Copy, scale=rstdh[:, T:T + 1], reads=[("hsum", T, hl), "rstdh"], writes=[("hn", i)])
                    P.tr(pT[:, i * 128:(i + 1) * 128], hn[:, i, :], c["ident_b"], reads=[("hn", i), "ident_b"], writes=["ps%d" % b])
                tx = tmpx[Tg % 2]; txk = "tmpx%d" % (Tg % 2)
                ty = tmpy[Tg % 2]; tyk = "tmpy%d" % (Tg % 2)
                P.act(tx, xcT[:, h, tok], AF.Copy, scale=msk[:, h:h + 1], reads=[("xcT", h), "c2_msk"], writes=[txk])
                P.stt(ty, pT[:, 0:512], mng[:, h:h + 1], tx, ALU.mult, ALU.add, reads=["ps%d" % b, "c2_mng", txk], writes=[tyk])
                P.tt("pool", mixedT[:, h, tok], ty, szT[:, h, tok], ALU.mult, reads=[tyk, ("szT", h, Tg)], writes=[("mixedT", h, Tg)])


O_X1 = 140 * KB


def phase_A4(P, A, dr, c, ps, mixedT):
    x1 = A.at(O_X1, [128, NT, D], F32)
    Woutb = A.at(O_QA, [128, 8, D], BF16)
    for kc in range(8):
        P.dma("pool", Woutb[:, kc, :], dr["w_out"][kc * 128:(kc + 1) * 128, :], "wout", writes=[("Woutb", kc)])
    xv = dr["x"].rearrange("(t p) d -> p t d", p=128)
    for T in range(NT):
        P.dma("sp", x1[:, T, :], xv[:, T, :], "x1ld%d" % (T % 4), writes=[("x1", T)])
    nb = 0
    for T in range(NT):
        for nh in range(2):
            b = nb % 4; nb += 1
            for kc in range(8):
                P.mm(ps[b][:, :], mixedT[:, kc, T * 128:(T + 1) * 128], Woutb[:, kc, nh * 512:(nh + 1) * 512], start=(kc == 0), stop=(kc == 7),
                     reads=[("mixedT", kc, T // 4), ("Woutb", kc)], writes=["ps%d" % b])
            P.tt("dve", x1[:, T, nh * 512:(nh + 1) * 512], ps[b][:, :], x1[:, T, nh * 512:(nh + 1) * 512], ALU.add,
                 reads=["ps%d" % b, ("x1", T)], writes=[("x1", T)])
    return x1


def host_inputs(inp, b):
    import ml_dtypes
    bf = ml_dtypes.bfloat16
    f32 = np.float32
    cosT, sinT, PT = rope_tables()
    d = {}
    xb = np.asarray(inp["x"][b], f32)
    d["x"] = np.ascontiguousarray(xb)
    d["xT"] = np.ascontiguousarray(xb.T)
    d["w_in"] = np.ascontiguousarray(inp["w_in"][0], dtype=f32)
    col = lambda v, n: np.ascontiguousarray(np.asarray(v, f32).reshape(n, 128).T)
    d["g1"] = col(inp["norm1_g"][0], 8)
    d["ropeC"] = cosT.astype(bf)
    d["ropeS"] = sinT.astype(bf)
    d["ropePT"] = PT.astype(bf)
    d["ident_f"] = np.eye(128, dtype=f32)
    d["ident_b"] = np.eye(128).astype(bf)
    d["tmask"] = toeplitz_mask().astype(bf)
    ho = np.zeros((128, 2, 128), f32)
    ho[:64, 0, :] = 1.0
    ho[64:, 1, :] = 1.0
    d["halfones"] = ho.astype(bf)
    d["attn_g"] = col(inp["attn_norm_g"][0], 4)
    wbd = np.zeros((128, 12, 128), f32)
    for m, key in enumerate(("wq_m", "wk_m", "wv_m")):
        w = np.asarray(inp[key][0], f32)
        for hc in range(4):
            for gl in range(32):
                wbd[gl * 4:gl * 4 + 4, m * 4 + hc, gl * 4:gl * 4 + 4] = w[hc * 32 + gl]
    d["wbd"] = wbd
    wif = np.concatenate([np.asarray(inp["w_if_fwd"][0], f32), np.asarray(inp["w_if_bwd"][0], f32)], axis=1)
    d["wif"] = np.ascontiguousarray(wif.reshape(12, 128, 16).transpose(1, 0, 2))
    brow = np.concatenate([np.asarray(inp["b_if_fwd"][0], f32), np.asarray(inp["b_if_bwd"][0], f32)])
    d["bif"] = np.ascontiguousarray(np.broadcast_to(np.tile(brow, 16)[None, :], (128, 256)))
    tri = np.zeros((128, 2, 128), f32)
    ss, tt = np.meshgrid(np.arange(128), np.arange(128), indexing="ij")
    tri[:, 0, :] = (ss <= tt)
    tri[:, 1, :] = (ss >= tt)
    d["tri"] = tri
    cw = np.asarray(inp["conv_w"][0], f32)
    d["convw"] = np.ascontiguousarray(cw.T.reshape(4, 128, 5).transpose(1, 0, 2))
    d["convb"] = col(inp["conv_b"][0], 4)
    d["mng"] = col(inp["mlstm_norm_g"][0], 4)
    d["msk"] = col(inp["mlstm_skip"][0], 4)
    d["w_out"] = np.ascontiguousarray(inp["w_out"][0], dtype=f32)
    d["g2bc"] = np.ascontiguousarray(np.broadcast_to(np.asarray(inp["norm2_g"][0], f32)[None, :], (128, D)))
    d["gfbc"] = np.ascontiguousarray(np.broadcast_to(np.asarray(inp["norm_f_g"], f32)[None, :], (128, D)))
    d["wr"] = np.ascontiguousarray(np.asarray(inp["w_router"][0], f32).reshape(8, 128, NE).transpose(1, 0, 2))
    es = np.zeros((16, NE * 128), f32)
    for e in range(NE):
        es[e, e * 128:(e + 1) * 128] = 1.0
    d["esel"] = es.astype(bf)
    d["iota_row"] = np.ascontiguousarray(np.broadcast_to(np.arange(256, dtype=f32)[None, :], (128, 256)))
    d["iota_p"] = np.stack([np.arange(128, dtype=f32), np.arange(128, dtype=f32) + 128], axis=1)
    if "w1" in inp:
        d["w1"] = np.ascontiguousarray(inp["w1"][0], dtype=f32)
        d["w3"] = np.ascontiguousarray(inp["w3"][0], dtype=f32)
        d["w2"] = np.ascontiguousarray(inp["w2"][0], dtype=f32)
    return d


IN_SPECS = {
    "x": ([S, D], F32), "xT": ([D, S], F32), "w_in": ([D, DIN], F32), "g1": ([128, 8], F32),
    "ropeC": ([128, S], BF16), "ropeS": ([128, S], BF16), "ropePT": ([128, 128], BF16),
    "ident_f": ([128, 128], F32), "ident_b": ([128, 128], BF16),
    "tmask": ([128, 2944], BF16), "halfones": ([128, 2, 128], BF16), "attn_g": ([128, 4], F32),
    "wbd": ([128, 12, 128], F32), "wif": ([128, 12, 16], F32), "bif": ([128, 256], F32), "tri": ([128, 2, 128], F32),
    "convw": ([128, 4, 5], F32), "convb": ([128, 4], F32), "mng": ([128, 4], F32), "msk": ([128, 4], F32),
    "w_out": ([D, D], F32),
    "g2bc": ([128, D], F32), "gfbc": ([128, D], F32), "wr": ([128, 8, NE], F32), "esel": ([16, NE * 128], BF16),
    "iota_row": ([128, 256], F32), "iota_p": ([128, 2], F32),
    "w1": ([NE, D, DFF], F32), "w3": ([NE, D, DFF], F32), "w2": ([NE, DFF, D], F32),
}


def build(debug=None):
    nc = bass.Bass("TRN2", target_bir_lowering=False)
    early = debug is not None and debug[0] in "AB"
    dr = {k: nc.dram_tensor(k, sh, dt, kind="ExternalInput").ap() for k, (sh, dt) in IN_SPECS.items()
          if not (early and k in ("w1", "w2", "w3"))}
    out = nc.dram_tensor("out", [S, D], F32, kind="ExternalOutput").ap()
    A = Arena(nc, ARENA)
    ps = [nc.alloc_psum_tensor("ps%d" % i, [128, 512], F32) for i in range(8)]
    P = Prog(nc)
    finals = []

    def dump(name, ap, shape, dt):
        t = nc.dram_tensor("dbg_" + name, shape, dt, kind="ExternalOutput").ap()
        P.barrier()
        if len(shape) >= 3 and shape[1] > 4:
            for i in range(shape[1]):
                P.dma("sp", t[:, i], ap[:, i])
        else:
            P.dma("sp", t, ap)

    c = build_consts(P, A, dr)
    a1 = phase_A1(P, A, dr, c, ps)
    if debug == "A1":
        for k in ("xmT", "szT", "qaT", "kaT"):
            dump(k, a1[k], [128, 4, S], BF16)
        dump("va", a1["va"], [128, NT, 8, 65], BF16)
        P.emit(finals)
        return nc
    P.barrier()
    a3 = phase_A3(P, A, dr, c, ps, a1)
    if debug == "A3":
        dump("yA", a3["yA"], [128, NT, 512], F32)
        P.memset("dve", a3["mixedT"][:, 0:4, :], 0.0, writes=[("mixedT", i, j) for i in range(4) for j in range(4)])
        dump("mixedT", a3["mixedT"], [128, 8, S], BF16)
        P.emit(finals)
        return nc
    P.barrier()
    phase_A2(P, A, dr, c, ps, a1, a3["mixedT"])
    if debug == "A2":
        dump("mixedT", a3["mixedT"], [128, 8, S], BF16)
        P.emit(finals)
        return nc
    P.barrier()
    x1 = phase_A4(P, A, dr, c, ps, a3["mixedT"])
    if debug == "A4":
        dump("mixedT", a3["mixedT"], [128, 8, S], BF16)
        dump("x1", x1, [128, NT, D], F32)
        P.emit(finals)
        return nc
    P.barrier()
    rb = phase_B(P, A, dr, c, ps, x1)
    if debug == "B":
        dump("aff", rb["aff"], [128, NT, NE], F32)
        dump("posm_tok", rb["posm_tok"], [128, NT, NE], F32)
        dump("h2b", rb["h2b"], [128, NT, D], BF16)
        P.emit(finals)
        return nc
    P.barrier()
    nexp = NE if debug is None else int(debug[1:]) if debug.startswith("C") else NE
    if debug is not None and debug.startswith("C"):
        dump("x1pre", x1, [128, NT, D], F32)
        P.barrier()
    phase_C(P, A, dr, c, ps, x1, rb, experts=range(nexp))
    if debug is not None and debug.startswith("C"):
        dump("x2", x1, [128, NT, D], F32)
        dump("aff", rb["aff"], [128, NT, NE], F32)
        dump("posm_tok", rb["posm_tok"], [128, NT, NE], F32)
        P.emit(finals)
        return nc
    P.barrier()
    phase_D(P, A, dr, c, ps, x1, out)
    P.emit(finals)
    return nc


def run(inp, cores=(0,), debug=None):
    nc = build(debug)
    in_maps = [host_inputs(inp, b) for b in cores]
    names = [a.memorylocations[0].name for a in nc.m.functions[0].allocations
             if isinstance(a, mybir.MemoryLocationSet) and a.kind == "ExternalInput"]
    in_maps = [{k: v for k, v in m.items() if k in names} for m in in_maps]
    res = run_bass_kernel_spmd(nc, in_maps, core_ids=list(range(len(cores))))
    return res.results


O_H2 = 12 * KB
O_BC = 44 * KB
O_PER = 132 * KB


def phase_B(P, A, dr, c, ps, x1):
    h2b = A.at(O_H2, [128, NT, D], BF16)
    o = O_PER
    aff = A.at(o, [128, NT, NE], F32); o += KB
    posm_tok = A.at(o, [128, NT, NE], F32); o += KB
    posmT_b = A.at(o, [16, S], BF16); o += 4 * KB
    assert o <= O_X1
    esel = A.at(3 * KB, [16, NE * 128], BF16)
    iota_row = A.at(7 * KB, [128, 256], F32)
    iota_p = A.at(8 * KB, [128, 2], F32)
    o = O_BC
    g2bc = A.at(o, [128, D], F32); o += 4 * KB
    h2f = [A.at(o + i * 4 * KB, [128, D], F32) for i in range(2)]; o += 8 * KB
    h2fT = A.at(o, [128, 8, 128], F32); o += 4 * KB
    junk = A.at(o, [128, D], BF16); o += 2 * KB
    wr = A.at(o, [128, 8, NE], F32); o += 512
    ss2 = A.at(o, [128, NT], F32); o += 64
    rstd2 = A.at(o, [128, NT], F32); o += 64
    mxs = A.at(o, [128, 4], F32); o += 16
    sms = A.at(o, [128, 4], F32); o += 16
    ex = [A.at(o + i * 64, [128, NE], F32) for i in range(2)]; o += 128
    affT = A.at(o, [16, S], F32); o += 8 * KB
    work = A.at(o, [16, S], F32); o += 8 * KB
    ones16 = A.at(o, [16, S], F32); o += 8 * KB
    cs = A.at(o, [16, S], F32); o += 8 * KB
    m8 = A.at(o, [16, 8], F32); o += 32
    assert o <= O_PER

    P.dma("sp", g2bc, dr["g2bc"], writes=["g2bc"])
    P.dma("sp", wr, dr["wr"], writes=["wr"])
    P.dma("sp", esel, dr["esel"], writes=["esel"])
    P.dma("sp", iota_row, dr["iota_row"], writes=["iota_row"])
    P.dma("sp", iota_p, dr["iota_p"], writes=["iota_p"])
    P.memset("pool", ones16, 1.0, writes=["ones16"])

    for T in range(NT):
        P.act(junk, x1[:, T, :], AF.Square, accum_out=ss2[:, T:T + 1], reads=[("x1", T)], writes=["junkB", "ss2"])
    P.act(rstd2, ss2, AF.Sqrt, bias=EPS, scale=1.0 / D, reads=["ss2"], writes=["rstd2"])
    P.recip(rstd2, rstd2, reads=["rstd2"], writes=["rstd2"])
    for T in range(NT):
        i2 = T % 2
        hf = h2f[i2]; hk = "h2f%d" % i2
        P.stt(hf, x1[:, T, :], rstd2[:, T:T + 1], g2bc, ALU.mult, ALU.mult, reads=[("x1", T), "rstd2", "g2bc"], writes=[hk])
        P.copy("act", h2b[:, T, :], hf, reads=[hk], writes=[("h2b", T)])
        for half in range(2):
            b = half
            for j in range(4):
                kc = half * 4 + j
                P.tr(ps[b][:, j * 128:(j + 1) * 128], hf[:, kc * 128:(kc + 1) * 128], c["ident_f"], reads=[hk, "ident_f"], writes=["ps%d" % b])
            P.copy("dve" if half == 0 else "act", h2fT[:, half * 4:half * 4 + 4, :], ps[b][:, :].rearrange("p (a b) -> p a b", a=4),
                   reads=["ps%d" % b], writes=[("h2fT", half)])
        for kc in range(8):
            P.mm(ps[2][:, 0:NE], h2fT[:, kc, :], wr[:, kc, :], start=(kc == 0), stop=(kc == 7),
                 reads=[("h2fT", kc // 4), "wr"], writes=["ps2"])
        m = T % 4
        P.op("dve", lambda e, m=m: e.reduce_max(out=mxs[:, m:m + 1], in_=ps[2][:, 0:NE], axis=AX.X), reads=["ps2"], writes=[("mxs", m)])
        P.ts("dve", mxs[:, m:m + 1], mxs[:, m:m + 1], -1.0, ALU.mult, reads=[("mxs", m)], writes=[("mxs", m)])
        P.act(ex[i2], ps[2][:, 0:NE], AF.Exp, bias=mxs[:, m:m + 1], accum_out=sms[:, m:m + 1], reads=["ps2", ("mxs", m)], writes=[("ex", i2), ("sms", m)])
        P.recip(sms[:, m:m + 1], sms[:, m:m + 1], reads=[("sms", m)], writes=[("sms", m)])
        P.ts("dve", aff[:, T, :], ex[i2], sms[:, m:m + 1], ALU.mult, reads=[("ex", i2), ("sms", m)], writes=[("aff", T)])
    for Tg in range(4):
        b = 4 + Tg % 2
        for i in range(4):
            T = Tg * 4 + i
            P.tr(ps[b][0:NE, i * 128:(i + 1) * 128], aff[:, T, :], c["ident_f"], reads=[("aff", T), "ident_f"], writes=["ps%d" % b])
        P.copy("dve", affT[:, Tg * 512:(Tg + 1) * 512], ps[b][0:NE, :], reads=["ps%d" % b], writes=["affT"])
    P.copy("dve", work, affT, reads=["affT"], writes=["work"])
    for r in range(CAP // 8):
        P.op("dve", lambda e: e.max(out=m8, in_=work), reads=["work"], writes=["m8"])
        if r < CAP // 8 - 1:
            P.op("dve", lambda e: e.match_replace(out=work, in_to_replace=m8, in_values=work, imm_value=-1.0), reads=["m8", "work"], writes=["work"])
    P.ts("dve", work, affT, m8[:, 7:8], ALU.is_ge, reads=["affT", "m8"], writes=["work"])
    P.op("dve", lambda e: e.tensor_tensor_scan(out=cs, data0=ones16, data1=work, initial=0.0, op0=ALU.mult, op1=ALU.add),
         reads=["ones16", "work"], writes=["cs"])
    P.tt("dve", cs, cs, work, ALU.mult, reads=["cs", "work"], writes=["cs"])
    P.ts("dve", posmT_b, cs, -1.0, ALU.add, reads=["cs"], writes=["posmT_b"])
    for Tg in range(4):
        b = 6 + Tg % 2
        pT = ps[b][:, :].bitcast(BF16)
        for i in range(4):
            T = Tg * 4 + i
            P.tr(pT[:, i * NE:(i + 1) * NE], posmT_b[:, T * 128:(T + 1) * 128], c["ident_b"][0:NE, 0:NE], reads=["posmT_b", "ident_b"], writes=["ps%d" % b])
        P.copy("dve", posm_tok[:, Tg * 4:Tg * 4 + 4, :], pT[:, 0:4 * NE].rearrange("p (a b) -> p a b", a=4), reads=["ps%d" % b], writes=["posm_tok"])
    return dict(h2b=h2b, aff=aff, posm_tok=posm_tok, posmT_b=posmT_b, esel=esel, iota_row=iota_row, iota_p=iota_p)


NFG = 11
NSLOT = 4


def phase_C(P, A, dr, c, ps, x1, rb, experts=range(NE)):
    h2b, aff, posm_tok, posmT_b = rb["h2b"], rb["aff"], rb["posm_tok"], rb["posmT_b"]
    esel, iota_row, iota_p = rb["esel"], rb["iota_row"], rb["iota_p"]
    o = O_BC
    Wslot = []
    for s in range(NSLOT):
        Wslot.append((A.at(o, [128, 8, 256], BF16), A.at(o + 4 * KB, [128, 8, 256], BF16), A.at(o + 8 * KB, [128, 2, D], BF16)))
        o += 12 * KB
    XeT = A.at(o, [128, 8, CAP], BF16); o += 4 * KB
    Sel = A.at(o, [128, NT, CAP], BF16); o += 8 * KB
    SelT = A.at(o, [128, 2, S], BF16); o += 8 * KB
    H = [A.at(o + i * 512, [128, CAP], BF16) for i in range(4)]; o += 2 * KB
    su = [A.at(o + i * 512, [128, CAP], BF16) for i in range(2)]; o += KB
    Yb = A.at(o, [128, 2, D], BF16); o += 4 * KB
    assert o <= O_PER, o

    w1v = dr["w1"].rearrange("e (kc p) f -> e p kc f", p=128)
    w3v = dr["w3"].rearrange("e (kc p) f -> e p kc f", p=128)
    w2v = dr["w2"].rearrange("e (fc p) d -> e p fc d", p=128)

    groups = [(e, fg) for e in experts for fg in range(NFG)]
    PREF = NSLOT - 1

    def load(gi):
        e, fg = groups[gi]
        s = gi % NSLOT
        W1g, W3g, W2g = Wslot[s]
        P.dma("pool", W1g, w1v[e, :, :, fg * 256:(fg + 1) * 256], writes=[("W1", s)])
        P.dma("pool", W3g, w3v[e, :, :, fg * 256:(fg + 1) * 256], writes=[("W3", s)])
        P.dma("pool", W2g, w2v[e, :, fg * 2:fg * 2 + 2, :], writes=[("W2", s)])

    for gi in range(min(PREF, len(groups))):
        load(gi)

    gi = 0
    nsc = 0
    for e in experts:
        for T in range(NT):
            P.ts("dve", Sel[:, T, :], iota_row, posm_tok[:, T, e:e + 1], ALU.is_equal, reads=["iota_row", "posm_tok"], writes=[("Sel", T)])
        for kp in range(4):
            b = 2 + kp % 2
            for j in range(2):
                kc = kp * 2 + j
                for T in range(NT):
                    P.mm(ps[b][:, j * 256:(j + 1) * 256], h2b[:, T, kc * 128:(kc + 1) * 128], Sel[:, T, :], start=(T == 0), stop=(T == NT - 1),
                         reads=[("h2b", T), ("Sel", T)], writes=["ps%d" % b])
            P.copy("act" if kp % 2 else "dve", XeT[:, kp * 2:kp * 2 + 2, :], ps[b][:, :].rearrange("p (a b) -> p a b", a=2),
                   reads=["ps%d" % b], writes=[("XeT", kp)])
        for tg in range(4):
            b = 2 + tg % 2
            P.mm(ps[b][:, :], esel[:, e * 128:(e + 1) * 128], posmT_b[:, tg * 512:(tg + 1) * 512], reads=["esel", "posmT_b"], writes=["ps%d" % b])
            for sc in range(2):
                P.ts("dve", SelT[:, sc, tg * 512:(tg + 1) * 512], ps[b][:, :], iota_p[:, sc:sc + 1], ALU.is_equal,
                     reads=["ps%d" % b, "iota_p"], writes=[("SelT", tg)])
        pend = None
        for fg in range(NFG):
            s = gi % NSLOT
            W1g, W3g, W2g = Wslot[s]
            for fc in range(2):
                fci = fg * 2 + fc
                b = fci % 2
                for kc in range(8):
                    P.mm(ps[b][:, 0:256], W1g[:, kc, fc * 128:(fc + 1) * 128], XeT[:, kc, :], start=(kc == 0), stop=(kc == 7),
                         reads=[("W1", s), ("XeT", kc // 2)], writes=["ps%d" % b])
                for kc in range(8):
                    P.mm(ps[b][:, 256:512], W3g[:, kc, fc * 128:(fc + 1) * 128], XeT[:, kc, :], start=(kc == 0), stop=(kc == 7),
                         reads=[("W3", s), ("XeT", kc // 2)], writes=["ps%d" % b])
                hi = fci % 4
                P.act(su[fci % 2], ps[b][:, 0:256], AF.Silu, reads=["ps%d" % b], writes=[("su", fci % 2)])
                P.tt("dve", H[hi], su[fci % 2], ps[b][:, 256:512], ALU.mult, reads=[("su", fci % 2), "ps%d" % b], writes=[("H", hi)])
                if pend is not None:
                    pend()

                def down(fci=fci, hi=hi, W2g=W2g, fc=fc, s=s):
                    for sc in range(2):
                        for dh in range(2):
                            P.mm(ps[4 + sc * 2 + dh][:, :], H[hi][:, sc * 128:(sc + 1) * 128], W2g[:, fc, dh * 512:(dh + 1) * 512],
                                 start=(fci == 0), stop=(fci == 2 * NFG - 1), reads=[("H", hi), ("W2", s)], writes=["ps%d" % (4 + sc * 2 + dh)])
                pend = down
            gi += 1
            if pend is not None and fg == NFG - 1:
                pend()
                pend = None
            if gi - 1 + PREF < len(groups):
                load(gi - 1 + PREF)
        for sc in range(2):
            for dh in range(2):
                k = sc * 2 + dh
                P.copy("act" if k % 2 else "dve", Yb[:, sc, dh * 512:(dh + 1) * 512], ps[4 + k][:, :], reads=["ps%d" % (4 + k)], writes=[("Yb", k)])
        for T in range(NT):
            for dh in range(2):
                b = nsc % 4; nsc += 1
                for sc in range(2):
                    P.mm(ps[b][:, :], SelT[:, sc, T * 128:(T + 1) * 128], Yb[:, sc, dh * 512:(dh + 1) * 512], start=(sc == 0), stop=(sc == 1),
                         reads=[("SelT", T // 4), ("Yb", sc * 2 + dh)], writes=["ps%d" % b])
                P.stt(x1[:, T, dh * 512:(dh + 1) * 512], ps[b][:, :], aff[:, T, e:e + 1], x1[:, T, dh * 512:(dh + 1) * 512], ALU.mult, ALU.add,
                      reads=["ps%d" % b, ("aff", T), ("x1", T)], writes=[("x1", T)])


def phase_D(P, A, dr, c, ps, x1, out):
    o = O_BC
    gfbc = A.at(o, [128, D], F32); o += 4 * KB
    junk = A.at(o, [128, D], BF16); o += 2 * KB
    ssf = A.at(o, [128, NT], F32); o += 64
    rstdf = A.at(o, [128, NT], F32); o += 64
    ob = [A.at(o + i * 4 * KB, [128, D], F32) for i in range(4)]; o += 16 * KB
    P.dma("sp", gfbc, dr["gfbc"], writes=["gfbc"])
    for T in range(NT):
        P.act(junk, x1[:, T, :], AF.Square, accum_out=ssf[:, T:T + 1], reads=[("x1", T)], writes=["junkD", "ssf"])
    P.act(rstdf, ssf, AF.Sqrt, bias=EPS, scale=1.0 / D, reads=["ssf"], writes=["rstdf"])
    P.recip(rstdf, rstdf, reads=["rstdf"], writes=["rstdf"])
    ov = out.rearrange("(t p) d -> p t d", p=128)
    for T in range(NT):
        i = T % 4
        P.stt(ob[i], x1[:, T, :], rstdf[:, T:T + 1], gfbc, ALU.mult, ALU.mult, reads=[("x1", T), "rstdf", "gfbc"], writes=[("ob", i)])
        P.dma("sp", ov[:, T, :], ob[i], reads=[("ob", i)])


def kernel(**inputs):
    nc = build(None)
    names = [a.memorylocations[0].name for a in nc.m.functions[0].allocations
             if isinstance(a, mybir.MemoryLocationSet) and a.kind == "ExternalInput"]
    in_maps = []
    for b in range(8):
        m = host_inputs(inputs, b)
        in_maps.append({k: v for k, v in m.items() if k in names})
    res = run_bass_kernel_spmd(nc, in_maps, core_ids=list(range(8)))
    return np.stack([np.asarray(r["out"], dtype=np.float32) for r in res.results], axis=0)
```

```python
import numpy as np
import concourse.bass as bass
import concourse.mybir as mybir

F32 = mybir.dt.float32
BF16 = mybir.dt.bfloat16
I32 = mybir.dt.int32
U8 = mybir.dt.uint8
AF = mybir.ActivationFunctionType
ALU = mybir.AluOpType
AX = mybir.AxisListType
DTSIZE = {F32: 4, BF16: 2, I32: 4, U8: 1}

ENGS = ["pe", "act", "dve", "pool", "sp"]
SAME_ENG_SYNC = {"pe": False, "act": True, "dve": True, "pool": True, "sp": False}


class Op:
    __slots__ = ("eng", "fn", "deps", "is_dma", "semkey", "semval", "signal", "sigcount", "gid")

    def __init__(self, eng, fn):
        self.eng = eng
        self.fn = fn
        self.deps = set()
        self.is_dma = False
        self.semkey = None
        self.semval = 0
        self.signal = False
        self.sigcount = 0
        self.gid = 0


class Prog:
    def __init__(self, nc):
        self.nc = nc
        self.ops = {e: [] for e in ENGS}
        self.lastw = {}
        self.readers = {}
        self.dmacount = {}
        self.nops = 0
        self.barrier_set = None
        self.barrier_applied = {e: True for e in ENGS}
        self.last_dma = {}

    def _add(self, op, reads, writes):
        op.gid = self.nops
        self.nops += 1
        deps = op.deps
        for r in reads:
            w = self.lastw.get(r)
            if w is not None:
                deps.add(w)
        for r in writes:
            w = self.lastw.get(r)
            if w is not None:
                deps.add(w)
            for o in self.readers.get(r, {}).values():
                deps.add(o)
        rk = (op.eng, op.semkey) if op.is_dma else op.eng
        for r in reads:
            self.readers.setdefault(r, {})[rk] = op
        for r in writes:
            self.lastw[r] = op
            self.readers[r] = {}
        if not self.barrier_applied[op.eng]:
            deps |= self.barrier_set
            self.barrier_applied[op.eng] = True
        deps.discard(op)
        self.ops[op.eng].append(op)
        return op

    def op(self, eng, fn, reads=(), writes=()):
        return self._add(Op(eng, fn), reads, writes)

    RING = 16

    def dma(self, eng, out, in_, semkey=None, reads=(), writes=(), **kw):
        def fn(e, out=out, in_=in_, kw=kw):
            return e.dma_start(out=out, in_=in_, **kw)
        op = Op(eng, fn)
        op.is_dma = True
        rc = self.__dict__.setdefault("_ringcnt", {})
        i = rc.get(eng, 0)
        rc[eng] = i + 1
        semkey = "%s_r%d" % (eng, i % self.RING)
        op.semkey = semkey
        prev = self.last_dma.get(semkey)
        if prev is not None:
            op.deps.add(prev)
        self.dmacount[semkey] = self.dmacount.get(semkey, 0) + 16
        op.semval = self.dmacount[semkey]
        self.last_dma[semkey] = op
        return self._add(op, reads, writes)


    def mm(self, out, lhsT, rhs, start=True, stop=True, reads=(), writes=(), **kw):
        return self.op("pe", lambda e: e.matmul(out, lhsT=lhsT, rhs=rhs, start=start, stop=stop, **kw), reads, writes)

    def tr(self, out, in_, ident, reads=(), writes=()):
        return self.op("pe", lambda e: e.transpose(out, in_, ident), reads, writes)

    def act(self, out, in_, func, reads=(), writes=(), eng="act", **kw):
        return self.op(eng, lambda e: e.activation(out=out, in_=in_, func=func, **kw), reads, writes)

    def tt(self, eng, out, in0, in1, op, reads=(), writes=()):
        return self.op(eng, lambda e: e.tensor_tensor(out=out, in0=in0, in1=in1, op=op), reads, writes)

    def ts(self, eng, out, in0, s1, op0, s2=None, op1=None, reads=(), writes=(), **kw):
        if op1 is None:
            return self.op(eng, lambda e: e.tensor_scalar(out=out, in0=in0, scalar1=s1, scalar2=None, op0=op0, **kw), reads, writes)
        return self.op(eng, lambda e: e.tensor_scalar(out=out, in0=in0, scalar1=s1, scalar2=s2, op0=op0, op1=op1, **kw), reads, writes)

    def stt(self, out, in0, scalar, in1, op0, op1, reads=(), writes=(), eng="dve"):
        return self.op(eng, lambda e: e.scalar_tensor_tensor(out=out, in0=in0, scalar=scalar, in1=in1, op0=op0, op1=op1), reads, writes)

    def copy(self, eng, out, in_, reads=(), writes=()):
        if eng == "act":
            return self.op(eng, lambda e: e.copy(out=out, in_=in_), reads, writes)
        return self.op(eng, lambda e: e.tensor_copy(out=out, in_=in_), reads, writes)

    def recip(self, out, in_, reads=(), writes=()):
        return self.op("dve", lambda e: e.reciprocal(out=out, in_=in_), reads, writes)

    def memset(self, eng, ap, val, reads=(), writes=()):
        return self.op(eng, lambda e: e.memset(ap, val), reads, writes)

    def barrier(self):
        s = set()
        for e in ENGS:
            if self.ops[e]:
                s.add(self.ops[e][-1])
        for op in self.last_dma.values():
            s.add(op)
        self.barrier_set = s
        self.barrier_applied = {e: False for e in ENGS}

    def emit(self, final_dma_keys):
        nc = self.nc
        for e in ENGS:
            for op in self.ops[e]:
                for d in op.deps:
                    if not d.is_dma:
                        if d.eng == op.eng and not SAME_ENG_SYNC[op.eng]:
                            continue
                        d.signal = True
        for e in ENGS:
            c = 0
            for op in self.ops[e]:
                if (not op.is_dma) and op.signal:
                    c += 1
                op.sigcount = c
        from contextlib import ExitStack
        with ExitStack() as es:
            engsem = {e: es.enter_context(nc.semaphore("S_" + e)) for e in ENGS if e != "sp"}
            dmasem = {k: es.enter_context(nc.semaphore("D_" + str(k))) for k in self.dmacount}
            print("semaphores used:", len(engsem) + len(dmasem), "ops:", {e: len(self.ops[e]) for e in ENGS})
            block = es.enter_context(nc.Block())

            def run(ename, eng):
                waited = {}
                for op in self.ops[ename]:
                    for d in sorted(op.deps, key=lambda o: o.gid):
                        if d.is_dma:
                            key = ("d", d.semkey)
                            sem, val = dmasem[d.semkey], d.semval
                        else:
                            if d.eng == op.eng and not SAME_ENG_SYNC[op.eng]:
                                continue
                            key = ("e", d.eng)
                            sem, val = engsem[d.eng], d.sigcount
                        if waited.get(key, 0) >= val:
                            continue
                        eng.wait_ge(sem, val)
                        waited[key] = val
                    inst = op.fn(eng)
                    if op.is_dma:
                        inst.then_inc(dmasem[op.semkey], 16)
                    elif op.signal:
                        inst.then_inc(engsem[op.eng], 1)
                if ename == "sp":
                    for k in sorted(self.dmacount):
                        if waited.get(("d", k), 0) < self.dmacount[k]:
                            eng.wait_ge(dmasem[k], self.dmacount[k])

            block.tensor(lambda e: run("pe", e))
            block.scalar(lambda e: run("act", e))
            block.vector(lambda e: run("dve", e))
            block.gpsimd(lambda e: run("pool", e))
            block.sync(lambda e: run("sp", e))


class Arena:
    def __init__(self, nc, nbytes):
        self.nc = nc
        self.t = nc.alloc_sbuf_tensor("arena", [128, nbytes], U8)
        self.nbytes = nbytes

    def at(self, off, shape, dt):
        n = int(np.prod(shape[1:])) * DTSIZE[dt]
        assert off + n <= self.nbytes, (off, n, self.nbytes)
        assert off % 4 == 0
        ap = self.t[0:shape[0], off:off + n].bitcast(dt)
        if len(shape) == 3:
            ap = ap.rearrange("p (a b) -> p a b", a=shape[1])
        elif len(shape) == 4:
            ap = ap.rearrange("p (a b c) -> p a b c", a=shape[1], b=shape[2])
        return ap


from concourse.bass_utils import run_bass_kernel_spmd

S = 2048
D = 1024
NT = 16
DIN = 2560
EPS = 1.0000001e-6
KB = 1024
NE = 16
CAP = 256
DFF = 2816
NFC = 22

O_CONST = 0
O_XM = 12 * KB
O_SZ = 28 * KB
O_MIX = 44 * KB
O_QA = 76 * KB
O_KA = 92 * KB
O_VA = 108 * KB
O_T = 125 * KB
ARENA = 206 * KB


def build_consts(P, A, dr):
    c = {}
    off = [O_CONST]

    def alloc(shape, dt):
        n = int(np.prod(shape[1:])) * DTSIZE[dt]
        n = (n + 31) // 32 * 32
        ap = A.at(off[0], shape, dt)
        off[0] += n
        assert off[0] <= 3 * KB
        return ap

    c["ident_b"] = alloc([128, 128], BF16)
    c["ident_f"] = alloc([128, 128], F32)
    c["ones_b"] = alloc([128, 128], BF16)
    c["ropePT"] = alloc([128, 128], BF16)
    c["g1"] = alloc([128, 8], F32)
    for k in ("ident_b", "ident_f", "ropePT", "g1"):
        P.dma("sp", c[k], dr[k], "c_" + k, writes=[k])
    P.memset("dve", c["ones_b"], 1.0, writes=["ones_b"])
    return c


def phase_A1(P, A, dr, c, ps):
    nc = P.nc
    xmT = A.at(O_XM, [128, 4, S], BF16)
    szT = A.at(O_SZ, [128, 4, S], BF16)
    qaT = A.at(O_QA, [128, 4, S], BF16)
    kaT = A.at(O_KA, [128, 4, S], BF16)
    va = A.at(O_VA, [128, NT, 8, 65], BF16)
    xTs = A.at(O_MIX, [128, 8, 512], F32)
    xTb = A.at(O_MIX + 16 * KB, [128, 8, 512], BF16)
    sq = A.at(O_MIX + 24 * KB, [128, 8, 512], BF16)
    Wb = A.at(O_T, [128, 8, DIN], BF16)
    Wst = [A.at(O_T + 40 * KB + i * 10 * KB, [128, DIN], F32) for i in range(2)]
    o = O_T + 60 * KB
    cosT = A.at(o, [128, S], BF16); o += 4 * KB
    sinT = A.at(o, [128, S], BF16); o += 4 * KB
    rstd_bc = A.at(o, [128, 512], F32); o += 2 * KB
    tmpA = [A.at(o + i * 2 * KB, [128, 512], F32) for i in range(2)]; o += 4 * KB
    tmpAb = [tmpA[i].bitcast(BF16)[:, 0:512] for i in range(2)]
    tmp1 = [A.at(o + i * 2 * KB, [128, 512], F32) for i in range(2)]; o += 4 * KB
    tmp2 = [A.at(o, [128, 512], F32) for i in range(2)]; o += 2 * KB
    rstd_t = A.at(o, [128, 4], F32); o += 32
    assert o <= ARENA

    P.dma("sp", cosT, dr["ropeC"], "c_cos", writes=["cosT"])
    P.dma("sp", sinT, dr["ropeS"], "c_sin", writes=["sinT"])
    P.memset("pool", va[:, :, :, 64:65], 1.0, writes=["va_ones"])

    for kc in range(8):
        s = kc % 2
        P.dma("sp", Wst[s], dr["w_in"][kc * 128:(kc + 1) * 128, :], "wst%d" % s, writes=["Wst%d" % s])
        if kc % 2 == 0:
            P.ts("dve", Wb[:, kc, :], Wst[s], c["g1"][:, kc:kc + 1], ALU.mult, reads=["Wst%d" % s, "g1"], writes=[("Wb", kc)])
        else:
            P.act(Wb[:, kc, :], Wst[s], AF.Copy, scale=c["g1"][:, kc:kc + 1], reads=["Wst%d" % s, "g1"], writes=[("Wb", kc)])

    xT_v = dr["xT"].rearrange("(kc p) t -> p kc t", p=128)
    ev = 0
    for n in range(4):
        tok = slice(n * 512, (n + 1) * 512)
        P.dma("sp", xTs, xT_v[:, :, tok], "xTs", writes=["xTs"])
        P.act(sq, xTs, AF.Square, reads=["xTs"], writes=["sq"])
        P.copy("dve", xTb, xTs, reads=["xTs"], writes=["xTb"])
        for kc in range(8):
            P.mm(ps[4][:, :], c["ones_b"], sq[:, kc, :], start=(kc == 0), stop=(kc == 7), reads=["sq", "ones_b"], writes=["ps4"])
        for tt in range(4):
            for kc in range(8):
                P.mm(ps[7][:, tt:tt + 1], sq[:, kc, tt * 128:(tt + 1) * 128], c["ones_b"][:, 0:1], start=(kc == 0), stop=(kc == 7),
                     reads=["sq", "ones_b"], writes=["ps7"])
        P.act(rstd_bc, ps[4][:, :], AF.Sqrt, bias=EPS, scale=1.0 / D, reads=["ps4"], writes=["rstd_bc"])
        P.recip(rstd_bc, rstd_bc, reads=["rstd_bc"], writes=["rstd_bc"])
        P.act(rstd_t, ps[7][:, 0:4], AF.Sqrt, bias=EPS, scale=1.0 / D, reads=["ps7"], writes=["rstd_t"])
        P.recip(rstd_t, rstd_t, reads=["rstd_t"], writes=["rstd_t"])
        for oc in range(16):
            b = oc % 4
            pb = ps[b]
            pk = "ps%d" % b
            for kc in range(8):
                P.mm(pb[:, :], Wb[:, kc, oc * 128:(oc + 1) * 128], xTb[:, kc, :], start=(kc == 0), stop=(kc == 7),
                     reads=["xTb", ("Wb", kc)], writes=[pk])
            if oc < 4:
                P.tt("dve", xmT[:, oc, tok], pb[:, :], rstd_bc, ALU.mult, reads=[pk, "rstd_bc"], writes=[("xmT", oc, n)])
            elif oc < 8:
                t = tmpA[ev % 2]; tk = "tmpA%d" % (ev % 2); ev += 1
                P.tt("dve", t, pb[:, :], rstd_bc, ALU.mult, reads=[pk, "rstd_bc"], writes=[tk])
                P.act(szT[:, oc - 4, tok], t, AF.Silu, reads=[tk], writes=[("szT", oc - 4, n)])
            else:
                isq = oc < 12
                dst = qaT if isq else kaT
                cc = (oc - 8) % 4
                i2 = ev % 2; ev += 1
                t = tmpAb[i2]; tk = "tmpA%d" % i2
                P.stt(t, pb[:, :], (0.125 if isq else 1.0), rstd_bc, ALU.mult, ALU.mult, reads=[pk, "rstd_bc"], writes=[tk])
                P.mm(ps[5][:, :], c["ropePT"], t, reads=[tk, "ropePT"], writes=["ps5"])
                t1 = tmp1[i2]; t1k = "tmp1%d" % i2
                t2 = tmp2[i2]; t2k = "tmp2"
                P.tt("pool", t1, t, cosT[:, tok], ALU.mult, reads=[tk, "cosT"], writes=[t1k])
                P.tt("dve", t2, ps[5][:, :], sinT[:, tok], ALU.mult, reads=["ps5", "sinT"], writes=[t2k])
                P.tt("pool", dst[:, cc, tok], t1, t2, ALU.add, reads=[t1k, t2k], writes=[("qaT" if isq else "kaT", cc, n)])
        for tt in range(4):
            T = n * 4 + tt
            for kc in range(8):
                P.mm(ps[6][:, :], xTb[:, kc, tt * 128:(tt + 1) * 128], Wb[:, kc, 2048:2560], start=(kc == 0), stop=(kc == 7),
                     reads=["xTb", ("Wb", kc)], writes=["ps6"])
            P.act(va[:, T, :, 0:64], ps[6][:, :].rearrange("p (h d) -> p h d", h=8), AF.Copy, scale=rstd_t[:, tt:tt + 1],
                  reads=["ps6", "rstd_t"], writes=[("va", T)])
    return dict(xmT=xmT, szT=szT, qaT=qaT, kaT=kaT, va=va)


def rope_tables():
    half = 8
    inv_freq = 500000.0 ** (-2.0 * np.arange(half, dtype=np.float64) / 16.0)
    pos = np.arange(S, dtype=np.float64)
    ang = (pos[:, None].astype(np.float32) * inv_freq[None, :].astype(np.float32)).astype(np.float32).astype(np.float64)
    cosT = np.ones((128, S), np.float32)
    sinT = np.zeros((128, S), np.float32)
    for p in range(128):
        f = p % 64
        if f < 16:
            cosT[p] = np.cos(ang[:, f % 8])
            sinT[p] = np.sin(ang[:, f % 8])
    PT = np.zeros((128, 128), np.float32)
    for m in range(128):
        f = m % 64
        if f < 8:
            PT[m + 8, m] = -1.0
        elif f < 16:
            PT[m - 8, m] = 1.0
    return cosT, sinT, PT


def toeplitz_mask():
    U0 = 1408
    W = 2944
    p = np.arange(128)[:, None]
    u = np.arange(W)[None, :]
    d = p - u + U0
    ad = np.abs(d)
    cnt = (ad <= 64).astype(np.float32) + ((d % 4 == 0) & (ad <= 256)) + ((d % 16 == 0) & (ad <= 1024))
    return cnt.astype(np.float32)


def phase_A3(P, A, dr, c, ps, a1):
    qaT, kaT, va = a1["qaT"], a1["kaT"], a1["va"]
    mixedT = A.at(O_MIX, [128, 8, S], BF16)
    o = O_T
    yA = A.at(o, [128, NT, 512], F32); o += 32 * KB
    tmask = A.at(o, [128, 2944], BF16); o += 5888
    sqt = A.at(o, [128, S], BF16); o += 4 * KB
    E = [A.at(o + i * KB, [128, 512], BF16) for i in range(3)]; o += 3 * KB
    PT = [A.at(o + i * KB, [128, 512], BF16) for i in range(4)]; o += 4 * KB
    mx = A.at(o, [128, 16, 4], F32); o += 256
    mx2 = A.at(o, [128, 16], F32); o += 64
    negm = A.at(o, [128, 8], F32); o += 32
    rden = [A.at(o + i * 16, [128, 4], F32) for i in range(2)]; o += 32
    ssa = A.at(o, [128, 16], F32); o += 64
    rstdA = A.at(o, [128, 16], F32); o += 64
    halfones = A.at(o, [128, 2, 128], BF16); o += 512
    attn_g = A.at(o, [128, 4], F32); o += 32
    yn = A.at(o, [128, 4, 512], BF16); o += 4 * KB
    junk = A.at(o, [128, 512], BF16); o += KB
    assert o <= ARENA

    P.dma("sp", tmask, dr["tmask"], "c_tmask", writes=["tmask"])
    P.dma("sp", halfones, dr["halfones"], "c_halfones", writes=["halfones"])
    P.dma("sp", attn_g, dr["attn_g"], "c_attn_g", writes=["attn_g"])

    nb = 0
    for qk, src, nm in ((0, qaT, "qaT"), (1, kaT, "kaT")):
        for cc in range(4):
            P.act(sqt, src[:, cc, :], AF.Square, reads=[(nm, cc, n) for n in range(4)], writes=["sqt"])
            for a in range(2):
                for g in range(4):
                    b = nb % 4; nb += 1
                    P.mm(ps[b][:, :], halfones[:, a, :], sqt[:, g * 512:(g + 1) * 512], reads=["sqt", "halfones"], writes=["ps%d" % b])
                    P.op("dve", lambda e, b=b, col=qk * 8 + cc * 2 + a, g=g: e.reduce_max(out=mx[:, col, g:g + 1], in_=ps[b][:, :], axis=AX.X),
                         reads=["ps%d" % b], writes=["mx"])
    P.op("dve", lambda e: e.reduce_max(out=mx2, in_=mx, axis=AX.X), reads=["mx"], writes=["mx2"])
    P.tt("dve", negm, mx2[:, 0:8], mx2[:, 8:16], ALU.mult, reads=["mx2"], writes=["negm"])
    P.act(negm, negm, AF.Sqrt, reads=["negm"], writes=["negm"])
    P.ts("dve", negm, negm, -1.0, ALU.mult, reads=["negm"], writes=["negm"])

    SKEW = 2
    iters = []
    grp = 0
    for h in range(8):
        for G in range(4):
            Js = list(range(max(0, 4 * G - 8), min(15, 4 * G + 3 + 8) + 1))
            lastJ = [max(j for j in Js if abs(j - (4 * G + i)) <= 8) for i in range(4)]
            firstJ = [min(j for j in Js if abs(j - (4 * G + i)) <= 8) for i in range(4)]
            for J in Js:
                iters.append((h, G, J, firstJ, lastJ, J == Js[-1], grp))
            grp += 1
    n_it = len(iters)
    sbanks = [0, 1, 6]

    def front(it):
        h, G, J = iters[it][0:3]
        cc, a = h // 2, h % 2
        pr = slice(a * 64, a * 64 + 64)
        sb = sbanks[it % 3]
        e_i = it % 3
        p_i = it % 4
        P.mm(ps[sb][:, :], kaT[pr, cc, J * 128:(J + 1) * 128], qaT[pr, cc, G * 512:(G + 1) * 512],
             reads=[("kaT", cc, J // 4), ("qaT", cc, G)], writes=["ps%d" % sb])
        P.act(E[e_i], ps[sb][:, :], AF.Exp, bias=negm[:, h:h + 1], reads=["ps%d" % sb, "negm"], writes=[("E", e_i)])
        u0 = 1408 - (J * 128 - G * 512)
        P.tt("dve", PT[p_i], E[e_i], tmask[:, u0:u0 + 512], ALU.mult, reads=[("E", e_i), "tmask"], writes=[("PT", p_i)])

    def back(it):
        h, G, J, firstJ, lastJ, is_last, g = iters[it]
        p_i = it % 4
        for i in range(4):
            I = 4 * G + i
            if abs(I - J) > 8:
                continue
            P.mm(ps[2 + i][:, 0:65], PT[p_i][:, i * 128:(i + 1) * 128], va[:, J, h, :], start=(J == firstJ[i]), stop=(J == lastJ[i]),
                 reads=[("PT", p_i), ("va", J), "va_ones"], writes=["ps%d" % (2 + i)])
        if is_last:
            rd = rden[g % 2]; rk = "rden%d" % (g % 2)
            for i in range(4):
                I = 4 * G + i
                P.recip(rd[:, i:i + 1], ps[2 + i][:, 64:65], reads=["ps%d" % (2 + i)], writes=[(rk, i)])
                P.ts("dve", yA[:, I, h * 64:(h + 1) * 64], ps[2 + i][:, 0:64], rd[:, i:i + 1], ALU.mult,
                     reads=["ps%d" % (2 + i), (rk, i)], writes=[("yA", I)])

    for it in range(n_it + SKEW):
        if it < n_it:
            front(it)
        if it - SKEW >= 0:
            back(it - SKEW)

    for T in range(NT):
        P.act(junk, yA[:, T, :], AF.Square, accum_out=ssa[:, T:T + 1], reads=[("yA", T)], writes=["junk", "ssa"])
    P.act(rstdA, ssa, AF.Sqrt, bias=EPS, scale=1.0 / 512, reads=["ssa"], writes=["rstdA"])
    P.recip(rstdA, rstdA, reads=["rstdA"], writes=["rstdA"])
    for Tg in range(4):
        for i in range(4):
            T = Tg * 4 + i
            P.ts("dve", yn[:, i, :], yA[:, T, :], rstdA[:, T:T + 1], ALU.mult, reads=[("yA", T), "rstdA"], writes=[("yn", i)])
        for cc in range(4):
            b = cc % 2
            pT = ps[b][:, :].bitcast(BF16)
            for i in range(4):
                P.tr(pT[:, i * 128:(i + 1) * 128], yn[:, i, cc * 128:(cc + 1) * 128], c["ident_b"], reads=[("yn", i), "ident_b"], writes=["ps%d" % b])
            P.act(mixedT[:, 4 + cc, Tg * 512:(Tg + 1) * 512], pT[:, 0:512], AF.Copy, scale=attn_g[:, cc:cc + 1],
                  reads=["ps%d" % b, "attn_g"], writes=[("mixedT", 4 + cc, Tg)])
    return dict(mixedT=mixedT, yA=yA)


def phase_A2(P, A, dr, c, ps, a1, mixedT):
    xmT, szT = a1["xmT"], a1["szT"]
    o = O_QA
    xcT = A.at(o, [128, 4, S], BF16); o += 16 * KB
    qT = A.at(o, [128, 4, S], BF16); o += 16 * KB
    kT = A.at(o, [128, 4, S], BF16); o += 16 * KB
    ktok = A.at(o, [128, NT, 4, 128], BF16); o += 16 * KB
    vtok = A.at(o, [128, NT, 4, 129], BF16); o += 16512
    hsum = A.at(o, [128, NT, 2, 128], F32); o += 16 * KB
    vTt = [A.at(o, [128, S], BF16) for i in range(2)]; o += 4 * KB
    acc = A.at(o, [128, S], F32); o += 8 * KB
    acc3 = acc.rearrange("p (t d) -> p t d", t=NT)
    wbd_f = acc[:, 0:1536].rearrange("p (a b) -> p a b", a=12)
    wbd = A.at(o, [128, 3, 4, 128], BF16); o += 3 * KB
    wif_f = acc[:, 1536:1728].rearrange("p (a b) -> p a b", a=12)
    wif = A.at(o, [128, 12, 16], BF16); o += 384
    bif = A.at(o, [128, 256], F32); o += KB
    gates = A.at(o, [128, NT, 16], F32); o += KB
    gflat = gates.rearrange("p t g -> p (t g)")
    small = {}
    for nm in ("lf", "ii", "bcs", "esc", "wk", "enb", "eg", "tg1", "tg2"):
        small[nm] = A.at(o, [128, 2, NT, 4], F32); o += 512
    tri_f = A.at(o, [128, 2, 128], F32); o += KB
    tri_b = A.at(o, [128, 2, 128], BF16); o += 512
    ones_f = A.at(o, [128, 128], F32); o += 512
    St32 = [A.at(o + i * 516, [128, 129], F32) for i in range(4)]; o += 4 * 516
    Stb = [[A.at(o + (i * 2 + p) * 260, [128, 129], BF16) for p in range(2)] for i in range(4)]; o += 8 * 260
    WT = [A.at(o + i * 256, [128, 128], BF16) for i in range(8)]; o += 2 * KB
    khat = [A.at(o + i * 256, [128, 128], BF16) for i in range(8)]; o += 2 * KB
    tmpS = [wbd.rearrange("p a b c -> p (a b c)")[:, i * 128:(i + 1) * 128] for i in range(8)]
    den2 = [A.at(o + i * 8, [128, 2], F32) for i in range(8)]; o += 64
    den = [A.at(o + i * 4, [128, 1], F32) for i in range(8)]; o += 32
    ssh = A.at(o, [128, NT], F32); o += 64
    rstdh = A.at(o, [128, NT], F32); o += 64
    hn = A.at(11 * KB, [128, 4, 128], BF16)
    tmpx = [A.at(3 * KB + i * 2 * KB, [128, 512], F32) for i in range(2)]
    tmpy = [A.at(7 * KB + i * 2 * KB, [128, 512], F32) for i in range(2)]
    convw = A.at(o, [128, 4, 5], F32); o += 96
    convb = A.at(o, [128, 4], F32); o += 32
    mng = A.at(o, [128, 4], F32); o += 32
    msk = A.at(o, [128, 4], F32); o += 32
    assert o <= ARENA, o

    for nm, ap in (("bif", bif), ("tri", tri_f), ("convw", convw), ("convb", convb), ("mng", mng), ("msk", msk)):
        P.dma("sp", ap, dr[nm], "c2_" + nm, writes=["c2_" + nm])
    P.dma("sp", wbd_f, dr["wbd"], "c2_wbd", writes=["acc"])
    P.dma("sp", wif_f, dr["wif"], "c2_wif", writes=["acc"])
    P.copy("dve", wbd.rearrange("p a b c -> p (a b) c"), wbd_f, reads=["acc"], writes=["wbd"])
    P.copy("dve", wif, wif_f, reads=["acc"], writes=["wif"])
    P.copy("dve", tri_b, tri_f, reads=["c2_tri"], writes=["tri_b"])
    P.memset("dve", ones_f, 1.0, writes=["ones_f"])
    P.memset("pool", vtok[:, :, :, 128:129], 1.0, writes=["vtok_ones"])

    inv_sqrt_dh = 128 ** -0.5
    nb = 0
    first_gate = True
    for hc in range(4):
        x = xmT[:, hc, :]
        xm_res = [("xmT", hc, n) for n in range(4)]
        P.ts("dve", acc, x, convw[:, hc, 2:3], ALU.mult, reads=xm_res + ["c2_convw"], writes=["acc"])
        for j in (0, 1, 3, 4):
            sh = j - 2
            if sh < 0:
                P.stt(acc[:, -sh:S], x[:, 0:S + sh], convw[:, hc, j:j + 1], acc[:, -sh:S], ALU.mult, ALU.add, reads=["acc"], writes=["acc"])
            else:
                P.stt(acc[:, 0:S - sh], x[:, sh:S], convw[:, hc, j:j + 1], acc[:, 0:S - sh], ALU.mult, ALU.add, reads=["acc"], writes=["acc"])
        P.act(xcT[:, hc, :], acc, AF.Silu, bias=convb[:, hc:hc + 1], reads=["acc", "c2_convb"], writes=[("xcT", hc)])
        vt = vTt[0]; vk = "vTt0"
        for g in range(4):
            tok = slice(g * 512, (g + 1) * 512)
            b = nb % 4; nb += 1
            P.mm(ps[b][:, :], wbd[:, 0, hc, :], xcT[:, hc, tok], reads=["wbd", ("xcT", hc)], writes=["ps%d" % b])
            P.copy("act", qT[:, hc, tok], ps[b][:, :], reads=["ps%d" % b], writes=[("qT", hc, g)])
            b = nb % 4; nb += 1
            P.mm(ps[b][:, :], wbd[:, 1, hc, :], xcT[:, hc, tok], reads=["wbd", ("xcT", hc)], writes=["ps%d" % b])
            P.ts("dve", kT[:, hc, tok], ps[b][:, :], inv_sqrt_dh, ALU.mult, reads=["ps%d" % b], writes=[("kT", hc, g)])
            b = nb % 4; nb += 1
            P.mm(ps[b][:, :], wbd[:, 2, hc, :], x[:, tok], reads=["wbd"] + xm_res, writes=["ps%d" % b])
            P.copy("act", vt[:, tok], ps[b][:, :], reads=["ps%d" % b], writes=[(vk, g)])
        for g in range(4):
            b = nb % 4; nb += 1
            for i in range(4):
                T = 4 * g + i
                P.mm(ps[b][:, i * 128:(i + 1) * 128], xcT[:, hc, T * 128:(T + 1) * 128], wbd[:, 1, hc, :], reads=["wbd", ("xcT", hc)], writes=["ps%d" % b])
            P.ts("dve", ktok[:, 4 * g:4 * g + 4, hc, :], ps[b][:, :].rearrange("p (i d) -> p i d", i=4), inv_sqrt_dh, ALU.mult,
                 reads=["ps%d" % b], writes=[("ktok", hc, g)])
            b = nb % 4; nb += 1
            for i in range(4):
                T = 4 * g + i
                P.mm(ps[b][:, i * 128:(i + 1) * 128], x[:, T * 128:(T + 1) * 128], wbd[:, 2, hc, :], reads=["wbd"] + xm_res, writes=["ps%d" % b])
            P.copy("act", vtok[:, 4 * g:4 * g + 4, hc, 0:128], ps[b][:, :].rearrange("p (i d) -> p i d", i=4),
                   reads=["ps%d" % b], writes=[("vtok", hc, g)])
        for ch, src, rk in ((hc, qT[:, hc, :], [("qT", hc, g) for g in range(4)]),
                            (4 + hc, kT[:, hc, :], [("kT", hc, g) for g in range(4)]),
                            (8 + hc, vt, [(vk, g) for g in range(4)])):
            for T in range(NT):
                P.mm(ps[4][:, T * 16:(T + 1) * 16], src[:, T * 128:(T + 1) * 128], wif[:, ch, :], reads=rk + ["wif"], writes=["ps4"])
            if first_gate:
                P.tt("dve", gflat, ps[4][:, 0:256], bif, ALU.add, reads=["ps4", "c2_bif"], writes=["gates"])
                first_gate = False
            else:
                P.tt("dve", gflat, ps[4][:, 0:256], gflat, ALU.add, reads=["ps4", "gates"], writes=["gates"])

    sm = small
    fl = lambda ap: ap.rearrange("p d t h -> p (d t h)")
    for d in range(2):
        i_pre = gates[:, :, d * 8:d * 8 + 4]
        f_pre = gates[:, :, d * 8 + 4:d * 8 + 8]
        P.act(sm["tg1"][:, d], f_pre, AF.Exp, scale=-1.0, reads=["gates"], writes=[("tg1", d)])
        P.act(sm["tg2"][:, d], sm["tg1"][:, d], AF.Ln, bias=1.0, reads=[("tg1", d)], writes=[("tg2", d)])
        P.ts("dve", sm["lf"][:, d], sm["tg2"][:, d], -1.0, ALU.mult, reads=[("tg2", d)], writes=[("lf", d)])
        P.copy("dve", sm["ii"][:, d], i_pre, reads=["gates"], writes=[("ii", d)])
    lf2 = [sm["lf"][:, d].rearrange("p t h -> p (t h)") for d in range(2)]
    P.mm(ps[5][:, 0:64], tri_f[:, 0, :], lf2[0], reads=[("lf", 0), "c2_tri"], writes=["ps5"])
    P.mm(ps[5][:, 64:128], tri_f[:, 1, :], lf2[1], reads=[("lf", 1), "c2_tri"], writes=["ps5"])
    P.mm(ps[6][:, 0:128], ones_f, fl(sm["lf"]), reads=[("lf", 0), ("lf", 1), "ones_f"], writes=["ps6"])
    P.copy("dve", fl(sm["bcs"]), ps[5][:, 0:128], reads=["ps5"], writes=["bcs"])
    P.tt("dve", fl(sm["tg1"]), fl(sm["ii"]), fl(sm["bcs"]), ALU.subtract, reads=[("ii", 0), ("ii", 1), "bcs", ("tg1", 0), ("tg1", 1)],
         writes=[("tg1", 0), ("tg1", 1)])
    P.act(fl(sm["esc"]), fl(sm["tg1"]), AF.Exp, reads=[("tg1", 0), ("tg1", 1)], writes=["esc"])
    P.act(fl(sm["enb"]), fl(sm["bcs"]), AF.Exp, scale=-1.0, reads=["bcs"], writes=["enb"])
    P.act(fl(sm["eg"]), ps[6][:, 0:128], AF.Exp, reads=["ps6"], writes=["eg"])
    P.tt("dve", fl(sm["wk"]), fl(sm["esc"]), fl(sm["eg"]), ALU.mult, reads=["esc", "eg"], writes=["wk"])

    cnt = {"w": 0, "k": 0, "d": 0}
    for pair in range(2):
        hp = (2 * pair, 2 * pair + 1)
        streams = [(d, hl) for d in range(2) for hl in range(2)]
        for si in range(4):
            P.memset("pool", St32[si], 0.0, writes=[("St32", si)])
            P.memset("pool", Stb[si][1], 0.0, writes=[("Stb", si, 1)])
        for step in range(NT):
            inf = []
            for k, (d, hl) in enumerate(streams):
                T = step if d == 0 else NT - 1 - step
                w = cnt["w"] % 8; cnt["w"] += 1
                kk = cnt["k"] % 8; cnt["k"] += 1
                inf.append(dict(d=d, hl=hl, h=hp[hl], si=d * 2 + hl, T=T, Tsl=slice(T * 128, (T + 1) * 128), g4=T // 4, w=w, kk=kk, k=k))
            bS = step % 2
            for q in inf:
                d, h, T, Tsl, g4, w, k = q["d"], q["h"], q["T"], q["Tsl"], q["g4"], q["w"], q["k"]
                rS = "ps%d" % bS
                P.mm(ps[bS][:, k * 128:(k + 1) * 128], kT[:, h, Tsl], qT[:, h, Tsl], reads=[("kT", h, g4), ("qT", h, g4)], writes=[rS])
                P.act(tmpS[w], ps[bS][:, k * 128:(k + 1) * 128], AF.Copy, scale=sm["esc"][:, d, T, h:h + 1], reads=[rS, "esc"], writes=[("tmpS", w)])
                P.tt("pool", WT[w], tmpS[w], tri_b[:, d, :], ALU.mult, reads=[("tmpS", w), "tri_b"], writes=[("WT", w)])
                if step < NT - 1:
                    P.act(khat[q["kk"]], ktok[:, T, h, :], AF.Copy, scale=sm["wk"][:, d, T, h:h + 1], reads=[("ktok", h, g4), "wk"],
                          writes=[("khat", q["kk"])])
            if step < NT - 1:
                for q in inf:
                    d, h, si, T, g4, k, kk = q["d"], q["h"], q["si"], q["T"], q["g4"], q["k"], q["kk"]
                    bC = 6 + k // 2
                    co = (k % 2) * 256
                    rC = "ps%d" % bC
                    P.mm(ps[bC][:, co:co + 129], khat[kk], vtok[:, T, h, :], reads=[("khat", kk), ("vtok", h, g4), "vtok_ones"], writes=[rC])
                    P.stt(St32[si], St32[si], sm["eg"][:, d, T, h:h + 1], ps[bC][:, co:co + 129], ALU.mult, ALU.add,
                          reads=[("St32", si), "eg", rC], writes=[("St32", si)])
                    P.copy("dve", Stb[si][step % 2], St32[si], reads=[("St32", si)], writes=[("Stb", si, step % 2)])
            pendO = []
            for q in inf:
                d, hl, h, si, T, Tsl, g4, w, k = q["d"], q["hl"], q["h"], q["si"], q["T"], q["Tsl"], q["g4"], q["w"], q["k"]
                bO = 2 + (step % 2) * 2 + k // 2
                co = (k % 2) * 256
                rO = "ps%d" % bO
                P.mm(ps[bO][:, co:co + 129], WT[w], vtok[:, T, h, :], start=True, stop=False,
                     reads=[("WT", w), ("vtok", h, g4), "vtok_ones"], writes=[rO])
                P.mm(ps[bO][:, co:co + 129], qT[:, h, Tsl], Stb[si][(step + 1) % 2], start=False, stop=True,
                     reads=[("qT", h, g4), ("Stb", si, (step + 1) % 2)], writes=[rO])
                if k % 2 == 1:
                    dn = den2[cnt["d"] % 8]; dk = ("den2", cnt["d"] % 8); cnt["d"] += 1
                    h0 = hp[0]
                    P.stt(dn, ps[bO][:, 128::256], -1.0, sm["enb"][:, d, T, h0:h0 + 2], ALU.mult, ALU.max, reads=[rO, "enb"], writes=[dk])
                    P.tt("dve", dn, dn, ps[bO][:, 128::256], ALU.max, reads=[rO, dk], writes=[dk])
                    P.recip(dn, dn, reads=[dk], writes=[dk])
                    pendO.append((dn, dk))
                continue
            for q in inf:
                d, hl, h, si, T, Tsl, g4, w, k = q["d"], q["hl"], q["h"], q["si"], q["T"], q["Tsl"], q["g4"], q["w"], q["k"]
                bO = 2 + (step % 2) * 2 + k // 2
                co = (k % 2) * 256
                rO = "ps%d" % bO
                dnp, dk = pendO[k // 2]
                dn = dnp[:, k % 2:k % 2 + 1]
                is_first = (d == 0) == (T < 8)
                if is_first:
                    P.act(hsum[:, T, hl, :], ps[bO][:, co:co + 128], AF.Copy, scale=dn, reads=[rO, dk], writes=[("hsum", T, hl)])
                else:
                    P.stt(hsum[:, T, hl, :], ps[bO][:, co:co + 128], dn, hsum[:, T, hl, :], ALU.mult, ALU.add,
                          reads=[rO, dk, ("hsum", T, hl)], writes=[("hsum", T, hl)])
        for hl in range(2):
            h = hp[hl]
            P.act(acc3, hsum[:, :, hl, :], AF.Square, reads=[("hsum", T, hl) for T in range(NT)], writes=["acc"])
            P.op("dve", lambda e: e.reduce_sum(out=ssh, in_=acc3, axis=AX.X), reads=["acc"], writes=["ssh"])
            P.act(rstdh, ssh, AF.Sqrt, bias=EPS, scale=1.0 / 128, reads=["ssh"], writes=["rstdh"])
            P.recip(rstdh, rstdh, reads=["rstdh"], writes=["rstdh"])
            for Tg in range(4):
                tok = slice(Tg * 512, (Tg + 1) * 512)
                b = 6 + Tg % 2
                pT = ps[b][:, :].bitcast(BF16)
                for i in range(4):
                    T = Tg * 4 + i
                    P.act(hn[:, i, :], hsum[:, T, hl, :], AF.Copy, scale=rstdh[:, T:T + 1], reads=[("hsum", T, hl), "rstdh"], writes=[("hn", i)])
                    P.tr(pT[:, i * 128:(i + 1) * 128], hn[:, i, :], c["ident_b"], reads=[("hn", i), "ident_b"], writes=["ps%d" % b])
                tx = tmpx[Tg % 2]; txk = "tmpx%d" % (Tg % 2)
                ty = tmpy[Tg % 2]; tyk = "tmpy%d" % (Tg % 2)
                P.act(tx, xcT[:, h, tok], AF.Copy, scale=msk[:, h:h + 1], reads=[("xcT", h), "c2_msk"], writes=[txk])
                P.stt(ty, pT[:, 0:512], mng[:, h:h + 1], tx, ALU.mult, ALU.add, reads=["ps%d" % b, "c2_mng", txk], writes=[tyk])
                P.tt("pool", mixedT[:, h, tok], ty, szT[:, h, tok], ALU.mult, reads=[tyk, ("szT", h, Tg)], writes=[("mixedT", h, Tg)])


O_X1 = 140 * KB


def phase_A4(P, A, dr, c, ps, mixedT):
    x1 = A.at(O_X1, [128, NT, D], F32)
    Woutb = A.at(O_QA, [128, 8, D], BF16)
    for kc in range(8):
        P.dma("pool", Woutb[:, kc, :], dr["w_out"][kc * 128:(kc + 1) * 128, :], "wout", writes=[("Woutb", kc)])
    xv = dr["x"].rearrange("(t p) d -> p t d", p=128)
    for T in range(NT):
        P.dma("sp", x1[:, T, :], xv[:, T, :], "x1ld%d" % (T % 4), writes=[("x1", T)])
    nb = 0
    for T in range(NT):
        for nh in range(2):
            b = nb % 4; nb += 1
            for kc in range(8):
                P.mm(ps[b][:, :], mixedT[:, kc, T * 128:(T + 1) * 128], Woutb[:, kc, nh * 512:(nh + 1) * 512], start=(kc == 0), stop=(kc == 7),
                     reads=[("mixedT", kc, T // 4), ("Woutb", kc)], writes=["ps%d" % b])
            P.tt("dve", x1[:, T, nh * 512:(nh + 1) * 512], ps[b][:, :], x1[:, T, nh * 512:(nh + 1) * 512], ALU.add,
                 reads=["ps%d" % b, ("x1", T)], writes=[("x1", T)])
    return x1


def host_inputs(inp, b):
    import ml_dtypes
    bf = ml_dtypes.bfloat16
    f32 = np.float32
    cosT, sinT, PT = rope_tables()
    d = {}
    xb = np.asarray(inp["x"][b], f32)
    d["x"] = np.ascontiguousarray(xb)
    d["xT"] = np.ascontiguousarray(xb.T)
    d["w_in"] = np.ascontiguousarray(inp["w_in"][0], dtype=f32)
    col = lambda v, n: np.ascontiguousarray(np.asarray(v, f32).reshape(n, 128).T)
    d["g1"] = col(inp["norm1_g"][0], 8)
    d["ropeC"] = cosT.astype(bf)
    d["ropeS"] = sinT.astype(bf)
    d["ropePT"] = PT.astype(bf)
    d["ident_f"] = np.eye(128, dtype=f32)
    d["ident_b"] = np.eye(128).astype(bf)
    d["tmask"] = toeplitz_mask().astype(bf)
    ho = np.zeros((128, 2, 128), f32)
    ho[:64, 0, :] = 1.0
    ho[64:, 1, :] = 1.0
    d["halfones"] = ho.astype(bf)
    d["attn_g"] = col(inp["attn_norm_g"][0], 4)
    wbd = np.zeros((128, 12, 128), f32)
    for m, key in enumerate(("wq_m", "wk_m", "wv_m")):
        w = np.asarray(inp[key][0], f32)
        for hc in range(4):
            for gl in range(32):
                wbd[gl * 4:gl * 4 + 4, m * 4 + hc, gl * 4:gl * 4 + 4] = w[hc * 32 + gl]
    d["wbd"] = wbd
    wif = np.concatenate([np.asarray(inp["w_if_fwd"][0], f32), np.asarray(inp["w_if_bwd"][0], f32)], axis=1)
    d["wif"] = np.ascontiguousarray(wif.reshape(12, 128, 16).transpose(1, 0, 2))
    brow = np.concatenate([np.asarray(inp["b_if_fwd"][0], f32), np.asarray(inp["b_if_bwd"][0], f32)])
    d["bif"] = np.ascontiguousarray(np.broadcast_to(np.tile(brow, 16)[None, :], (128, 256)))
    tri = np.zeros((128, 2, 128), f32)
    ss, tt = np.meshgrid(np.arange(128), np.arange(128), indexing="ij")
    tri[:, 0, :] = (ss <= tt)
    tri[:, 1, :] = (ss >= tt)
    d["tri"] = tri
    cw = np.asarray(inp["conv_w"][0], f32)
    d["convw"] = np.ascontiguousarray(cw.T.reshape(4, 128, 5).transpose(1, 0, 2))
    d["convb"] = col(inp["conv_b"][0], 4)
    d["mng"] = col(inp["mlstm_norm_g"][0], 4)
    d["msk"] = col(inp["mlstm_skip"][0], 4)
    d["w_out"] = np.ascontiguousarray(inp["w_out"][0], dtype=f32)
    d["g2bc"] = np.ascontiguousarray(np.broadcast_to(np.asarray(inp["norm2_g"][0], f32)[None, :], (128, D)))
    d["gfbc"] = np.ascontiguousarray(np.broadcast_to(np.asarray(inp["norm_f_g"], f32)[None, :], (128, D)))
    d["wr"] = np.ascontiguousarray(np.asarray(inp["w_router"][0], f32).reshape(8, 128, NE).transpose(1, 0, 2))
    es = np.zeros((16, NE * 128), f32)
    for e in range(NE):
        es[e, e * 128:(e + 1) * 128] = 1.0
    d["esel"] = es.astype(bf)
    d["iota_row"] = np.ascontiguousarray(np.broadcast_to(np.arange(256, dtype=f32)[None, :], (128, 256)))
    d["iota_p"] = np.stack([np.arange(128, dtype=f32), np.arange(128, dtype=f32) + 128], axis=1)
    if "w1" in inp:
        d["w1"] = np.ascontiguousarray(inp["w1"][0], dtype=f32)
        d["w3"] = np.ascontiguousarray(inp["w3"][0], dtype=f32)
        d["w2"] = np.ascontiguousarray(inp["w2"][0], dtype=f32)
    return d


IN_SPECS = {
    "x": ([S, D], F32), "xT": ([D, S], F32), "w_in": ([D, DIN], F32), "g1": ([128, 8], F32),
    "ropeC": ([128, S], BF16), "ropeS": ([128, S], BF16), "ropePT": ([128, 128], BF16),
    "ident_f": ([128, 128], F32), "ident_b": ([128, 128], BF16),
    "tmask": ([128, 2944], BF16), "halfones": ([128, 2, 128], BF16), "attn_g": ([128, 4], F32),
    "wbd": ([128, 12, 128], F32), "wif": ([128, 12, 16], F32), "bif": ([128, 256], F32), "tri": ([128, 2, 128], F32),
    "convw": ([128, 4, 5], F32), "convb": ([128, 4], F32), "mng": ([128, 4], F32), "msk": ([128, 4], F32),
    "w_out": ([D, D], F32),
    "g2bc": ([128, D], F32), "gfbc": ([128, D], F32), "wr": ([128, 8, NE], F32), "esel": ([16, NE * 128], BF16),
    "iota_row": ([128, 256], F32), "iota_p": ([128, 2], F32),
    "w1": ([NE, D, DFF], F32), "w3": ([NE, D, DFF], F32), "w2": ([NE, DFF, D], F32),
}


def build(debug=None):
    nc = bass.Bass("TRN2", target_bir_lowering=False)
    early = debug is not None and debug[0] in "AB"
    dr = {k: nc.dram_tensor(k, sh, dt, kind="ExternalInput").ap() for k, (sh, dt) in IN_SPECS.items()
          if not (early and k in ("w1", "w2", "w3"))}
    out = nc.dram_tensor("out", [S, D], F32, kind="ExternalOutput").ap()
    A = Arena(nc, ARENA)
    ps = [nc.alloc_psum_tensor("ps%d" % i, [128, 512], F32) for i in range(8)]
    P = Prog(nc)
    finals = []

    def dump(name, ap, shape, dt):
        t = nc.dram_tensor("dbg_" + name, shape, dt, kind="ExternalOutput").ap()
        P.barrier()
        if len(shape) >= 3 and shape[1] > 4:
            for i in range(shape[1]):
                P.dma("sp", t[:, i], ap[:, i])
        else:
            P.dma("sp", t, ap)

    c = build_consts(P, A, dr)
    a1 = phase_A1(P, A, dr, c, ps)
    if debug == "A1":
        for k in ("xmT", "szT", "qaT", "kaT"):
            dump(k, a1[k], [128, 4, S], BF16)
        dump("va", a1["va"], [128, NT, 8, 65], BF16)
        P.emit(finals)
        return nc
    P.barrier()
    a3 = phase_A3(P, A, dr, c, ps, a1)
    if debug == "A3":
        dump("yA", a3["yA"], [128, NT, 512], F32)
        P.memset("dve", a3["mixedT"][:, 0:4, :], 0.0, writes=[("mixedT", i, j) for i in range(4) for j in range(4)])
        dump("mixedT", a3["mixedT"], [128, 8, S], BF16)
        P.emit(finals)
        return nc
    P.barrier()
    phase_A2(P, A, dr, c, ps, a1, a3["mixedT"])
    if debug == "A2":
        dump("mixedT", a3["mixedT"], [128, 8, S], BF16)
        P.emit(finals)
        return nc
    P.barrier()
    x1 = phase_A4(P, A, dr, c, ps, a3["mixedT"])
    if debug == "A4":
        dump("mixedT", a3["mixedT"], [128, 8, S], BF16)
        dump("x1", x1, [128, NT, D], F32)
        P.emit(finals)
        return nc
    P.barrier()
    rb = phase_B(P, A, dr, c, ps, x1)
    if debug == "B":
        dump("aff", rb["aff"], [128, NT, NE], F32)
        dump("posm_tok", rb["posm_tok"], [128, NT, NE], F32)
        dump("h2b", rb["h2b"], [128, NT, D], BF16)
        P.emit(finals)
        return nc
    P.barrier()
    nexp = NE if debug is None else int(debug[1:]) if debug.startswith("C") else NE
    if debug is not None and debug.startswith("C"):
        dump("x1pre", x1, [128, NT, D], F32)
        P.barrier()
    phase_C(P, A, dr, c, ps, x1, rb, experts=range(nexp))
    if debug is not None and debug.startswith("C"):
        dump("x2", x1, [128, NT, D], F32)
        dump("aff", rb["aff"], [128, NT, NE], F32)
        dump("posm_tok", rb["posm_tok"], [128, NT, NE], F32)
        P.emit(finals)
        return nc
    P.barrier()
    phase_D(P, A, dr, c, ps, x1, out)
    P.emit(finals)
    return nc


def run(inp, cores=(0,), debug=None):
    nc = build(debug)
    in_maps = [host_inputs(inp, b) for b in cores]
    names = [a.memorylocations[0].name for a in nc.m.functions[0].allocations
             if isinstance(a, mybir.MemoryLocationSet) and a.kind == "ExternalInput"]
    in_maps = [{k: v for k, v in m.items() if k in names} for m in in_maps]
    res = run_bass_kernel_spmd(nc, in_maps, core_ids=list(range(len(cores))))
    return res.results


O_H2 = 12 * KB
O_BC = 44 * KB
O_PER = 132 * KB


def phase_B(P, A, dr, c, ps, x1):
    h2b = A.at(O_H2, [128, NT, D], BF16)
    o = O_PER
    aff = A.at(o, [128, NT, NE], F32); o += KB
    posm_tok = A.at(o, [128, NT, NE], F32); o += KB
    posmT_b = A.at(o, [16, S], BF16); o += 4 * KB
    assert o <= O_X1
    esel = A.at(3 * KB, [16, NE * 128], BF16)
    iota_row = A.at(7 * KB, [128, 256], F32)
    iota_p = A.at(8 * KB, [128, 2], F32)
    o = O_BC
    g2bc = A.at(o, [128, D], F32); o += 4 * KB
    h2f = [A.at(o + i * 4 * KB, [128, D], F32) for i in range(2)]; o += 8 * KB
    h2fT = A.at(o, [128, 8, 128], F32); o += 4 * KB
    junk = A.at(o, [128, D], BF16); o += 2 * KB
    wr = A.at(o, [128, 8, NE], F32); o += 512
    ss2 = A.at(o, [128, NT], F32); o += 64
    rstd2 = A.at(o, [128, NT], F32); o += 64
    mxs = A.at(o, [128, 4], F32); o += 16
    sms = A.at(o, [128, 4], F32); o += 16
    ex = [A.at(o + i * 64, [128, NE], F32) for i in range(2)]; o += 128
    affT = A.at(o, [16, S], F32); o += 8 * KB
    work = A.at(o, [16, S], F32); o += 8 * KB
    ones16 = A.at(o, [16, S], F32); o += 8 * KB
    cs = A.at(o, [16, S], F32); o += 8 * KB
    m8 = A.at(o, [16, 8], F32); o += 32
    assert o <= O_PER

    P.dma("sp", g2bc, dr["g2bc"], writes=["g2bc"])
    P.dma("sp", wr, dr["wr"], writes=["wr"])
    P.dma("sp", esel, dr["esel"], writes=["esel"])
    P.dma("sp", iota_row, dr["iota_row"], writes=["iota_row"])
    P.dma("sp", iota_p, dr["iota_p"], writes=["iota_p"])
    P.memset("pool", ones16, 1.0, writes=["ones16"])

    for T in range(NT):
        P.act(junk, x1[:, T, :], AF.Square, accum_out=ss2[:, T:T + 1], reads=[("x1", T)], writes=["junkB", "ss2"])
    P.act(rstd2, ss2, AF.Sqrt, bias=EPS, scale=1.0 / D, reads=["ss2"], writes=["rstd2"])
    P.recip(rstd2, rstd2, reads=["rstd2"], writes=["rstd2"])
    for T in range(NT):
        i2 = T % 2
        hf = h2f[i2]; hk = "h2f%d" % i2
        P.stt(hf, x1[:, T, :], rstd2[:, T:T + 1], g2bc, ALU.mult, ALU.mult, reads=[("x1", T), "rstd2", "g2bc"], writes=[hk])
        P.copy("act", h2b[:, T, :], hf, reads=[hk], writes=[("h2b", T)])
        for half in range(2):
            b = half
            for j in range(4):
                kc = half * 4 + j
                P.tr(ps[b][:, j * 128:(j + 1) * 128], hf[:, kc * 128:(kc + 1) * 128], c["ident_f"], reads=[hk, "ident_f"], writes=["ps%d" % b])
            P.copy("dve" if half == 0 else "act", h2fT[:, half * 4:half * 4 + 4, :], ps[b][:, :].rearrange("p (a b) -> p a b", a=4),
                   reads=["ps%d" % b], writes=[("h2fT", half)])
        for kc in range(8):
            P.mm(ps[2][:, 0:NE], h2fT[:, kc, :], wr[:, kc, :], start=(kc == 0), stop=(kc == 7),
                 reads=[("h2fT", kc // 4), "wr"], writes=["ps2"])
        m = T % 4
        P.op("dve", lambda e, m=m: e.reduce_max(out=mxs[:, m:m + 1], in_=ps[2][:, 0:NE], axis=AX.X), reads=["ps2"], writes=[("mxs", m)])
        P.ts("dve", mxs[:, m:m + 1], mxs[:, m:m + 1], -1.0, ALU.mult, reads=[("mxs", m)], writes=[("mxs", m)])
        P.act(ex[i2], ps[2][:, 0:NE], AF.Exp, bias=mxs[:, m:m + 1], accum_out=sms[:, m:m + 1], reads=["ps2", ("mxs", m)], writes=[("ex", i2), ("sms", m)])
        P.recip(sms[:, m:m + 1], sms[:, m:m + 1], reads=[("sms", m)], writes=[("sms", m)])
        P.ts("dve", aff[:, T, :], ex[i2], sms[:, m:m + 1], ALU.mult, reads=[("ex", i2), ("sms", m)], writes=[("aff", T)])
    for Tg in range(4):
        b = 4 + Tg % 2
        for i in range(4):
            T = Tg * 4 + i
            P.tr(ps[b][0:NE, i * 128:(i + 1) * 128], aff[:, T, :], c["ident_f"], reads=[("aff", T), "ident_f"], writes=["ps%d" % b])
        P.copy("dve", affT[:, Tg * 512:(Tg + 1) * 512], ps[b][0:NE, :], reads=["ps%d" % b], writes=["affT"])
    P.copy("dve", work, affT, reads=["affT"], writes=["work"])
    for r in range(CAP // 8):
        P.op("dve", lambda e: e.max(out=m8, in_=work), reads=["work"], writes=["m8"])
        if r < CAP // 8 - 1:
            P.op("dve", lambda e: e.match_replace(out=work, in_to_replace=m8, in_values=work, imm_value=-1.0), reads=["m8", "work"], writes=["work"])
    P.ts("dve", work, affT, m8[:, 7:8], ALU.is_ge, reads=["affT", "m8"], writes=["work"])
    P.op("dve", lambda e: e.tensor_tensor_scan(out=cs, data0=ones16, data1=work, initial=0.0, op0=ALU.mult, op1=ALU.add),
         reads=["ones16", "work"], writes=["cs"])
    P.tt("dve", cs, cs, work, ALU.mult, reads=["cs", "work"], writes=["cs"])
    P.ts("dve", posmT_b, cs, -1.0, ALU.add, reads=["cs"], writes=["posmT_b"])
    for Tg in range(4):
        b = 6 + Tg % 2
        pT = ps[b][:, :].bitcast(BF16)
        for i in range(4):
            T = Tg * 4 + i
            P.tr(pT[:, i * NE:(i + 1) * NE], posmT_b[:, T * 128:(T + 1) * 128], c["ident_b"][0:NE, 0:NE], reads=["posmT_b", "ident_b"], writes=["ps%d" % b])
        P.copy("dve", posm_tok[:, Tg * 4:Tg * 4 + 4, :], pT[:, 0:4 * NE].rearrange("p (a b) -> p a b", a=4), reads=["ps%d" % b], writes=["posm_tok"])
    return dict(h2b=h2b, aff=aff, posm_tok=posm_tok, posmT_b=posmT_b, esel=esel, iota_row=iota_row, iota_p=iota_p)


NFG = 11
NSLOT = 4


def phase_C(P, A, dr, c, ps, x1, rb, experts=range(NE)):
    h2b, aff, posm_tok, posmT_b = rb["h2b"], rb["aff"], rb["posm_tok"], rb["posmT_b"]
    esel, iota_row, iota_p = rb["esel"], rb["iota_row"], rb["iota_p"]
    experts = list(experts)
    o = O_BC
    Wslot = []
    for s in range(NSLOT):
        Wslot.append((A.at(o, [128, 8, 256], BF16), A.at(o + 4 * KB, [128, 8, 256], BF16), A.at(o + 8 * KB, [128, 2, D], BF16)))
        o += 12 * KB
    XeT = [A.at(o + i * 4 * KB, [128, 8, CAP], BF16) for i in range(2)]; o += 8 * KB
    Sel = A.at(o, [128, NT, CAP], BF16); o += 8 * KB
    SelT = [A.at(o + i * 8 * KB, [128, 2, S], BF16) for i in range(2)]; o += 16 * KB
    H = [A.at(o + i * 512, [128, CAP], BF16) for i in range(4)]; o += 2 * KB
    su = [A.at(o + i * 512, [128, CAP], BF16) for i in range(2)]; o += KB
    Yb = A.at(o, [128, 2, D], BF16); o += 4 * KB
    assert o <= O_PER, o

    w1v = dr["w1"].rearrange("e (kc p) f -> e p kc f", p=128)
    w3v = dr["w3"].rearrange("e (kc p) f -> e p kc f", p=128)
    w2v = dr["w2"].rearrange("e (fc p) d -> e p fc d", p=128)

    groups = [(e, fg) for e in experts for fg in range(NFG)]
    PREF = NSLOT - 1

    def load(gi):
        e, fg = groups[gi]
        s = gi % NSLOT
        W1g, W3g, W2g = Wslot[s]
        P.dma("pool", W1g, w1v[e, :, :, fg * 256:(fg + 1) * 256], writes=[("W1", s)])
        P.dma("pool", W3g, w3v[e, :, :, fg * 256:(fg + 1) * 256], writes=[("W3", s)])
        P.dma("pool", W2g, w2v[e, :, fg * 2:fg * 2 + 2, :], writes=[("W2", s)])

    for gi in range(min(PREF, len(groups))):
        load(gi)

    side_bank = [0]

    def sbank():
        b = 2 + side_bank[0] % 2
        side_bank[0] += 1
        return b

    def gen_sel(e):
        for T in range(NT):
            P.ts("dve", Sel[:, T, :], iota_row, posm_tok[:, T, e:e + 1], ALU.is_equal, reads=["iota_row", "posm_tok"], writes=[("Sel", T)])

    def gather_unit(e, xb, kp):
        b = sbank()
        for j in range(2):
            kc = kp * 2 + j
            for T in range(NT):
                P.mm(ps[b][:, j * 256:(j + 1) * 256], h2b[:, T, kc * 128:(kc + 1) * 128], Sel[:, T, :], start=(T == 0), stop=(T == NT - 1),
                     reads=[("h2b", T), ("Sel", T)], writes=["ps%d" % b])
        P.copy("act", XeT[xb][:, kp * 2:kp * 2 + 2, :], ps[b][:, :].rearrange("p (a b) -> p a b", a=2),
               reads=["ps%d" % b], writes=[("XeT", xb, kp)])

    def selT_unit(e, sbuf, tg):
        b = sbank()
        P.mm(ps[b][:, :], esel[:, e * 128:(e + 1) * 128], posmT_b[:, tg * 512:(tg + 1) * 512], reads=["esel", "posmT_b"], writes=["ps%d" % b])
        for sc in range(2):
            P.ts("dve", SelT[sbuf][:, sc, tg * 512:(tg + 1) * 512], ps[b][:, :], iota_p[:, sc:sc + 1], ALU.is_equal,
                 reads=["ps%d" % b, "iota_p"], writes=[("SelT", sbuf, tg)])

    def scatter_unit(e, sbuf, T, dh):
        b = sbank()
        for sc in range(2):
            P.mm(ps[b][:, :], SelT[sbuf][:, sc, T * 128:(T + 1) * 128], Yb[:, sc, dh * 512:(dh + 1) * 512], start=(sc == 0), stop=(sc == 1),
                 reads=[("SelT", sbuf, T // 4), ("Yb", sc * 2 + dh)], writes=["ps%d" % b])
        P.stt(x1[:, T, dh * 512:(dh + 1) * 512], ps[b][:, :], aff[:, T, e:e + 1], x1[:, T, dh * 512:(dh + 1) * 512], ALU.mult, ALU.add,
              reads=["ps%d" % b, ("aff", T), ("x1", T)], writes=[("x1", T)])

    gen_sel(experts[0])
    for kp in range(4):
        gather_unit(experts[0], 0, kp)

    gi = 0
    for ei, e in enumerate(experts):
        xb = ei % 2
        sbuf = ei % 2
        e_next = experts[ei + 1] if ei + 1 < len(experts) else None
        e_prev = experts[ei - 1] if ei > 0 else None
        side = {}
        if e_prev is not None:
            units = [(T, dh) for T in range(NT) for dh in range(2)]
            for i, (T, dh) in enumerate(units):
                side.setdefault(i // 2, []).append(lambda T=T, dh=dh: scatter_unit(e_prev, 1 - sbuf, T, dh))
        for tg in range(4):
            side.setdefault(16 + tg, []).append(lambda tg=tg: selT_unit(e, sbuf, tg))
        if e_next is not None:
            side.setdefault(0, []).insert(0, lambda: gen_sel(e_next))
            for kp in range(4):
                side.setdefault(5 + 4 * kp, []).append(lambda kp=kp: gather_unit(e_next, 1 - xb, kp))
        pend = None
        for fg in range(NFG):
            s = gi % NSLOT
            W1g, W3g, W2g = Wslot[s]
            for fc in range(2):
                fci = fg * 2 + fc
                b = fci % 2
                for kc in range(8):
                    P.mm(ps[b][:, 0:256], W1g[:, kc, fc * 128:(fc + 1) * 128], XeT[xb][:, kc, :], start=(kc == 0), stop=(kc == 7),
                         reads=[("W1", s), ("XeT", xb, kc // 2)], writes=["ps%d" % b])
                for kc in range(8):
                    P.mm(ps[b][:, 256:512], W3g[:, kc, fc * 128:(fc + 1) * 128], XeT[xb][:, kc, :], start=(kc == 0), stop=(kc == 7),
                         reads=[("W3", s), ("XeT", xb, kc // 2)], writes=["ps%d" % b])
                hi = fci % 4
                P.act(su[fci % 2], ps[b][:, 0:256], AF.Silu, reads=["ps%d" % b], writes=[("su", fci % 2)])
                P.tt("dve", H[hi], su[fci % 2], ps[b][:, 256:512], ALU.mult, reads=[("su", fci % 2), "ps%d" % b], writes=[("H", hi)])
                for u in side.get(fci, []):
                    u()
                if pend is not None:
                    pend()

                def down(fci=fci, hi=hi, W2g=W2g, fc=fc, s=s):
                    for sc in range(2):
                        for dh in range(2):
                            P.mm(ps[4 + sc * 2 + dh][:, :], H[hi][:, sc * 128:(sc + 1) * 128], W2g[:, fc, dh * 512:(dh + 1) * 512],
                                 start=(fci == 0), stop=(fci == 2 * NFG - 1), reads=[("H", hi), ("W2", s)], writes=["ps%d" % (4 + sc * 2 + dh)])
                pend = down
            gi += 1
            if fg == NFG - 1:
                pend()
                pend = None
            if gi - 1 + PREF < len(groups):
                load(gi - 1 + PREF)
        for sc in range(2):
            for dh in range(2):
                k = sc * 2 + dh
                P.copy("act" if k % 2 else "dve", Yb[:, sc, dh * 512:(dh + 1) * 512], ps[4 + k][:, :], reads=["ps%d" % (4 + k)], writes=[("Yb", k)])
    e_last = experts[-1]
    sb_last = (len(experts) - 1) % 2
    for T in range(NT):
        for dh in range(2):
            scatter_unit(e_last, sb_last, T, dh)


def phase_D(P, A, dr, c, ps, x1, out):
    o = O_BC
    gfbc = A.at(o, [128, D], F32); o += 4 * KB
    junk = A.at(o, [128, D], BF16); o += 2 * KB
    ssf = A.at(o, [128, NT], F32); o += 64
    rstdf = A.at(o, [128, NT], F32); o += 64
    ob = [A.at(o + i * 4 * KB, [128, D], F32) for i in range(4)]; o += 16 * KB
    P.dma("sp", gfbc, dr["gfbc"], writes=["gfbc"])
    for T in range(NT):
        P.act(junk, x1[:, T, :], AF.Square, accum_out=ssf[:, T:T + 1], reads=[("x1", T)], writes=["junkD", "ssf"])
    P.act(rstdf, ssf, AF.Sqrt, bias=EPS, scale=1.0 / D, reads=["ssf"], writes=["rstdf"])
    P.recip(rstdf, rstdf, reads=["rstdf"], writes=["rstdf"])
    ov = out.rearrange("(t p) d -> p t d", p=128)
    for T in range(NT):
        i = T % 4
        P.stt(ob[i], x1[:, T, :], rstdf[:, T:T + 1], gfbc, ALU.mult, ALU.mult, reads=[("x1", T), "rstdf", "gfbc"], writes=[("ob", i)])
        P.dma("sp", ov[:, T, :], ob[i], reads=[("ob", i)])


def kernel(**inputs):
    nc = build(None)
    names = [a.memorylocations[0].name for a in nc.m.functions[0].allocations
             if isinstance(a, mybir.MemoryLocationSet) and a.kind == "ExternalInput"]
    in_maps = []
    for b in range(8):
        m = host_inputs(inputs, b)
        in_maps.append({k: v for k, v in m.items() if k in names})
    res = run_bass_kernel_spmd(nc, in_maps, core_ids=list(range(8)))
    return np.stack([np.asarray(r["out"], dtype=np.float32) for r in res.results], axis=0)
```

```python
import numpy as np
import concourse.bass as bass
import concourse.mybir as mybir

F32 = mybir.dt.float32
BF16 = mybir.dt.bfloat16
I32 = mybir.dt.int32
U8 = mybir.dt.uint8
AF = mybir.ActivationFunctionType
ALU = mybir.AluOpType
AX = mybir.AxisListType
DTSIZE = {F32: 4, BF16: 2, I32: 4, U8: 1}

ENGS = ["pe", "act", "dve", "pool", "sp"]
SAME_ENG_SYNC = {"pe": False, "act": True, "dve": True, "pool": True, "sp": False}


class Op:
    __slots__ = ("eng", "fn", "deps", "is_dma", "semkey", "semval", "signal", "sigcount", "gid")

    def __init__(self, eng, fn):
        self.eng = eng
        self.fn = fn
        self.deps = set()
        self.is_dma = False
        self.semkey = None
        self.semval = 0
        self.signal = False
        self.sigcount = 0
        self.gid = 0


class Prog:
    def __init__(self, nc):
        self.nc = nc
        self.ops = {e: [] for e in ENGS}
        self.lastw = {}
        self.readers = {}
        self.dmacount = {}
        self.nops = 0
        self.barrier_set = None
        self.barrier_applied = {e: True for e in ENGS}
        self.last_dma = {}

    def _add(self, op, reads, writes):
        op.gid = self.nops
        self.nops += 1
        deps = op.deps
        for r in reads:
            w = self.lastw.get(r)
            if w is not None:
                deps.add(w)
        for r in writes:
            w = self.lastw.get(r)
            if w is not None:
                deps.add(w)
            for o in self.readers.get(r, {}).values():
                deps.add(o)
        rk = (op.eng, op.semkey) if op.is_dma else op.eng
        for r in reads:
            self.readers.setdefault(r, {})[rk] = op
        for r in writes:
            self.lastw[r] = op
            self.readers[r] = {}
        if not self.barrier_applied[op.eng]:
            deps |= self.barrier_set
            self.barrier_applied[op.eng] = True
        deps.discard(op)
        self.ops[op.eng].append(op)
        return op

    def op(self, eng, fn, reads=(), writes=()):
        return self._add(Op(eng, fn), reads, writes)

    RING = 16

    def dma(self, eng, out, in_, semkey=None, reads=(), writes=(), **kw):
        def fn(e, out=out, in_=in_, kw=kw):
            return e.dma_start(out=out, in_=in_, **kw)
        op = Op(eng, fn)
        op.is_dma = True
        rc = self.__dict__.setdefault("_ringcnt", {})
        i = rc.get(eng, 0)
        rc[eng] = i + 1
        semkey = "%s_r%d" % (eng, i % self.RING)
        op.semkey = semkey
        prev = self.last_dma.get(semkey)
        if prev is not None:
            op.deps.add(prev)
        self.dmacount[semkey] = self.dmacount.get(semkey, 0) + 16
        op.semval = self.dmacount[semkey]
        self.last_dma[semkey] = op
        return self._add(op, reads, writes)


    def mm(self, out, lhsT, rhs, start=True, stop=True, reads=(), writes=(), **kw):
        return self.op("pe", lambda e: e.matmul(out, lhsT=lhsT, rhs=rhs, start=start, stop=stop, **kw), reads, writes)

    def tr(self, out, in_, ident, reads=(), writes=()):
        return self.op("pe", lambda e: e.transpose(out, in_, ident), reads, writes)

    def act(self, out, in_, func, reads=(), writes=(), eng="act", **kw):
        return self.op(eng, lambda e: e.activation(out=out, in_=in_, func=func, **kw), reads, writes)

    def tt(self, eng, out, in0, in1, op, reads=(), writes=()):
        return self.op(eng, lambda e: e.tensor_tensor(out=out, in0=in0, in1=in1, op=op), reads, writes)

    def ts(self, eng, out, in0, s1, op0, s2=None, op1=None, reads=(), writes=(), **kw):
        if op1 is None:
            return self.op(eng, lambda e: e.tensor_scalar(out=out, in0=in0, scalar1=s1, scalar2=None, op0=op0, **kw), reads, writes)
        return self.op(eng, lambda e: e.tensor_scalar(out=out, in0=in0, scalar1=s1, scalar2=s2, op0=op0, op1=op1, **kw), reads, writes)

    def stt(self, out, in0, scalar, in1, op0, op1, reads=(), writes=(), eng="dve"):
        return self.op(eng, lambda e: e.scalar_tensor_tensor(out=out, in0=in0, scalar=scalar, in1=in1, op0=op0, op1=op1), reads, writes)

    def copy(self, eng, out, in_, reads=(), writes=()):
        if eng == "act":
            return self.op(eng, lambda e: e.copy(out=out, in_=in_), reads, writes)
        return self.op(eng, lambda e: e.tensor_copy(out=out, in_=in_), reads, writes)

    def recip(self, out, in_, reads=(), writes=()):
        return self.op("dve", lambda e: e.reciprocal(out=out, in_=in_), reads, writes)

    def memset(self, eng, ap, val, reads=(), writes=()):
        return self.op(eng, lambda e: e.memset(ap, val), reads, writes)

    def barrier(self):
        s = set()
        for e in ENGS:
            if self.ops[e]:
                s.add(self.ops[e][-1])
        for op in self.last_dma.values():
            s.add(op)
        self.barrier_set = s
        self.barrier_applied = {e: False for e in ENGS}

    def emit(self, final_dma_keys):
        nc = self.nc
        for e in ENGS:
            for op in self.ops[e]:
                for d in op.deps:
                    if not d.is_dma:
                        if d.eng == op.eng and not SAME_ENG_SYNC[op.eng]:
                            continue
                        d.signal = True
        for e in ENGS:
            c = 0
            for op in self.ops[e]:
                if (not op.is_dma) and op.signal:
                    c += 1
                op.sigcount = c
        from contextlib import ExitStack
        with ExitStack() as es:
            engsem = {e: es.enter_context(nc.semaphore("S_" + e)) for e in ENGS if e != "sp"}
            dmasem = {k: es.enter_context(nc.semaphore("D_" + str(k))) for k in self.dmacount}
            print("semaphores used:", len(engsem) + len(dmasem), "ops:", {e: len(self.ops[e]) for e in ENGS})
            block = es.enter_context(nc.Block())

            def run(ename, eng):
                waited = {}
                for op in self.ops[ename]:
                    for d in sorted(op.deps, key=lambda o: o.gid):
                        if d.is_dma:
                            key = ("d", d.semkey)
                            sem, val = dmasem[d.semkey], d.semval
                        else:
                            if d.eng == op.eng and not SAME_ENG_SYNC[op.eng]:
                                continue
                            key = ("e", d.eng)
                            sem, val = engsem[d.eng], d.sigcount
                        if waited.get(key, 0) >= val:
                            continue
                        eng.wait_ge(sem, val)
                        waited[key] = val
                    inst = op.fn(eng)
                    if op.is_dma:
                        inst.then_inc(dmasem[op.semkey], 16)
                    elif op.signal:
                        inst.then_inc(engsem[op.eng], 1)
                if ename == "sp":
                    for k in sorted(self.dmacount):
                        if waited.get(("d", k), 0) < self.dmacount[k]:
                            eng.wait_ge(dmasem[k], self.dmacount[k])

            block.tensor(lambda e: run("pe", e))
            block.scalar(lambda e: run("act", e))
            block.vector(lambda e: run("dve", e))
            block.gpsimd(lambda e: run("pool", e))
            block.sync(lambda e: run("sp", e))


class Arena:
    def __init__(self, nc, nbytes):
        self.nc = nc
        self.t = nc.alloc_sbuf_tensor("arena", [128, nbytes], U8)
        self.nbytes = nbytes

    def at(self, off, shape, dt):
        n = int(np.prod(shape[1:])) * DTSIZE[dt]
        assert off + n <= self.nbytes, (off, n, self.nbytes)
        assert off % 4 == 0
        ap = self.t[0:shape[0], off:off + n].bitcast(dt)
        if len(shape) == 3:
            ap = ap.rearrange("p (a b) -> p a b", a=shape[1])
        elif len(shape) == 4:
            ap = ap.rearrange("p (a b c) -> p a b c", a=shape[1], b=shape[2])
        return ap


from concourse.bass_utils import run_bass_kernel_spmd

S = 2048
D = 1024
NT = 16
DIN = 2560
EPS = 1.0000001e-6
KB = 1024
NE = 16
CAP = 256
DFF = 2816
NFC = 22

O_CONST = 0
O_XM = 12 * KB
O_SZ = 28 * KB
O_MIX = 44 * KB
O_QA = 76 * KB
O_KA = 92 * KB
O_VA = 108 * KB
O_T = 125 * KB
ARENA = 206 * KB


def build_consts(P, A, dr):
    c = {}
    off = [O_CONST]

    def alloc(shape, dt):
        n = int(np.prod(shape[1:])) * DTSIZE[dt]
        n = (n + 31) // 32 * 32
        ap = A.at(off[0], shape, dt)
        off[0] += n
        assert off[0] <= 3 * KB
        return ap

    c["ident_b"] = alloc([128, 128], BF16)
    c["ident_f"] = alloc([128, 128], F32)
    c["ones_b"] = alloc([128, 128], BF16)
    c["ropePT"] = alloc([128, 128], BF16)
    c["g1"] = alloc([128, 8], F32)
    for k in ("ident_b", "ident_f", "ropePT", "g1"):
        P.dma("sp", c[k], dr[k], "c_" + k, writes=[k])
    P.memset("dve", c["ones_b"], 1.0, writes=["ones_b"])
    return c


def phase_A1(P, A, dr, c, ps):
    nc = P.nc
    xmT = A.at(O_XM, [128, 4, S], BF16)
    szT = A.at(O_SZ, [128, 4, S], BF16)
    qaT = A.at(O_QA, [128, 4, S], BF16)
    kaT = A.at(O_KA, [128, 4, S], BF16)
    va = A.at(O_VA, [128, NT, 8, 65], BF16)
    xTs = A.at(O_MIX, [128, 8, 512], F32)
    xTb = A.at(O_MIX + 16 * KB, [128, 8, 512], BF16)
    sq = A.at(O_MIX + 24 * KB, [128, 8, 512], BF16)
    Wb = A.at(O_T, [128, 8, DIN], BF16)
    Wst = [A.at(O_T + 40 * KB + i * 10 * KB, [128, DIN], F32) for i in range(2)]
    o = O_T + 60 * KB
    cosT = A.at(o, [128, S], BF16); o += 4 * KB
    sinT = A.at(o, [128, S], BF16); o += 4 * KB
    rstd_bc = A.at(o, [128, 512], F32); o += 2 * KB
    tmpA = [A.at(o + i * 2 * KB, [128, 512], F32) for i in range(2)]; o += 4 * KB
    tmpAb = [tmpA[i].bitcast(BF16)[:, 0:512] for i in range(2)]
    tmp1 = [A.at(o + i * 2 * KB, [128, 512], F32) for i in range(2)]; o += 4 * KB
    tmp2 = [A.at(o, [128, 512], F32) for i in range(2)]; o += 2 * KB
    rstd_t = A.at(o, [128, 4], F32); o += 32
    assert o <= ARENA

    P.dma("sp", cosT, dr["ropeC"], "c_cos", writes=["cosT"])
    P.dma("sp", sinT, dr["ropeS"], "c_sin", writes=["sinT"])
    P.memset("pool", va[:, :, :, 64:65], 1.0, writes=["va_ones"])

    for kc in range(8):
        s = kc % 2
        P.dma("sp", Wst[s], dr["w_in"][kc * 128:(kc + 1) * 128, :], "wst%d" % s, writes=["Wst%d" % s])
        if kc % 2 == 0:
            P.ts("dve", Wb[:, kc, :], Wst[s], c["g1"][:, kc:kc + 1], ALU.mult, reads=["Wst%d" % s, "g1"], writes=[("Wb", kc)])
        else:
            P.act(Wb[:, kc, :], Wst[s], AF.Copy, scale=c["g1"][:, kc:kc + 1], reads=["Wst%d" % s, "g1"], writes=[("Wb", kc)])

    xT_v = dr["xT"].rearrange("(kc p) t -> p kc t", p=128)
    ev = 0
    pend_rope = []
    for n in range(4):
        tok = slice(n * 512, (n + 1) * 512)
        P.dma("sp", xTs, xT_v[:, :, tok], "xTs", writes=["xTs"])
        P.act(sq, xTs, AF.Square, reads=["xTs"], writes=["sq"])
        P.copy("dve", xTb, xTs, reads=["xTs"], writes=["xTb"])
        for kc in range(8):
            P.mm(ps[4][:, :], c["ones_b"], sq[:, kc, :], start=(kc == 0), stop=(kc == 7), reads=["sq", "ones_b"], writes=["ps4"])
        for tt in range(4):
            for kc in range(8):
                P.mm(ps[7][:, tt:tt + 1], sq[:, kc, tt * 128:(tt + 1) * 128], c["ones_b"][:, 0:1], start=(kc == 0), stop=(kc == 7),
                     reads=["sq", "ones_b"], writes=["ps7"])
        P.act(rstd_bc, ps[4][:, :], AF.Sqrt, bias=EPS, scale=1.0 / D, reads=["ps4"], writes=["rstd_bc"])
        P.recip(rstd_bc, rstd_bc, reads=["rstd_bc"], writes=["rstd_bc"])
        P.act(rstd_t, ps[7][:, 0:4], AF.Sqrt, bias=EPS, scale=1.0 / D, reads=["ps7"], writes=["rstd_t"])
        P.recip(rstd_t, rstd_t, reads=["rstd_t"], writes=["rstd_t"])
        for oc in range(16):
            b = oc % 4
            pb = ps[b]
            pk = "ps%d" % b
            for kc in range(8):
                P.mm(pb[:, :], Wb[:, kc, oc * 128:(oc + 1) * 128], xTb[:, kc, :], start=(kc == 0), stop=(kc == 7),
                     reads=["xTb", ("Wb", kc)], writes=[pk])
            while pend_rope:
                pend_rope.pop(0)()
            if oc < 4:
                P.tt("dve", xmT[:, oc, tok], pb[:, :], rstd_bc, ALU.mult, reads=[pk, "rstd_bc"], writes=[("xmT", oc, n)])
            elif oc < 8:
                t = tmpA[ev % 2]; tk = "tmpA%d" % (ev % 2); ev += 1
                P.tt("dve", t, pb[:, :], rstd_bc, ALU.mult, reads=[pk, "rstd_bc"], writes=[tk])
                P.act(szT[:, oc - 4, tok], t, AF.Silu, reads=[tk], writes=[("szT", oc - 4, n)])
            else:
                isq = oc < 12
                dst = qaT if isq else kaT
                cc = (oc - 8) % 4
                i2 = ev % 2; ev += 1
                t = tmpAb[i2]; tk = "tmpA%d" % i2
                P.stt(t, pb[:, :], (0.125 if isq else 1.0), rstd_bc, ALU.mult, ALU.mult, reads=[pk, "rstd_bc"], writes=[tk])

                def rope(t=t, tk=tk, i2=i2, dst=dst, cc=cc, isq=isq, tok=tok, n=n):
                    P.mm(ps[5][:, :], c["ropePT"], t, reads=[tk, "ropePT"], writes=["ps5"])
                    t1 = tmp1[i2]; t1k = "tmp1%d" % i2
                    t2 = tmp2[i2]; t2k = "tmp2"
                    P.tt("pool", t1, t, cosT[:, tok], ALU.mult, reads=[tk, "cosT"], writes=[t1k])
                    P.tt("dve", t2, ps[5][:, :], sinT[:, tok], ALU.mult, reads=["ps5", "sinT"], writes=[t2k])
                    P.tt("pool", dst[:, cc, tok], t1, t2, ALU.add, reads=[t1k, t2k], writes=[("qaT" if isq else "kaT", cc, n)])
                pend_rope.append(rope)
        for tt in range(4):
            T = n * 4 + tt
            for kc in range(8):
                P.mm(ps[6][:, :], xTb[:, kc, tt * 128:(tt + 1) * 128], Wb[:, kc, 2048:2560], start=(kc == 0), stop=(kc == 7),
                     reads=["xTb", ("Wb", kc)], writes=["ps6"])
            while pend_rope:
                pend_rope.pop(0)()
            P.act(va[:, T, :, 0:64], ps[6][:, :].rearrange("p (h d) -> p h d", h=8), AF.Copy, scale=rstd_t[:, tt:tt + 1],
                  reads=["ps6", "rstd_t"], writes=[("va", T)])
    return dict(xmT=xmT, szT=szT, qaT=qaT, kaT=kaT, va=va)


def rope_tables():
    half = 8
    inv_freq = 500000.0 ** (-2.0 * np.arange(half, dtype=np.float64) / 16.0)
    pos = np.arange(S, dtype=np.float64)
    ang = (pos[:, None].astype(np.float32) * inv_freq[None, :].astype(np.float32)).astype(np.float32).astype(np.float64)
    cosT = np.ones((128, S), np.float32)
    sinT = np.zeros((128, S), np.float32)
    for p in range(128):
        f = p % 64
        if f < 16:
            cosT[p] = np.cos(ang[:, f % 8])
            sinT[p] = np.sin(ang[:, f % 8])
    PT = np.zeros((128, 128), np.float32)
    for m in range(128):
        f = m % 64
        if f < 8:
            PT[m + 8, m] = -1.0
        elif f < 16:
            PT[m - 8, m] = 1.0
    return cosT, sinT, PT


def toeplitz_mask():
    U0 = 1408
    W = 2944
    p = np.arange(128)[:, None]
    u = np.arange(W)[None, :]
    d = p - u + U0
    ad = np.abs(d)
    cnt = (ad <= 64).astype(np.float32) + ((d % 4 == 0) & (ad <= 256)) + ((d % 16 == 0) & (ad <= 1024))
    return cnt.astype(np.float32)


def phase_A3(P, A, dr, c, ps, a1):
    qaT, kaT, va = a1["qaT"], a1["kaT"], a1["va"]
    mixedT = A.at(O_MIX, [128, 8, S], BF16)
    o = O_T
    yA = A.at(o, [128, NT, 512], F32); o += 32 * KB
    tmask = A.at(o, [128, 2944], BF16); o += 5888
    sqt = A.at(o, [128, S], BF16); o += 4 * KB
    E = [A.at(o + i * KB, [128, 512], BF16) for i in range(3)]; o += 3 * KB
    PT = [A.at(o + i * KB, [128, 512], BF16) for i in range(4)]; o += 4 * KB
    mx = A.at(o, [128, 16, 4], F32); o += 256
    mx2 = A.at(o, [128, 16], F32); o += 64
    negm = A.at(o, [128, 8], F32); o += 32
    rden = [A.at(o + i * 16, [128, 4], F32) for i in range(2)]; o += 32
    ssa = A.at(o, [128, 16], F32); o += 64
    rstdA = A.at(o, [128, 16], F32); o += 64
    halfones = A.at(o, [128, 2, 128], BF16); o += 512
    attn_g = A.at(o, [128, 4], F32); o += 32
    yn = A.at(o, [128, 4, 512], BF16); o += 4 * KB
    junk = A.at(o, [128, 512], BF16); o += KB
    kpad = [A.at(o + i * 4 * KB, [128, S], BF16) for i in range(2)]; o += 8 * KB
    assert o <= ARENA

    P.dma("sp", tmask, dr["tmask"], "c_tmask", writes=["tmask"])
    P.dma("sp", halfones, dr["halfones"], "c_halfones", writes=["halfones"])
    P.dma("sp", attn_g, dr["attn_g"], "c_attn_g", writes=["attn_g"])

    nb = 0
    for qk, src, nm in ((0, qaT, "qaT"), (1, kaT, "kaT")):
        for cc in range(4):
            P.act(sqt, src[:, cc, :], AF.Square, reads=[(nm, cc, n) for n in range(4)], writes=["sqt"])
            for a in range(2):
                for g in range(4):
                    b = nb % 4; nb += 1
                    P.mm(ps[b][:, :], halfones[:, a, :], sqt[:, g * 512:(g + 1) * 512], reads=["sqt", "halfones"], writes=["ps%d" % b])
                    P.op("dve", lambda e, b=b, col=qk * 8 + cc * 2 + a, g=g: e.reduce_max(out=mx[:, col, g:g + 1], in_=ps[b][:, :], axis=AX.X),
                         reads=["ps%d" % b], writes=["mx"])
    P.op("dve", lambda e: e.reduce_max(out=mx2, in_=mx, axis=AX.X), reads=["mx"], writes=["mx2"])
    P.tt("dve", negm, mx2[:, 0:8], mx2[:, 8:16], ALU.mult, reads=["mx2"], writes=["negm"])
    P.act(negm, negm, AF.Sqrt, reads=["negm"], writes=["negm"])
    P.ts("dve", negm, negm, -1.0, ALU.mult, reads=["negm"], writes=["negm"])

    SKEW = 2
    iters = []
    grp = 0
    for h in range(8):
        for G in range(4):
            Js = list(range(max(0, 4 * G - 8), min(15, 4 * G + 3 + 8) + 1))
            lastJ = [max(j for j in Js if abs(j - (4 * G + i)) <= 8) for i in range(4)]
            firstJ = [min(j for j in Js if abs(j - (4 * G + i)) <= 8) for i in range(4)]
            for J in Js:
                iters.append((h, G, J, firstJ, lastJ, J == Js[-1], grp))
            grp += 1
    n_it = len(iters)
    sbanks = [0, 1, 6]


    def make_kpad(h):
        cc, a = h // 2, h % 2
        P.ts("dve", kpad[h % 2], kaT[:, cc, :], halfones[:, a, 0:1], ALU.mult,
             reads=[("kaT", cc, n) for n in range(4)] + ["halfones"], writes=[("kpad", h % 2)])

    make_kpad(0)

    def front(it):
        h, G, J = iters[it][0:3]
        cc, a = h // 2, h % 2
        sb = sbanks[it % 3]
        e_i = it % 3
        p_i = it % 4
        if h + 1 < 8 and (it == 0 or iters[it - 1][0] != h):
            make_kpad(h + 1)
        P.mm(ps[sb][:, :], kpad[h % 2][:, J * 128:(J + 1) * 128], qaT[:, cc, G * 512:(G + 1) * 512],
             reads=[("kpad", h % 2), ("qaT", cc, G)], writes=["ps%d" % sb])
        P.act(E[e_i], ps[sb][:, :], AF.Exp, bias=negm[:, h:h + 1], reads=["ps%d" % sb, "negm"], writes=[("E", e_i)])
        u0 = 1408 - (J * 128 - G * 512)
        P.tt("dve", PT[p_i], E[e_i], tmask[:, u0:u0 + 512], ALU.mult, reads=[("E", e_i), "tmask"], writes=[("PT", p_i)])

    def back(it):
        h, G, J, firstJ, lastJ, is_last, g = iters[it]
        p_i = it % 4
        for i in range(4):
            I = 4 * G + i
            if abs(I - J) > 8:
                continue
            P.mm(ps[2 + i][:, 0:65], PT[p_i][:, i * 128:(i + 1) * 128], va[:, J, h, :], start=(J == firstJ[i]), stop=(J == lastJ[i]),
                 reads=[("PT", p_i), ("va", J), "va_ones"], writes=["ps%d" % (2 + i)])
        if is_last:
            rd = rden[g % 2]; rk = "rden%d" % (g % 2)
            for i in range(4):
                I = 4 * G + i
                P.recip(rd[:, i:i + 1], ps[2 + i][:, 64:65], reads=["ps%d" % (2 + i)], writes=[(rk, i)])
                P.ts("dve", yA[:, I, h * 64:(h + 1) * 64], ps[2 + i][:, 0:64], rd[:, i:i + 1], ALU.mult,
                     reads=["ps%d" % (2 + i), (rk, i)], writes=[("yA", I)])

    for it in range(n_it + SKEW):
        if it < n_it:
            front(it)
        if it - SKEW >= 0:
            back(it - SKEW)

    for T in range(NT):
        P.act(junk, yA[:, T, :], AF.Square, accum_out=ssa[:, T:T + 1], reads=[("yA", T)], writes=["junk", "ssa"])
    P.act(rstdA, ssa, AF.Sqrt, bias=EPS, scale=1.0 / 512, reads=["ssa"], writes=["rstdA"])
    P.recip(rstdA, rstdA, reads=["rstdA"], writes=["rstdA"])
    for Tg in range(4):
        for i in range(4):
            T = Tg * 4 + i
            P.ts("dve", yn[:, i, :], yA[:, T, :], rstdA[:, T:T + 1], ALU.mult, reads=[("yA", T), "rstdA"], writes=[("yn", i)])
        for cc in range(4):
            b = cc % 2
            pT = ps[b][:, :].bitcast(BF16)
            for i in range(4):
                P.tr(pT[:, i * 128:(i + 1) * 128], yn[:, i, cc * 128:(cc + 1) * 128], c["ident_b"], reads=[("yn", i), "ident_b"], writes=["ps%d" % b])
            P.act(mixedT[:, 4 + cc, Tg * 512:(Tg + 1) * 512], pT[:, 0:512], AF.Copy, scale=attn_g[:, cc:cc + 1],
                  reads=["ps%d" % b, "attn_g"], writes=[("mixedT", 4 + cc, Tg)])
    return dict(mixedT=mixedT, yA=yA)


def phase_A2(P, A, dr, c, ps, a1, mixedT):
    xmT, szT = a1["xmT"], a1["szT"]
    o = O_QA
    xcT = A.at(o, [128, 4, S], BF16); o += 16 * KB
    qT = A.at(o, [128, 4, S], BF16); o += 16 * KB
    kT = A.at(o, [128, 4, S], BF16); o += 16 * KB
    ktok = A.at(o, [128, NT, 4, 128], BF16); o += 16 * KB
    vtok = A.at(o, [128, NT, 4, 129], BF16); o += 16512
    hsum = A.at(o, [128, NT, 2, 128], F32); o += 16 * KB
    vTt = [A.at(o, [128, S], BF16) for i in range(2)]; o += 4 * KB
    acc = A.at(o, [128, S], F32); o += 8 * KB
    acc3 = acc.rearrange("p (t d) -> p t d", t=NT)
    wbd_f = acc[:, 0:1536].rearrange("p (a b) -> p a b", a=12)
    wbd = A.at(o, [128, 3, 4, 128], BF16); o += 3 * KB
    wif_f = acc[:, 1536:1728].rearrange("p (a b) -> p a b", a=12)
    wif = A.at(o, [128, 12, 16], BF16); o += 384
    bif = A.at(o, [128, 256], F32); o += KB
    gates = A.at(o, [128, NT, 16], F32); o += KB
    gflat = gates.rearrange("p t g -> p (t g)")
    small = {}
    for nm in ("lf", "ii", "bcs", "esc", "wk", "enb", "eg", "tg1", "tg2"):
        small[nm] = A.at(o, [128, 2, NT, 4], F32); o += 512
    tri_f = A.at(o, [128, 2, 128], F32); o += KB
    tri_b = A.at(o, [128, 2, 128], BF16); o += 512
    ones_f = A.at(o, [128, 128], F32); o += 512
    St32 = [A.at(o + i * 516, [128, 129], F32) for i in range(4)]; o += 4 * 516
    Stb = [[A.at(o + (i * 2 + p) * 260, [128, 129], BF16) for p in range(2)] for i in range(4)]; o += 8 * 260
    WT = [A.at(o + i * 256, [128, 128], BF16) for i in range(8)]; o += 2 * KB
    khat = [A.at(o + i * 256, [128, 128], BF16) for i in range(8)]; o += 2 * KB
    tmpS = [wbd.rearrange("p a b c -> p (a b c)")[:, i * 128:(i + 1) * 128] for i in range(8)]
    den2 = [A.at(o + i * 8, [128, 2], F32) for i in range(8)]; o += 64
    den = [A.at(o + i * 4, [128, 1], F32) for i in range(8)]; o += 32
    ssh = A.at(o, [128, NT], F32); o += 64
    rstdh = A.at(o, [128, NT], F32); o += 64
    hn = A.at(11 * KB, [128, 4, 128], BF16)
    tmpx = [A.at(3 * KB + i * 2 * KB, [128, 512], F32) for i in range(2)]
    tmpy = [A.at(7 * KB + i * 2 * KB, [128, 512], F32) for i in range(2)]
    convw = A.at(o, [128, 4, 5], F32); o += 96
    convb = A.at(o, [128, 4], F32); o += 32
    mng = A.at(o, [128, 4], F32); o += 32
    msk = A.at(o, [128, 4], F32); o += 32
    assert o <= ARENA, o

    for nm, ap in (("bif", bif), ("tri", tri_f), ("convw", convw), ("convb", convb), ("mng", mng), ("msk", msk)):
        P.dma("sp", ap, dr[nm], "c2_" + nm, writes=["c2_" + nm])
    P.dma("sp", wbd_f, dr["wbd"], "c2_wbd", writes=["acc"])
    P.dma("sp", wif_f, dr["wif"], "c2_wif", writes=["acc"])
    P.copy("dve", wbd.rearrange("p a b c -> p (a b) c"), wbd_f, reads=["acc"], writes=["wbd"])
    P.copy("dve", wif, wif_f, reads=["acc"], writes=["wif"])
    P.copy("dve", tri_b, tri_f, reads=["c2_tri"], writes=["tri_b"])
    P.memset("dve", ones_f, 1.0, writes=["ones_f"])
    P.memset("pool", vtok[:, :, :, 128:129], 1.0, writes=["vtok_ones"])

    inv_sqrt_dh = 128 ** -0.5
    nb = 0
    first_gate = True
    for hc in range(4):
        x = xmT[:, hc, :]
        xm_res = [("xmT", hc, n) for n in range(4)]
        P.ts("dve", acc, x, convw[:, hc, 2:3], ALU.mult, reads=xm_res + ["c2_convw"], writes=["acc"])
        for j in (0, 1, 3, 4):
            sh = j - 2
            if sh < 0:
                P.stt(acc[:, -sh:S], x[:, 0:S + sh], convw[:, hc, j:j + 1], acc[:, -sh:S], ALU.mult, ALU.add, reads=["acc"], writes=["acc"])
            else:
                P.stt(acc[:, 0:S - sh], x[:, sh:S], convw[:, hc, j:j + 1], acc[:, 0:S - sh], ALU.mult, ALU.add, reads=["acc"], writes=["acc"])
        P.act(xcT[:, hc, :], acc, AF.Silu, bias=convb[:, hc:hc + 1], reads=["acc", "c2_convb"], writes=[("xcT", hc)])
        vt = vTt[0]; vk = "vTt0"
        for g in range(4):
            tok = slice(g * 512, (g + 1) * 512)
            b = nb % 4; nb += 1
            P.mm(ps[b][:, :], wbd[:, 0, hc, :], xcT[:, hc, tok], reads=["wbd", ("xcT", hc)], writes=["ps%d" % b])
            P.copy("act", qT[:, hc, tok], ps[b][:, :], reads=["ps%d" % b], writes=[("qT", hc, g)])
            b = nb % 4; nb += 1
            P.mm(ps[b][:, :], wbd[:, 1, hc, :], xcT[:, hc, tok], reads=["wbd", ("xcT", hc)], writes=["ps%d" % b])
            P.ts("dve", kT[:, hc, tok], ps[b][:, :], inv_sqrt_dh, ALU.mult, reads=["ps%d" % b], writes=[("kT", hc, g)])
            b = nb % 4; nb += 1
            P.mm(ps[b][:, :], wbd[:, 2, hc, :], x[:, tok], reads=["wbd"] + xm_res, writes=["ps%d" % b])
            P.copy("act", vt[:, tok], ps[b][:, :], reads=["ps%d" % b], writes=[(vk, g)])
        for g in range(4):
            b = nb % 4; nb += 1
            for i in range(4):
                T = 4 * g + i
                P.mm(ps[b][:, i * 128:(i + 1) * 128], xcT[:, hc, T * 128:(T + 1) * 128], wbd[:, 1, hc, :], reads=["wbd", ("xcT", hc)], writes=["ps%d" % b])
            P.ts("dve", ktok[:, 4 * g:4 * g + 4, hc, :], ps[b][:, :].rearrange("p (i d) -> p i d", i=4), inv_sqrt_dh, ALU.mult,
                 reads=["ps%d" % b], writes=[("ktok", hc, g)])
            b = nb % 4; nb += 1
            for i in range(4):
                T = 4 * g + i
                P.mm(ps[b][:, i * 128:(i + 1) * 128], x[:, T * 128:(T + 1) * 128], wbd[:, 2, hc, :], reads=["wbd"] + xm_res, writes=["ps%d" % b])
            P.copy("act", vtok[:, 4 * g:4 * g + 4, hc, 0:128], ps[b][:, :].rearrange("p (i d) -> p i d", i=4),
                   reads=["ps%d" % b], writes=[("vtok", hc, g)])
        for ch, src, rk in ((hc, qT[:, hc, :], [("qT", hc, g) for g in range(4)]),
                            (4 + hc, kT[:, hc, :], [("kT", hc, g) for g in range(4)]),
                            (8 + hc, vt, [(vk, g) for g in range(4)])):
            for T in range(NT):
                P.mm(ps[4][:, T * 16:(T + 1) * 16], src[:, T * 128:(T + 1) * 128], wif[:, ch, :], reads=rk + ["wif"], writes=["ps4"])
            if first_gate:
                P.tt("dve", gflat, ps[4][:, 0:256], bif, ALU.add, reads=["ps4", "c2_bif"], writes=["gates"])
                first_gate = False
            else:
                P.tt("dve", gflat, ps[4][:, 0:256], gflat, ALU.add, reads=["ps4", "gates"], writes=["gates"])

    sm = small
    fl = lambda ap: ap.rearrange("p d t h -> p (d t h)")
    for d in range(2):
        i_pre = gates[:, :, d * 8:d * 8 + 4]
        f_pre = gates[:, :, d * 8 + 4:d * 8 + 8]
        P.act(sm["tg1"][:, d], f_pre, AF.Exp, scale=-1.0, reads=["gates"], writes=[("tg1", d)])
        P.act(sm["tg2"][:, d], sm["tg1"][:, d], AF.Ln, bias=1.0, reads=[("tg1", d)], writes=[("tg2", d)])
        P.ts("dve", sm["lf"][:, d], sm["tg2"][:, d], -1.0, ALU.mult, reads=[("tg2", d)], writes=[("lf", d)])
        P.copy("dve", sm["ii"][:, d], i_pre, reads=["gates"], writes=[("ii", d)])
    lf2 = [sm["lf"][:, d].rearrange("p t h -> p (t h)") for d in range(2)]
    P.mm(ps[5][:, 0:64], tri_f[:, 0, :], lf2[0], reads=[("lf", 0), "c2_tri"], writes=["ps5"])
    P.mm(ps[5][:, 64:128], tri_f[:, 1, :], lf2[1], reads=[("lf", 1), "c2_tri"], writes=["ps5"])
    P.mm(ps[6][:, 0:128], ones_f, fl(sm["lf"]), reads=[("lf", 0), ("lf", 1), "ones_f"], writes=["ps6"])
    P.copy("dve", fl(sm["bcs"]), ps[5][:, 0:128], reads=["ps5"], writes=["bcs"])
    P.tt("dve", fl(sm["tg1"]), fl(sm["ii"]), fl(sm["bcs"]), ALU.subtract, reads=[("ii", 0), ("ii", 1), "bcs", ("tg1", 0), ("tg1", 1)],
         writes=[("tg1", 0), ("tg1", 1)])
    P.act(fl(sm["esc"]), fl(sm["tg1"]), AF.Exp, reads=[("tg1", 0), ("tg1", 1)], writes=["esc"])
    P.act(fl(sm["enb"]), fl(sm["bcs"]), AF.Exp, scale=-1.0, reads=["bcs"], writes=["enb"])
    P.act(fl(sm["eg"]), ps[6][:, 0:128], AF.Exp, reads=["ps6"], writes=["eg"])
    P.tt("dve", fl(sm["wk"]), fl(sm["esc"]), fl(sm["eg"]), ALU.mult, reads=["esc", "eg"], writes=["wk"])

    cnt = {"w": 0, "k": 0, "d": 0}
    for pair in range(2):
        hp = (2 * pair, 2 * pair + 1)
        streams = [(d, hl) for d in range(2) for hl in range(2)]
        for si in range(4):
            P.memset("pool", St32[si], 0.0, writes=[("St32", si)])
            P.memset("pool", Stb[si][1], 0.0, writes=[("Stb", si, 1)])
        for step in range(NT):
            inf = []
            for k, (d, hl) in enumerate(streams):
                T = step if d == 0 else NT - 1 - step
                w = cnt["w"] % 8; cnt["w"] += 1
                kk = cnt["k"] % 8; cnt["k"] += 1
                inf.append(dict(d=d, hl=hl, h=hp[hl], si=d * 2 + hl, T=T, Tsl=slice(T * 128, (T + 1) * 128), g4=T // 4, w=w, kk=kk, k=k))
            bS = step % 2
            for q in inf:
                d, h, T, Tsl, g4, w, k = q["d"], q["h"], q["T"], q["Tsl"], q["g4"], q["w"], q["k"]
                rS = "ps%d" % bS
                P.mm(ps[bS][:, k * 128:(k + 1) * 128], kT[:, h, Tsl], qT[:, h, Tsl], reads=[("kT", h, g4), ("qT", h, g4)], writes=[rS])
                P.act(tmpS[w], ps[bS][:, k * 128:(k + 1) * 128], AF.Copy, scale=sm["esc"][:, d, T, h:h + 1], reads=[rS, "esc"], writes=[("tmpS", w)])
                P.tt("pool", WT[w], tmpS[w], tri_b[:, d, :], ALU.mult, reads=[("tmpS", w), "tri_b"], writes=[("WT", w)])
                if step < NT - 1:
                    P.act(khat[q["kk"]], ktok[:, T, h, :], AF.Copy, scale=sm["wk"][:, d, T, h:h + 1], reads=[("ktok", h, g4), "wk"],
                          writes=[("khat", q["kk"])])
            if step < NT - 1:
                for q in inf:
                    d, h, si, T, g4, k, kk = q["d"], q["h"], q["si"], q["T"], q["g4"], q["k"], q["kk"]
                    bC = 6 + k // 2
                    co = (k % 2) * 256
                    rC = "ps%d" % bC
                    P.mm(ps[bC][:, co:co + 129], khat[kk], vtok[:, T, h, :], reads=[("khat", kk), ("vtok", h, g4), "vtok_ones"], writes=[rC])
                    P.stt(St32[si], St32[si], sm["eg"][:, d, T, h:h + 1], ps[bC][:, co:co + 129], ALU.mult, ALU.add,
                          reads=[("St32", si), "eg", rC], writes=[("St32", si)])
                    P.copy("dve", Stb[si][step % 2], St32[si], reads=[("St32", si)], writes=[("Stb", si, step % 2)])
            pendO = []
            for q in inf:
                d, hl, h, si, T, Tsl, g4, w, k = q["d"], q["hl"], q["h"], q["si"], q["T"], q["Tsl"], q["g4"], q["w"], q["k"]
                bO = 2 + (step % 2) * 2 + k // 2
                co = (k % 2) * 256
                rO = "ps%d" % bO
                P.mm(ps[bO][:, co:co + 129], WT[w], vtok[:, T, h, :], start=True, stop=False,
                     reads=[("WT", w), ("vtok", h, g4), "vtok_ones"], writes=[rO])
                P.mm(ps[bO][:, co:co + 129], qT[:, h, Tsl], Stb[si][(step + 1) % 2], start=False, stop=True,
                     reads=[("qT", h, g4), ("Stb", si, (step + 1) % 2)], writes=[rO])
                if k % 2 == 1:
                    dn = den2[cnt["d"] % 8]; dk = ("den2", cnt["d"] % 8); cnt["d"] += 1
                    h0 = hp[0]
                    P.stt(dn, ps[bO][:, 128::256], -1.0, sm["enb"][:, d, T, h0:h0 + 2], ALU.mult, ALU.max, reads=[rO, "enb"], writes=[dk])
                    P.tt("dve", dn, dn, ps[bO][:, 128::256], ALU.max, reads=[rO, dk], writes=[dk])
                    P.recip(dn, dn, reads=[dk], writes=[dk])
                    pendO.append((dn, dk))
                continue
            for q in inf:
                d, hl, h, si, T, Tsl, g4, w, k = q["d"], q["hl"], q["h"], q["si"], q["T"], q["Tsl"], q["g4"], q["w"], q["k"]
                bO = 2 + (step % 2) * 2 + k // 2
                co = (k % 2) * 256
                rO = "ps%d" % bO
                dnp, dk = pendO[k // 2]
                dn = dnp[:, k % 2:k % 2 + 1]
                is_first = (d == 0) == (T < 8)
                if is_first:
                    P.act(hsum[:, T, hl, :], ps[bO][:, co:co + 128], AF.Copy, scale=dn, reads=[rO, dk], writes=[("hsum", T, hl)])
                else:
                    P.stt(hsum[:, T, hl, :], ps[bO][:, co:co + 128], dn, hsum[:, T, hl, :], ALU.mult, ALU.add,
                          reads=[rO, dk, ("hsum", T, hl)], writes=[("hsum", T, hl)])
        for hl in range(2):
            h = hp[hl]
            P.act(acc3, hsum[:, :, hl, :], AF.Square, reads=[("hsum", T, hl) for T in range(NT)], writes=["acc"])
            P.op("dve", lambda e: e.reduce_sum(out=ssh, in_=acc3, axis=AX.X), reads=["acc"], writes=["ssh"])
            P.act(rstdh, ssh, AF.Sqrt, bias=EPS, scale=1.0 / 128, reads=["ssh"], writes=["rstdh"])
            P.recip(rstdh, rstdh, reads=["rstdh"], writes=["rstdh"])
            for Tg in range(4):
                tok = slice(Tg * 512, (Tg + 1) * 512)
                b = 6 + Tg % 2
                pT = ps[b][:, :].bitcast(BF16)
                for i in range(4):
                    T = Tg * 4 + i
                    P.act(hn[:, i, :], hsum[:, T, hl, :], AF.Copy, scale=rstdh[:, T:T + 1], reads=[("hsum", T, hl), "rstdh"], writes=[("hn", i)])
                    P.tr(pT[:, i * 128:(i + 1) * 128], hn[:, i, :], c["ident_b"], reads=[("hn", i), "ident_b"], writes=["ps%d" % b])
                tx = tmpx[Tg % 2]; txk = "tmpx%d" % (Tg % 2)
                ty = tmpy[Tg % 2]; tyk = "tmpy%d" % (Tg % 2)
                P.act(tx, xcT[:, h, tok], AF.Copy, scale=msk[:, h:h + 1], reads=[("xcT", h), "c2_msk"], writes=[txk])
                P.stt(ty, pT[:, 0:512], mng[:, h:h + 1], tx, ALU.mult, ALU.add, reads=["ps%d" % b, "c2_mng", txk], writes=[tyk])
                P.tt("pool", mixedT[:, h, tok], ty, szT[:, h, tok], ALU.mult, reads=[tyk, ("szT", h, Tg)], writes=[("mixedT", h, Tg)])


O_X1 = 140 * KB


def phase_A4(P, A, dr, c, ps, mixedT):
    x1 = A.at(O_X1, [128, NT, D], F32)
    Woutb = A.at(O_QA, [128, 8, D], BF16)
    for kc in range(8):
        P.dma("pool", Woutb[:, kc, :], dr["w_out"][kc * 128:(kc + 1) * 128, :], "wout", writes=[("Woutb", kc)])
    xv = dr["x"].rearrange("(t p) d -> p t d", p=128)
    for T in range(NT):
        P.dma("sp", x1[:, T, :], xv[:, T, :], "x1ld%d" % (T % 4), writes=[("x1", T)])
    nb = 0
    for T in range(NT):
        for nh in range(2):
            b = nb % 4; nb += 1
            for kc in range(8):
                P.mm(ps[b][:, :], mixedT[:, kc, T * 128:(T + 1) * 128], Woutb[:, kc, nh * 512:(nh + 1) * 512], start=(kc == 0), stop=(kc == 7),
                     reads=[("mixedT", kc, T // 4), ("Woutb", kc)], writes=["ps%d" % b])
            P.tt("dve", x1[:, T, nh * 512:(nh + 1) * 512], ps[b][:, :], x1[:, T, nh * 512:(nh + 1) * 512], ALU.add,
                 reads=["ps%d" % b, ("x1", T)], writes=[("x1", T)])
    return x1


def host_inputs(inp, b):
    import ml_dtypes
    bf = ml_dtypes.bfloat16
    f32 = np.float32
    cosT, sinT, PT = rope_tables()
    d = {}
    xb = np.asarray(inp["x"][b], f32)
    d["x"] = np.ascontiguousarray(xb)
    d["xT"] = np.ascontiguousarray(xb.T)
    d["w_in"] = np.ascontiguousarray(inp["w_in"][0], dtype=f32)
    col = lambda v, n: np.ascontiguousarray(np.asarray(v, f32).reshape(n, 128).T)
    d["g1"] = col(inp["norm1_g"][0], 8)
    d["ropeC"] = cosT.astype(bf)
    d["ropeS"] = sinT.astype(bf)
    d["ropePT"] = PT.astype(bf)
    d["ident_f"] = np.eye(128, dtype=f32)
    d["ident_b"] = np.eye(128).astype(bf)
    d["tmask"] = toeplitz_mask().astype(bf)
    ho = np.zeros((128, 2, 128), f32)
    ho[:64, 0, :] = 1.0
    ho[64:, 1, :] = 1.0
    d["halfones"] = ho.astype(bf)
    d["attn_g"] = col(inp["attn_norm_g"][0], 4)
    wbd = np.zeros((128, 12, 128), f32)
    for m, key in enumerate(("wq_m", "wk_m", "wv_m")):
        w = np.asarray(inp[key][0], f32)
        for hc in range(4):
            for gl in range(32):
                wbd[gl * 4:gl * 4 + 4, m * 4 + hc, gl * 4:gl * 4 + 4] = w[hc * 32 + gl]
    d["wbd"] = wbd
    wif = np.concatenate([np.asarray(inp["w_if_fwd"][0], f32), np.asarray(inp["w_if_bwd"][0], f32)], axis=1)
    d["wif"] = np.ascontiguousarray(wif.reshape(12, 128, 16).transpose(1, 0, 2))
    brow = np.concatenate([np.asarray(inp["b_if_fwd"][0], f32), np.asarray(inp["b_if_bwd"][0], f32)])
    d["bif"] = np.ascontiguousarray(np.broadcast_to(np.tile(brow, 16)[None, :], (128, 256)))
    tri = np.zeros((128, 2, 128), f32)
    ss, tt = np.meshgrid(np.arange(128), np.arange(128), indexing="ij")
    tri[:, 0, :] = (ss <= tt)
    tri[:, 1, :] = (ss >= tt)
    d["tri"] = tri
    cw = np.asarray(inp["conv_w"][0], f32)
    d["convw"] = np.ascontiguousarray(cw.T.reshape(4, 128, 5).transpose(1, 0, 2))
    d["convb"] = col(inp["conv_b"][0], 4)
    d["mng"] = col(inp["mlstm_norm_g"][0], 4)
    d["msk"] = col(inp["mlstm_skip"][0], 4)
    d["w_out"] = np.ascontiguousarray(inp["w_out"][0], dtype=f32)
    d["g2bc"] = np.ascontiguousarray(np.broadcast_to(np.asarray(inp["norm2_g"][0], f32)[None, :], (128, D)))
    d["gfbc"] = np.ascontiguousarray(np.broadcast_to(np.asarray(inp["norm_f_g"], f32)[None, :], (128, D)))
    d["wr"] = np.ascontiguousarray(np.asarray(inp["w_router"][0], f32).reshape(8, 128, NE).transpose(1, 0, 2))
    es = np.zeros((16, NE * 128), f32)
    for e in range(NE):
        es[e, e * 128:(e + 1) * 128] = 1.0
    d["esel"] = es.astype(bf)
    d["iota_row"] = np.ascontiguousarray(np.broadcast_to(np.arange(256, dtype=f32)[None, :], (128, 256)))
    d["iota_p"] = np.stack([np.arange(128, dtype=f32), np.arange(128, dtype=f32) + 128], axis=1)
    if "w1" in inp:
        d["w1"] = np.ascontiguousarray(inp["w1"][0], dtype=f32)
        d["w3"] = np.ascontiguousarray(inp["w3"][0], dtype=f32)
        d["w2"] = np.ascontiguousarray(inp["w2"][0], dtype=f32)
    return d


IN_SPECS = {
    "x": ([S, D], F32), "xT": ([D, S], F32), "w_in": ([D, DIN], F32), "g1": ([128, 8], F32),
    "ropeC": ([128, S], BF16), "ropeS": ([128, S], BF16), "ropePT": ([128, 128], BF16),
    "ident_f": ([128, 128], F32), "ident_b": ([128, 128], BF16),
    "tmask": ([128, 2944], BF16), "halfones": ([128, 2, 128], BF16), "attn_g": ([128, 4], F32),
    "wbd": ([128, 12, 128], F32), "wif": ([128, 12, 16], F32), "bif": ([128, 256], F32), "tri": ([128, 2, 128], F32),
    "convw": ([128, 4, 5], F32), "convb": ([128, 4], F32), "mng": ([128, 4], F32), "msk": ([128, 4], F32),
    "w_out": ([D, D], F32),
    "g2bc": ([128, D], F32), "gfbc": ([128, D], F32), "wr": ([128, 8, NE], F32), "esel": ([16, NE * 128], BF16),
    "iota_row": ([128, 256], F32), "iota_p": ([128, 2], F32),
    "w1": ([NE, D, DFF], F32), "w3": ([NE, D, DFF], F32), "w2": ([NE, DFF, D], F32),
}


def build(debug=None):
    nc = bass.Bass("TRN2", target_bir_lowering=False)
    early = debug is not None and debug[0] in "AB"
    dr = {k: nc.dram_tensor(k, sh, dt, kind="ExternalInput").ap() for k, (sh, dt) in IN_SPECS.items()
          if not (early and k in ("w1", "w2", "w3"))}
    out = nc.dram_tensor("out", [S, D], F32, kind="ExternalOutput").ap()
    A = Arena(nc, ARENA)
    ps = [nc.alloc_psum_tensor("ps%d" % i, [128, 512], F32) for i in range(8)]
    P = Prog(nc)
    finals = []

    def dump(name, ap, shape, dt):
        t = nc.dram_tensor("dbg_" + name, shape, dt, kind="ExternalOutput").ap()
        P.barrier()
        if len(shape) >= 3 and shape[1] > 4:
            for i in range(shape[1]):
                P.dma("sp", t[:, i], ap[:, i])
        else:
            P.dma("sp", t, ap)

    c = build_consts(P, A, dr)
    a1 = phase_A1(P, A, dr, c, ps)
    if debug == "A1":
        for k in ("xmT", "szT", "qaT", "kaT"):
            dump(k, a1[k], [128, 4, S], BF16)
        dump("va", a1["va"], [128, NT, 8, 65], BF16)
        P.emit(finals)
        return nc
    P.barrier()
    a3 = phase_A3(P, A, dr, c, ps, a1)
    if debug == "A3":
        dump("yA", a3["yA"], [128, NT, 512], F32)
        P.memset("dve", a3["mixedT"][:, 0:4, :], 0.0, writes=[("mixedT", i, j) for i in range(4) for j in range(4)])
        dump("mixedT", a3["mixedT"], [128, 8, S], BF16)
        P.emit(finals)
        return nc
    P.barrier()
    phase_A2(P, A, dr, c, ps, a1, a3["mixedT"])
    if debug == "A2":
        dump("mixedT", a3["mixedT"], [128, 8, S], BF16)
        P.emit(finals)
        return nc
    P.barrier()
    x1 = phase_A4(P, A, dr, c, ps, a3["mixedT"])
    if debug == "A4":
        dump("mixedT", a3["mixedT"], [128, 8, S], BF16)
        dump("x1", x1, [128, NT, D], F32)
        P.emit(finals)
        return nc
    P.barrier()
    rb = phase_B(P, A, dr, c, ps, x1)
    if debug == "B":
        dump("aff", rb["aff"], [128, NT, NE], F32)
        dump("posm_tok", rb["posm_tok"], [128, NT, NE], F32)
        dump("h2b", rb["h2b"], [128, NT, D], BF16)
        P.emit(finals)
        return nc
    P.barrier()
    nexp = NE if debug is None else int(debug[1:]) if debug.startswith("C") else NE
    if debug is not None and debug.startswith("C"):
        dump("x1pre", x1, [128, NT, D], F32)
        P.barrier()
    phase_C(P, A, dr, c, ps, x1, rb, experts=range(nexp))
    if debug is not None and debug.startswith("C"):
        dump("x2", x1, [128, NT, D], F32)
        dump("aff", rb["aff"], [128, NT, NE], F32)
        dump("posm_tok", rb["posm_tok"], [128, NT, NE], F32)
        P.emit(finals)
        return nc
    P.barrier()
    phase_D(P, A, dr, c, ps, x1, out)
    P.emit(finals)
    return nc


def run(inp, cores=(0,), debug=None):
    nc = build(debug)
    in_maps = [host_inputs(inp, b) for b in cores]
    names = [a.memorylocations[0].name for a in nc.m.functions[0].allocations
             if isinstance(a, mybir.MemoryLocationSet) and a.kind == "ExternalInput"]
    in_maps = [{k: v for k, v in m.items() if k in names} for m in in_maps]
    res = run_bass_kernel_spmd(nc, in_maps, core_ids=list(range(len(cores))))
    return res.results


O_H2 = 12 * KB
O_BC = 44 * KB
O_PER = 132 * KB


def phase_B(P, A, dr, c, ps, x1):
    h2b = A.at(O_H2, [128, NT, D], BF16)
    o = O_PER
    aff = A.at(o, [128, NT, NE], F32); o += KB
    posm_tok = A.at(o, [128, NT, NE], F32); o += KB
    posmT_b = A.at(o, [16, S], BF16); o += 4 * KB
    assert o <= O_X1
    esel = A.at(3 * KB, [16, NE * 128], BF16)
    iota_row = A.at(7 * KB, [128, 256], F32)
    iota_p = A.at(8 * KB, [128, 2], F32)
    o = O_BC
    g2bc = A.at(o, [128, D], F32); o += 4 * KB
    h2f = [A.at(o + i * 4 * KB, [128, D], F32) for i in range(2)]; o += 8 * KB
    h2fT = A.at(o, [128, 8, 128], F32); o += 4 * KB
    junk = A.at(o, [128, D], BF16); o += 2 * KB
    wr = A.at(o, [128, 8, NE], F32); o += 512
    ss2 = A.at(o, [128, NT], F32); o += 64
    rstd2 = A.at(o, [128, NT], F32); o += 64
    mxs = A.at(o, [128, 4], F32); o += 16
    sms = A.at(o, [128, 4], F32); o += 16
    ex = [A.at(o + i * 64, [128, NE], F32) for i in range(2)]; o += 128
    affT = A.at(o, [16, S], F32); o += 8 * KB
    work = A.at(o, [16, S], F32); o += 8 * KB
    ones16 = A.at(o, [16, S], F32); o += 8 * KB
    cs = A.at(o, [16, S], F32); o += 8 * KB
    m8 = A.at(o, [16, 8], F32); o += 32
    assert o <= O_PER

    P.dma("sp", g2bc, dr["g2bc"], writes=["g2bc"])
    P.dma("sp", wr, dr["wr"], writes=["wr"])
    P.dma("sp", esel, dr["esel"], writes=["esel"])
    P.dma("sp", iota_row, dr["iota_row"], writes=["iota_row"])
    P.dma("sp", iota_p, dr["iota_p"], writes=["iota_p"])
    P.memset("pool", ones16, 1.0, writes=["ones16"])

    for T in range(NT):
        P.act(junk, x1[:, T, :], AF.Square, accum_out=ss2[:, T:T + 1], reads=[("x1", T)], writes=["junkB", "ss2"])
    P.act(rstd2, ss2, AF.Sqrt, bias=EPS, scale=1.0 / D, reads=["ss2"], writes=["rstd2"])
    P.recip(rstd2, rstd2, reads=["rstd2"], writes=["rstd2"])
    for T in range(NT):
        i2 = T % 2
        hf = h2f[i2]; hk = "h2f%d" % i2
        P.stt(hf, x1[:, T, :], rstd2[:, T:T + 1], g2bc, ALU.mult, ALU.mult, reads=[("x1", T), "rstd2", "g2bc"], writes=[hk])
        P.copy("act", h2b[:, T, :], hf, reads=[hk], writes=[("h2b", T)])
        for half in range(2):
            b = half
            for j in range(4):
                kc = half * 4 + j
                P.tr(ps[b][:, j * 128:(j + 1) * 128], hf[:, kc * 128:(kc + 1) * 128], c["ident_f"], reads=[hk, "ident_f"], writes=["ps%d" % b])
            P.copy("dve" if half == 0 else "act", h2fT[:, half * 4:half * 4 + 4, :], ps[b][:, :].rearrange("p (a b) -> p a b", a=4),
                   reads=["ps%d" % b], writes=[("h2fT", half)])
        for kc in range(8):
            P.mm(ps[2][:, 0:NE], h2fT[:, kc, :], wr[:, kc, :], start=(kc == 0), stop=(kc == 7),
                 reads=[("h2fT", kc // 4), "wr"], writes=["ps2"])
        m = T % 4
        P.op("dve", lambda e, m=m: e.reduce_max(out=mxs[:, m:m + 1], in_=ps[2][:, 0:NE], axis=AX.X), reads=["ps2"], writes=[("mxs", m)])
        P.ts("dve", mxs[:, m:m + 1], mxs[:, m:m + 1], -1.0, ALU.mult, reads=[("mxs", m)], writes=[("mxs", m)])
        P.act(ex[i2], ps[2][:, 0:NE], AF.Exp, bias=mxs[:, m:m + 1], accum_out=sms[:, m:m + 1], reads=["ps2", ("mxs", m)], writes=[("ex", i2), ("sms", m)])
        P.recip(sms[:, m:m + 1], sms[:, m:m + 1], reads=[("sms", m)], writes=[("sms", m)])
        P.ts("dve", aff[:, T, :], ex[i2], sms[:, m:m + 1], ALU.mult, reads=[("ex", i2), ("sms", m)], writes=[("aff", T)])
    for Tg in range(4):
        b = 4 + Tg % 2
        for i in range(4):
            T = Tg * 4 + i
            P.tr(ps[b][0:NE, i * 128:(i + 1) * 128], aff[:, T, :], c["ident_f"], reads=[("aff", T), "ident_f"], writes=["ps%d" % b])
        P.copy("dve", affT[:, Tg * 512:(Tg + 1) * 512], ps[b][0:NE, :], reads=["ps%d" % b], writes=["affT"])
    P.copy("dve", work, affT, reads=["affT"], writes=["work"])
    for r in range(CAP // 8):
        P.op("dve", lambda e: e.max(out=m8, in_=work), reads=["work"], writes=["m8"])
        if r < CAP // 8 - 1:
            P.op("dve", lambda e: e.match_replace(out=work, in_to_replace=m8, in_values=work, imm_value=-1.0), reads=["m8", "work"], writes=["work"])
    P.ts("dve", work, affT, m8[:, 7:8], ALU.is_ge, reads=["affT", "m8"], writes=["work"])
    P.op("dve", lambda e: e.tensor_tensor_scan(out=cs, data0=ones16, data1=work, initial=0.0, op0=ALU.mult, op1=ALU.add),
         reads=["ones16", "work"], writes=["cs"])
    P.tt("dve", cs, cs, work, ALU.mult, reads=["cs", "work"], writes=["cs"])
    P.ts("dve", posmT_b, cs, -1.0, ALU.add, reads=["cs"], writes=["posmT_b"])
    for Tg in range(4):
        b = 6 + Tg % 2
        pT = ps[b][:, :].bitcast(BF16)
        for i in range(4):
            T = Tg * 4 + i
            P.tr(pT[:, i * NE:(i + 1) * NE], posmT_b[:, T * 128:(T + 1) * 128], c["ident_b"][0:NE, 0:NE], reads=["posmT_b", "ident_b"], writes=["ps%d" % b])
        P.copy("dve", posm_tok[:, Tg * 4:Tg * 4 + 4, :], pT[:, 0:4 * NE].rearrange("p (a b) -> p a b", a=4), reads=["ps%d" % b], writes=["posm_tok"])
    return dict(h2b=h2b, aff=aff, posm_tok=posm_tok, posmT_b=posmT_b, esel=esel, iota_row=iota_row, iota_p=iota_p)


NFG = 11
NSLOT = 4


def phase_C(P, A, dr, c, ps, x1, rb, experts=range(NE)):
    h2b, aff, posm_tok, posmT_b = rb["h2b"], rb["aff"], rb["posm_tok"], rb["posmT_b"]
    esel, iota_row, iota_p = rb["esel"], rb["iota_row"], rb["iota_p"]
    experts = list(experts)
    o = O_BC
    Wslot = []
    for s in range(NSLOT):
        Wslot.append((A.at(o, [128, 8, 256], BF16), A.at(o + 4 * KB, [128, 8, 256], BF16), A.at(o + 8 * KB, [128, 2, D], BF16)))
        o += 12 * KB
    XeT = [A.at(o + i * 4 * KB, [128, 8, CAP], BF16) for i in range(2)]; o += 8 * KB
    Sel = A.at(o, [128, NT, CAP], BF16); o += 8 * KB
    SelT = [A.at(o + i * 8 * KB, [128, 2, S], BF16) for i in range(2)]; o += 16 * KB
    H = [A.at(o + i * 512, [128, CAP], BF16) for i in range(4)]; o += 2 * KB
    su = [A.at(o + i * 512, [128, CAP], BF16) for i in range(2)]; o += KB
    Yb = A.at(o, [128, 2, D], BF16); o += 4 * KB
    assert o <= O_PER, o

    w1v = dr["w1"].rearrange("e (kc p) f -> e p kc f", p=128)
    w3v = dr["w3"].rearrange("e (kc p) f -> e p kc f", p=128)
    w2v = dr["w2"].rearrange("e (fc p) d -> e p fc d", p=128)

    groups = [(e, fg) for e in experts for fg in range(NFG)]
    PREF = NSLOT - 1

    def load(gi):
        e, fg = groups[gi]
        s = gi % NSLOT
        W1g, W3g, W2g = Wslot[s]
        P.dma("pool", W1g, w1v[e, :, :, fg * 256:(fg + 1) * 256], writes=[("W1", s)])
        P.dma("pool", W3g, w3v[e, :, :, fg * 256:(fg + 1) * 256], writes=[("W3", s)])
        P.dma("pool", W2g, w2v[e, :, fg * 2:fg * 2 + 2, :], writes=[("W2", s)])

    for gi in range(min(PREF, len(groups))):
        load(gi)

    side_bank = [0]

    def sbank():
        b = 2 + side_bank[0] % 2
        side_bank[0] += 1
        return b

    def gen_sel(e):
        for T in range(NT):
            P.ts("dve", Sel[:, T, :], iota_row, posm_tok[:, T, e:e + 1], ALU.is_equal, reads=["iota_row", "posm_tok"], writes=[("Sel", T)])

    def gather_unit(e, xb, kp):
        b = sbank()
        for j in range(2):
            kc = kp * 2 + j
            for T in range(NT):
                P.mm(ps[b][:, j * 256:(j + 1) * 256], h2b[:, T, kc * 128:(kc + 1) * 128], Sel[:, T, :], start=(T == 0), stop=(T == NT - 1),
                     reads=[("h2b", T), ("Sel", T)], writes=["ps%d" % b])
        P.copy("act", XeT[xb][:, kp * 2:kp * 2 + 2, :], ps[b][:, :].rearrange("p (a b) -> p a b", a=2),
               reads=["ps%d" % b], writes=[("XeT", xb, kp)])

    def selT_unit(e, sbuf, tg):
        b = sbank()
        P.mm(ps[b][:, :], esel[:, e * 128:(e + 1) * 128], posmT_b[:, tg * 512:(tg + 1) * 512], reads=["esel", "posmT_b"], writes=["ps%d" % b])
        for sc in range(2):
            P.ts("dve", SelT[sbuf][:, sc, tg * 512:(tg + 1) * 512], ps[b][:, :], iota_p[:, sc:sc + 1], ALU.is_equal,
                 reads=["ps%d" % b, "iota_p"], writes=[("SelT", sbuf, tg)])

    def scatter_unit(e, sbuf, T, dh):
        b = sbank()
        for sc in range(2):
            P.mm(ps[b][:, :], SelT[sbuf][:, sc, T * 128:(T + 1) * 128], Yb[:, sc, dh * 512:(dh + 1) * 512], start=(sc == 0), stop=(sc == 1),
                 reads=[("SelT", sbuf, T // 4), ("Yb", sc * 2 + dh)], writes=["ps%d" % b])
        P.stt(x1[:, T, dh * 512:(dh + 1) * 512], ps[b][:, :], aff[:, T, e:e + 1], x1[:, T, dh * 512:(dh + 1) * 512], ALU.mult, ALU.add,
              reads=["ps%d" % b, ("aff", T), ("x1", T)], writes=[("x1", T)])

    gen_sel(experts[0])
    for kp in range(4):
        gather_unit(experts[0], 0, kp)

    gi = 0
    for ei, e in enumerate(experts):
        xb = ei % 2
        sbuf = ei % 2
        e_next = experts[ei + 1] if ei + 1 < len(experts) else None
        e_prev = experts[ei - 1] if ei > 0 else None
        side = {}
        if e_prev is not None:
            units = [(T, dh) for T in range(NT) for dh in range(2)]
            for i, (T, dh) in enumerate(units):
                side.setdefault(i // 2, []).append(lambda T=T, dh=dh: scatter_unit(e_prev, 1 - sbuf, T, dh))
        for tg in range(4):
            side.setdefault(16 + tg, []).append(lambda tg=tg: selT_unit(e, sbuf, tg))
        if e_next is not None:
            side.setdefault(0, []).insert(0, lambda: gen_sel(e_next))
            for kp in range(4):
                side.setdefault(5 + 4 * kp, []).append(lambda kp=kp: gather_unit(e_next, 1 - xb, kp))
        pend = None
        for fg in range(NFG):
            s = gi % NSLOT
            W1g, W3g, W2g = Wslot[s]
            for fc in range(2):
                fci = fg * 2 + fc
                b = fci % 2
                for kc in range(8):
                    P.mm(ps[b][:, 0:256], W1g[:, kc, fc * 128:(fc + 1) * 128], XeT[xb][:, kc, :], start=(kc == 0), stop=(kc == 7),
                         reads=[("W1", s), ("XeT", xb, kc // 2)], writes=["ps%d" % b])
                for kc in range(8):
                    P.mm(ps[b][:, 256:512], W3g[:, kc, fc * 128:(fc + 1) * 128], XeT[xb][:, kc, :], start=(kc == 0), stop=(kc == 7),
                         reads=[("W3", s), ("XeT", xb, kc // 2)], writes=["ps%d" % b])
                hi = fci % 4
                P.act(su[fci % 2], ps[b][:, 0:256], AF.Silu, reads=["ps%d" % b], writes=[("su", fci % 2)])
                P.tt("dve", H[hi], su[fci % 2], ps[b][:, 256:512], ALU.mult, reads=[("su", fci % 2), "ps%d" % b], writes=[("H", hi)])
                for u in side.get(fci, []):
                    u()
                if pend is not None:
                    pend()

                def down(fci=fci, hi=hi, W2g=W2g, fc=fc, s=s):
                    for sc in range(2):
                        for dh in range(2):
                            P.mm(ps[4 + sc * 2 + dh][:, :], H[hi][:, sc * 128:(sc + 1) * 128], W2g[:, fc, dh * 512:(dh + 1) * 512],
                                 start=(fci == 0), stop=(fci == 2 * NFG - 1), reads=[("H", hi), ("W2", s)], writes=["ps%d" % (4 + sc * 2 + dh)])
                pend = down
            gi += 1
            if fg == NFG - 1:
                pend()
                pend = None
            if gi - 1 + PREF < len(groups):
                load(gi - 1 + PREF)
        for sc in range(2):
            for dh in range(2):
                k = sc * 2 + dh
                P.copy("act" if k % 2 else "dve", Yb[:, sc, dh * 512:(dh + 1) * 512], ps[4 + k][:, :], reads=["ps%d" % (4 + k)], writes=[("Yb", k)])
    e_last = experts[-1]
    sb_last = (len(experts) - 1) % 2
    for T in range(NT):
        for dh in range(2):
            scatter_unit(e_last, sb_last, T, dh)


def phase_D(P, A, dr, c, ps, x1, out):
    o = O_BC
    gfbc = A.at(o, [128, D], F32); o += 4 * KB
    junk = A.at(o, [128, D], BF16); o += 2 * KB
    ssf = A.at(o, [128, NT], F32); o += 64
    rstdf = A.at(o, [128, NT], F32); o += 64
    ob = [A.at(o + i * 4 * KB, [128, D], F32) for i in range(4)]; o += 16 * KB
    P.dma("sp", gfbc, dr["gfbc"], writes=["gfbc"])
    for T in range(NT):
        P.act(junk, x1[:, T, :], AF.Square, accum_out=ssf[:, T:T + 1], reads=[("x1", T)], writes=["junkD", "ssf"])
    P.act(rstdf, ssf, AF.Sqrt, bias=EPS, scale=1.0 / D, reads=["ssf"], writes=["rstdf"])
    P.recip(rstdf, rstdf, reads=["rstdf"], writes=["rstdf"])
    ov = out.rearrange("(t p) d -> p t d", p=128)
    for T in range(NT):
        i = T % 4
        P.stt(ob[i], x1[:, T, :], rstdf[:, T:T + 1], gfbc, ALU.mult, ALU.mult, reads=[("x1", T), "rstdf", "gfbc"], writes=[("ob", i)])
        P.dma("sp", ov[:, T, :], ob[i], reads=[("ob", i)])


def kernel(**inputs):
    nc = build(None)
    names = [a.memorylocations[0].name for a in nc.m.functions[0].allocations
             if isinstance(a, mybir.MemoryLocationSet) and a.kind == "ExternalInput"]
    in_maps = []
    for b in range(8):
        m = host_inputs(inputs, b)
        in_maps.append({k: v for k, v in m.items() if k in names})
    res = run_bass_kernel_spmd(nc, in_maps, core_ids=list(range(8)))
    return np.stack([np.asarray(r["out"], dtype=np.float32) for r in res.results], axis=0)
```
